# Optimizing a Trainium2 kernel written in Bass

```python
import jax, jax.numpy as jnp
from jax import lax
import numpy as np

D_MODEL = 1024
BATCH = 16
SEQ = 2048
DEPTH = 1

M_HEADS = 4
M_V_DIM = 128
M_QK_DIM = 64
M_CHUNK = 64
A_HEADS = 8
A_HEAD_DIM = 64
WINDOWS = (128, 512, 2048)
DILATIONS = (1, 4, 16)
ROT_DIM = A_HEAD_DIM // 4
ROPE_THETA = 500000.0
MIX_WIDTH = M_HEADS * M_V_DIM + A_HEADS * A_HEAD_DIM
IN_SPLITS = (M_HEADS * M_QK_DIM, M_HEADS * M_QK_DIM, M_HEADS * M_V_DIM, M_HEADS * M_V_DIM,
             4 * M_HEADS, A_HEADS * A_HEAD_DIM, A_HEADS * A_HEAD_DIM, A_HEADS * A_HEAD_DIM)
IN_COLS = sum(IN_SPLITS)
D_FF = 2816
N_MOD = 9
HALF_STEP = 0.5
EPS = 1e-6
NEG_INF = -1e30

kernel_name = 'hybrid_mlstm_dilated_attn_macaron_adaln'


def rms_norm(x, g):
    xf = x.astype(jnp.float32)
    y = xf * lax.rsqrt(jnp.mean(xf * xf, axis=-1, keepdims=True) + EPS)
    return (y * g.astype(jnp.float32)).astype(x.dtype)


def modulate(h, shift, scale):
    return h * (1.0 + scale) + shift


def swiglu(h, w_gu, w_down):
    g, u = jnp.split(h @ w_gu, 2, axis=-1)
    return (jax.nn.silu(g) * u) @ w_down


def partial_rope(t, pos):
    half = ROT_DIM // 2
    inv_freq = ROPE_THETA ** (-2.0 * jnp.arange(half, dtype=jnp.float32) / ROT_DIM)
    ang = pos.astype(jnp.float32)[:, None] * inv_freq[None, :]
    cos, sin = jnp.cos(ang), jnp.sin(ang)
    t1, t2 = t[..., :half], t[..., half:ROT_DIM]
    return jnp.concatenate([t1 * cos - t2 * sin, t2 * cos + t1 * sin, t[..., ROT_DIM:]], axis=-1)


def mlstm_scan(q, k, v, log_i, log_f):
    B, H, S, Dk = q.shape
    Dv = v.shape[-1]
    lc = min(M_CHUNK, S)
    nc = S // lc

    def chunks(t):
        return jnp.moveaxis(t.reshape((B, H, nc, lc) + t.shape[3:]), 2, 0)

    xs = (chunks(q), chunks(k), chunks(v), chunks(log_i), chunks(log_f))
    tril = jnp.tril(jnp.ones((lc, lc), dtype=bool))

    def step(carry, inp):
        C, n, m = carry
        q_c, k_c, v_c, li, lf = inp
        b = jnp.cumsum(lf, axis=-1)
        a_inter = b + m[..., None]
        d = jnp.where(tril, b[..., :, None] - b[..., None, :] + li[..., None, :], NEG_INF)
        m_t = jnp.maximum(a_inter, jnp.max(d, axis=-1))
        w_inter = jnp.exp(a_inter - m_t)
        s = jnp.exp(d - m_t[..., None]) * jnp.einsum('bhtd,bhsd->bhts', q_c, k_c)
        num = (w_inter[..., None] * jnp.einsum('bhvd,bhtd->bhtv', C, q_c)
               + jnp.einsum('bhts,bhsv->bhtv', s, v_c))
        den = w_inter * jnp.einsum('bhd,bhtd->bht', n, q_c) + jnp.sum(s, axis=-1)
        h = num / jnp.maximum(jnp.abs(den), jnp.exp(-m_t))[..., None]
        b_end = b[..., -1]
        g = b_end[..., None] - b + li
        m_new = jnp.maximum(b_end + m, jnp.max(g, axis=-1))
        decay = jnp.exp(b_end + m - m_new)
        wg = jnp.exp(g - m_new[..., None])
        C_new = decay[..., None, None] * C + jnp.einsum('bhs,bhsv,bhsd->bhvd', wg, v_c, k_c)
        n_new = decay[..., None] * n + jnp.einsum('bhs,bhsd->bhd', wg, k_c)
        return (C_new, n_new, m_new), h

    init = (jnp.zeros((B, H, Dv, Dk), jnp.float32), jnp.zeros((B, H, Dk), jnp.float32),
            jnp.full((B, H), NEG_INF, jnp.float32))
    _, hs = lax.scan(step, init, xs)
    return jnp.moveaxis(hs, 0, 2).reshape(B, H, S, Dv)


def bidirectional_mlstm(q, k, v, li_f, lf_f, li_b, lf_b):
    h_fwd = mlstm_scan(q, k, v, li_f, lf_f)
    flip = lambda t: jnp.flip(t, axis=2)
    h_bwd = flip(mlstm_scan(flip(q), flip(k), flip(v), flip(li_b), flip(lf_b)))
    return h_fwd + h_bwd


def dilated_band_attention(q, k, v, dilation, n_side):
    B, H, S, Dh = q.shape
    L = S // dilation

    def to_sub(t):
        return t.reshape(B, H, L, dilation, Dh).transpose(0, 1, 3, 2, 4)

    blk = n_side
    nb = -(-L // blk)
    lp = nb * blk
    qs = jnp.pad(to_sub(q), ((0, 0),) * 3 + ((0, lp - L), (0, 0)))
    kpad = ((0, 0),) * 3 + ((n_side, lp - L + n_side), (0, 0))
    ks = jnp.pad(to_sub(k), kpad)
    vs = jnp.pad(to_sub(v), kpad)
    qb = qs.reshape(B, H, dilation, nb, blk, Dh)

    def windows(t):
        tb = t.reshape(B, H, dilation, nb + 2, blk, Dh)
        return jnp.concatenate([tb[:, :, :, :-2], tb[:, :, :, 1:-1], tb[:, :, :, 2:]], axis=4)

    kw, vw = windows(ks), windows(vs)
    s = jnp.einsum('bhrnqe,bhrnke->bhrnqk', qb, kw)
    qi = jnp.arange(blk)[:, None]
    kk = jnp.arange(3 * blk)[None, :]
    band = jnp.abs(kk - n_side - qi) <= n_side
    key_idx = jnp.arange(nb)[:, None] * blk + kk - n_side
    in_range = (key_idx >= 0) & (key_idx < L)
    mask = band[None, :, :] & in_range[:, None, :]
    s = jnp.where(mask, s, NEG_INF)
    m = jnp.max(s, axis=-1, keepdims=True)
    p = jnp.exp(s - m)
    den = jnp.sum(p, axis=-1)
    o = jnp.einsum('bhrnqk,bhrnke->bhrnqe', p, vw) / den[..., None]
    lse = m[..., 0] + jnp.log(den)
    o = o.reshape(B, H, dilation, lp, Dh)[:, :, :, :L].transpose(0, 1, 3, 2, 4).reshape(B, H, S, Dh)
    lse = lse.reshape(B, H, dilation, lp)[..., :L].transpose(0, 1, 3, 2).reshape(B, H, S)
    return o, lse


def dilated_attention(q, k, v):
    outs, lses = [], []
    for w, d in zip(WINDOWS, DILATIONS):
        o, l = dilated_band_attention(q, k, v, d, w // (2 * d))
        outs.append(o)
        lses.append(l)
    wts = jax.nn.softmax(jnp.stack(lses), axis=0)
    return jnp.sum(wts[..., None] * jnp.stack(outs), axis=0)


def hybrid_mixer(h, w_in, gate_bias, g_q, g_k, g_mh, w_out):
    B, S, _ = h.shape
    proj = (h @ w_in).astype(jnp.float32)
    idx, acc = [], 0
    for n_cols in IN_SPLITS[:-1]:
        acc += n_cols
        idx.append(acc)
    mq, mk, mv, mo, mg, aq, ak, av = jnp.split(proj, idx, axis=-1)

    def heads(t, n):
        return t.reshape(B, S, n, -1).transpose(0, 2, 1, 3)

    gates = mg.reshape(B, S, 4, M_HEADS).transpose(2, 0, 3, 1) + gate_bias.astype(jnp.float32)[:, None, :, None]
    hm = bidirectional_mlstm(heads(mq, M_HEADS), heads(mk, M_HEADS) * (M_QK_DIM ** -0.5), heads(mv, M_HEADS),
                             gates[0], jax.nn.log_sigmoid(gates[1]), gates[2], jax.nn.log_sigmoid(gates[3]))
    hm = rms_norm(hm.transpose(0, 2, 1, 3), g_mh.reshape(M_HEADS, M_V_DIM))
    hm = (hm * jax.nn.sigmoid(mo.reshape(B, S, M_HEADS, M_V_DIM))).reshape(B, S, M_HEADS * M_V_DIM)

    pos = jnp.arange(S)
    qa = partial_rope(rms_norm(heads(aq, A_HEADS), g_q), pos) * (A_HEAD_DIM ** -0.5)
    ka = partial_rope(rms_norm(heads(ak, A_HEADS), g_k), pos)
    ha = dilated_attention(qa, ka, heads(av, A_HEADS))
    ha = ha.transpose(0, 2, 1, 3).reshape(B, S, A_HEADS * A_HEAD_DIM)

    return jnp.concatenate([hm, ha], axis=-1).astype(h.dtype) @ w_out


def setup_inputs(seed: int = 0) -> dict:
    key = jax.random.key(seed)
    ks = jax.random.split(key, 20)
    f32 = jnp.float32

    def nrm(k, shape, scale):
        return jax.random.normal(k, shape, f32) * scale

    gate_bias = (nrm(ks[9], (DEPTH, 4, M_HEADS), 0.1)
                 + jnp.array([0.0, 1.0, 0.0, 1.0], f32)[None, :, None]
                 * jnp.linspace(3.0, 6.0, M_HEADS, dtype=f32)[None, None, :])
    return {
        'x': nrm(ks[0], (BATCH, SEQ, D_MODEL), 1.0),
        'c': nrm(ks[1], (BATCH, D_MODEL), 1.0),
        'w_ada': nrm(ks[2], (DEPTH, D_MODEL, N_MOD * D_MODEL), 0.5 * D_MODEL ** -0.5),
        'b_ada': nrm(ks[3], (DEPTH, N_MOD * D_MODEL), 0.02),
        'g_ffn1': 1.0 + nrm(ks[4], (DEPTH, D_MODEL), 0.02),
        'w_gu1': nrm(ks[5], (DEPTH, D_MODEL, 2 * D_FF), D_MODEL ** -0.5),
        'w_down1': nrm(ks[6], (DEPTH, D_FF, D_MODEL), D_FF ** -0.5),
        'g_mix': 1.0 + nrm(ks[7], (DEPTH, D_MODEL), 0.02),
        'w_in': nrm(ks[8], (DEPTH, D_MODEL, IN_COLS), D_MODEL ** -0.5),
        'gate_bias': gate_bias,
        'g_q': 1.0 + nrm(ks[10], (DEPTH, A_HEAD_DIM), 0.02),
        'g_k': 1.0 + nrm(ks[11], (DEPTH, A_HEAD_DIM), 0.02),
        'g_mh': 1.0 + nrm(ks[12], (DEPTH, M_HEADS * M_V_DIM), 0.02),
        'w_out': nrm(ks[13], (DEPTH, MIX_WIDTH, D_MODEL), MIX_WIDTH ** -0.5),
        'g_ffn2': 1.0 + nrm(ks[14], (DEPTH, D_MODEL), 0.02),
        'w_gu2': nrm(ks[15], (DEPTH, D_MODEL, 2 * D_FF), D_MODEL ** -0.5),
        'w_down2': nrm(ks[16], (DEPTH, D_FF, D_MODEL), D_FF ** -0.5),
        'g_final': 1.0 + nrm(ks[17], (DEPTH, D_MODEL), 0.02),
    }


def reference(x, c, w_ada, b_ada, g_ffn1, w_gu1, w_down1, g_mix, w_in, gate_bias, g_q, g_k, g_mh,
              w_out, g_ffn2, w_gu2, w_down2, g_final):
    B, S, D = x.shape
    cs = jax.nn.silu(c)
    for l in range(DEPTH):
        mod = (cs @ w_ada[l] + b_ada[l]).reshape(B, N_MOD, D)[:, :, None, :]
        sh1, sc1, gt1, sh2, sc2, gt2, sh3, sc3, gt3 = [mod[:, i] for i in range(N_MOD)]
        h = modulate(rms_norm(x, g_ffn1[l]), sh1, sc1)
        x = x + HALF_STEP * gt1 * swiglu(h, w_gu1[l], w_down1[l])
        h = modulate(rms_norm(x, g_mix[l]), sh2, sc2)
        x = x + gt2 * hybrid_mixer(h, w_in[l], gate_bias[l], g_q[l], g_k[l], g_mh[l], w_out[l])
        h = modulate(rms_norm(x, g_ffn2[l]), sh3, sc3)
        x = x + HALF_STEP * gt3 * swiglu(h, w_gu2[l], w_down2[l])
        x = rms_norm(x, g_final[l])
    return x
```

```python
import os
import contextlib
import numpy as np
import ml_dtypes
import concourse.bass as bass
import concourse.mybir as mybir
from concourse.bass_utils import run_bass_kernel_spmd

F32 = mybir.dt.float32
BF16 = mybir.dt.bfloat16
AF = mybir.ActivationFunctionType
ALU = mybir.AluOpType
AX = mybir.AxisListType

ENG_NAMES = ("pe", "act", "dve", "pool", "sp")
D = 1024
S = 2048
DFF = 2816
NCH = 8
NF = 22
EPS = 1e-6
MASK_C = 1408
MASK_W = 2944
NSLOT = 3
SLOT_ELEMS = 4096


class Op:
    __slots__ = ("eng", "fn", "deps", "is_dma", "sem", "val", "has_dep", "idx", "name", "prewait")

    def __init__(self, eng, fn, name=""):
        self.eng = eng
        self.fn = fn
        self.deps = []
        self.is_dma = False
        self.sem = None
        self.val = 0
        self.has_dep = False
        self.idx = 0
        self.name = name
        self.prewait = None


class Graph:
    def __init__(self, nc, n_dma_sems=16):
        self.nc = nc
        self.ops = {e: [] for e in ENG_NAMES}
        self.last_writer = {}
        self.readers = {}
        self.n_dma_sems = n_dma_sems
        self.dma_count = {e: 0 for e in ENG_NAMES}
        self.dma_ops = {e: [] for e in ENG_NAMES}
        self.barrier_deps = {}
        self.sp_since_barrier = []

    def _link(self, op, deps):
        latest = {}
        keep = []
        for d in deps:
            if d is op:
                continue
            if d.is_dma:
                keep.append(d)
                continue
            if d.eng == "pe" and op.eng == "pe":
                continue
            cur = latest.get(d.eng)
            if cur is None or d.idx > cur.idx:
                latest[d.eng] = d
        keep.extend(latest.values())
        seen = set(id(d) for d in op.deps)
        for d in keep:
            if id(d) in seen:
                continue
            seen.add(id(d))
            op.deps.append(d)
            d.has_dep = True

    def _add_deps(self, op, reads, writes):
        deps = []
        for r in reads:
            w = self.last_writer.get(r)
            if w is not None:
                deps.append(w)
            if (isinstance(r, tuple) and r[0] == "ps") or (isinstance(r, str) and r.startswith("psT")):
                deps.extend(x for x in self.readers.get(r, ()) if x.eng != op.eng)
        for w_ in writes:
            w = self.last_writer.get(w_)
            if w is not None:
                deps.append(w)
            deps.extend(self.readers.get(w_, ()))
        b = self.barrier_deps.pop(op.eng, None)
        if b:
            deps.extend(b)
        self._link(op, deps)
        for r in reads:
            self.readers.setdefault(r, []).append(op)
        for w_ in writes:
            self.last_writer[w_] = op
            self.readers[w_] = []

    def op(self, eng, fn, reads=(), writes=(), name=""):
        o = Op(eng, fn, name)
        o.idx = len(self.ops[eng])
        self._add_deps(o, reads, writes)
        self.ops[eng].append(o)
        return o

    def dma(self, eng, out, in_, reads=(), writes=(), name=""):
        def fn(e, out=out, in_=in_):
            return e.dma_start(out=out, in_=in_)
        o = Op(eng, fn, name)
        o.is_dma = True
        i = self.dma_count[eng]
        self.dma_count[eng] += 1
        o.idx = i
        o.val = 16 * (i // self.n_dma_sems + 1)
        if i >= self.n_dma_sems:
            o.prewait = self.dma_ops[eng][i - self.n_dma_sems]
        self.dma_ops[eng].append(o)
        self._add_deps(o, reads, writes)
        self.ops[eng].append(o)
        if eng == "sp":
            self.sp_since_barrier.append(o)
        return o

    def barrier(self):
        b = []
        for e in ("pe", "act", "dve"):
            for o in reversed(self.ops[e]):
                b.append(o)
                break
        b.extend(self.sp_since_barrier)
        self.sp_since_barrier = []
        for e in ("pe", "act", "dve", "sp"):
            self.barrier_deps[e] = list(b) + list(self.barrier_deps.get(e, ()))

    def emit(self, final_wait_ops=()):
        nc = self.nc
        with contextlib.ExitStack() as st:
            esem = {e: st.enter_context(nc.semaphore("s_" + e)) for e in ENG_NAMES}
            dsem = {}
            for e in ENG_NAMES:
                if self.dma_count[e]:
                    dsem[e] = [st.enter_context(nc.semaphore("d_%s_%d" % (e, i)))
                               for i in range(min(self.n_dma_sems, self.dma_count[e]))]
            for e in ENG_NAMES:
                m = 0
                for o in self.ops[e]:
                    if o.is_dma:
                        o.sem = dsem[e][o.idx % self.n_dma_sems]
                    elif o.has_dep:
                        m += 1
                        o.sem = esem[e]
                        o.val = m
            block = st.enter_context(nc.Block())
            handles = {"pe": block.tensor, "act": block.scalar, "dve": block.vector,
                       "pool": block.gpsimd, "sp": block.sync}

            def make(e):
                ops = self.ops[e]

                def body(eng):
                    waited = {}

                    def wait(d):
                        key = id(d.sem)
                        if waited.get(key, 0) >= d.val:
                            return
                        waited[key] = d.val
                        eng.wait_ge(d.sem, d.val)
                    for o in ops:
                        if o.prewait is not None:
                            wait(o.prewait)
                        for d in o.deps:
                            wait(d)
                        inst = o.fn(eng)
                        if o.is_dma:
                            inst.then_inc(o.sem, 16)
                        elif o.has_dep:
                            inst.then_inc(o.sem, 1)
                    if e == "sp":
                        for d in final_wait_ops:
                            wait(d)
                return body
            for e in ENG_NAMES:
                if self.ops[e] or (e == "sp" and final_wait_ops):
                    handles[e](make(e))


class Arena:
    def __init__(self, ap2d, nelem_bf16):
        self.ap = ap2d
        self.cap = nelem_bf16
        self.off = 0

    def mark(self):
        return self.off

    def release(self, m):
        self.off = m

    def alloc(self, nelem, dt):
        n16 = nelem * (2 if dt == F32 else 1)
        self.off = (self.off + 1) // 2 * 2
        assert self.off + n16 <= self.cap, ("arena overflow", self.off, n16, self.cap)
        a = self.ap[:, self.off:self.off + n16]
        self.off += n16
        if dt == F32:
            a = a.bitcast(F32)
        return a


def build_program(stage=3):
    nc = bass.Bass("TRN2", target_bir_lowering=False)

    def din(name, shape, dt=F32):
        return nc.dram_tensor(name, list(shape), dt, kind="ExternalInput").ap()
    xT_d = din("xT", [2, D, S])
    cT_d = din("cT", [128, NCH, 2])
    wada_d = din("w_ada", [D, 9 * D])
    bada_d = din("b_adaT", [128, 72])
    g4_d = din("g4", [128, 4, NCH])
    wgu_d = [din("w_gu1", [D, 2 * DFF]), din("w_gu2", [D, 2 * DFF])]
    wdn_d = [din("w_down1", [DFF, D]), din("w_down2", [DFF, D])]
    win_d = din("w_in", [D, 3088])
    wout_d = din("w_out", [D, D])
    gbias_d = din("gbias_rep", [128, 16])
    gqk_d = din("gqk_rep", [128, 4, 64])
    gmh_d = din("gmh_rep", [128, 512])
    mask_d = din("maskT", [128, MASK_W], BF16)
    identb_d = din("identb", [128, 128], BF16)
    trif_d = din("trif", [128, 128])
    trib_d = din("trib", [128, 128])
    mkf_d = din("maskf", [128, 128], BF16)
    mkb_d = din("maskb", [128, 128], BF16)
    cos_d = din("cos4", [128, 16, 4, 8])
    sin_d = din("sin4", [128, 16, 4, 8])
    out_d = nc.dram_tensor("outT", [2, D, S], F32, kind="ExternalOutput").ap()

    st = contextlib.ExitStack()
    with st:
        def sbt(name, shape, dt):
            return st.enter_context(nc.sbuf_tensor(name, shape, dt))
        xT = sbt("xT_sb", [128, NCH, S], F32)
        ring = sbt("ring", [128, NSLOT, SLOT_ELEMS], BF16)
        maskT = sbt("maskT_sb", [128, MASK_W], BF16)
        identb = sbt("identb_sb", [128, 128], BF16)
        onesb = sbt("onesb", [128, 128], BF16)
        onesf = sbt("onesf", [128, 128], F32)
        trif = sbt("trif_sb", [128, 128], F32)
        trib = sbt("trib_sb", [128, 128], F32)
        mkf = sbt("mkf_sb", [128, 128], BF16)
        mkb = sbt("mkb_sb", [128, 128], BF16)
        cos4 = sbt("cos4_sb", [128, 16, 4, 8], F32)
        sin4 = sbt("sin4_sb", [128, 16, 4, 8], F32)
        gqk = sbt("gqk_sb", [128, 4, 64], F32)
        gmh = sbt("gmh_sb", [128, 512], F32)
        gbias = sbt("gbias_sb", [128, 16], F32)
        g4 = sbt("g4_sb", [128, 4, NCH], F32)
        badaT = sbt("bada_sb", [128, 72], F32)
        cT = sbt("cT_sb", [128, NCH, 2], F32)
        csT = sbt("csT_sb", [128, NCH, 2], BF16)
        modT = sbt("modT", [128, 72, 2], F32)
        geff = sbt("geff", [128, 2, 3, NCH], F32)
        gate = sbt("gate", [128, 2, 3, NCH], F32)
        ARENA_N = 52736
        arena_t = sbt("arena", [128, ARENA_N], BF16)
        ar = Arena(arena_t[:, :], ARENA_N)
        ps = [st.enter_context(nc.psum_tensor("ps%d" % i, [128, 512], F32)) for i in range(8)]
        psT = ps[7][:, :].bitcast(BF16)
        PST = ("ps", 7)

        g = Graph(nc)
        ring_ctr = [0]
        DBG = os.environ.get("MK_DEBUG") == "1"
        dbg_outs = []

        def dbg(name, ap, shape, dt, reads, b=0):
            if not DBG or b != 0:
                return
            t = nc.dram_tensor("dbg_" + name, list(shape), dt, kind="ExternalOutput").ap()
            dbg_outs.append(g.dma("sp", t, ap, reads=reads))

        def wload(src_aps, views, name=""):
            s = ring_ctr[0] % NSLOT
            ring_ctr[0] += 1
            for src, vw in zip(src_aps, views):
                g.dma("pool", vw(ring[:, s, :]), src, writes=[("ring", s)], name=name)
            return s

        for dst, src, key in ((maskT[:, :], mask_d, "maskT"), (identb[:, :], identb_d, "identb"),
                              (trif[:, :], trif_d, "trif"), (trib[:, :], trib_d, "trib"),
                              (mkf[:, :], mkf_d, "mkf"), (mkb[:, :], mkb_d, "mkb"),
                              (cos4[:], cos_d, "cos4"), (sin4[:], sin_d, "sin4"),
                              (gqk[:], gqk_d, "gqk"), (gmh[:, :], gmh_d, "gmh"), (gbias[:, :], gbias_d, "gbias"),
                              (g4[:], g4_d, "g4"), (badaT[:, :], bada_d, "badaT"), (cT[:], cT_d, "cT")):
            g.dma("sp", dst, src, writes=[key])
        g.op("dve", lambda e: e.memset(onesb[:, :], 1.0), writes=["onesb"])
        g.op("dve", lambda e: e.memset(onesf[:, :], 1.0), writes=["onesf"])
        g.op("dve", lambda e: e.tensor_scalar(out=gqk[:, 0:2, :], in0=gqk[:, 0:2, :], scalar1=0.125, scalar2=None,
                                              op0=ALU.mult), reads=["gqk"], writes=["gqk"])

        def load_x(b):
            for c in range(NCH):
                g.dma("sp", xT[:, c, :], xT_d[b, c * 128:(c + 1) * 128, :],
                      writes=[("xT", c, blk) for blk in range(4)])

        g.op("act", lambda e: e.activation(out=csT[:], in_=cT[:], func=AF.Silu), reads=["cT"], writes=["csT"])
        wada_v = wada_d.rearrange("(c p) n -> p c n", p=128)

        def adaln(groups):
            for grp in groups:
                s = wload([wada_v[:, :, grp * 512:(grp + 1) * 512]],
                          [lambda sl: sl.rearrange("p (c n) -> p c n", c=NCH)])
                wv = ring[:, s, :].rearrange("p (c n) -> p c n", c=NCH)
                for j in range(4):
                    n = grp * 4 + j
                    for k in range(NCH):
                        g.op("pe", lambda e, n=n, k=k, j=j, wv=wv: e.matmul(
                            ps[6][:, 2 * n:2 * n + 2], lhsT=wv[:, k, j * 128:(j + 1) * 128], rhs=csT[:, k, :],
                            start=(k == 0), stop=(k == NCH - 1), skip_group_check=True),
                            reads=[("ring", s), "csT"], writes=[("ps", 6)])
            n0, n1 = groups[0] * 4, groups[-1] * 4 + 4
            part = 0 if n0 == 0 else 1
            for b in range(2):
                g.op("dve", lambda e, b=b: e.tensor_tensor(
                    out=modT[:, n0:n1, b], in0=ps[6][:, 2 * n0 + b:2 * n1 + b:2], in1=badaT[:, n0:n1], op=ALU.add),
                    reads=[("ps", 6), "badaT"], writes=[("modT", part, b)])

        def derive(i):
            n0 = 3 * i * 8
            part = 0 if i == 0 else 1
            for b in range(2):
                g.op("dve", lambda e, b=b: e.scalar_tensor_tensor(
                    out=geff[:, b, i, :], in0=modT[:, n0 + 8:n0 + 16, b], scalar=1.0, in1=g4[:, i, :],
                    op0=ALU.add, op1=ALU.mult), reads=[("modT", part, b), "g4"], writes=[("geff", b, i)])
                g.op("dve", lambda e, b=b: e.tensor_scalar(
                    out=gate[:, b, i, :], in0=modT[:, n0 + 16:n0 + 24, b], scalar1=(1.0 if i == 1 else 0.5),
                    scalar2=None, op0=ALU.mult), reads=[("modT", part, b)], writes=[("gate", b, i)])

        def shift_col(b, i, c):
            return modT[:, 3 * i * 8 + c, b:b + 1]

        def rms_rstd(b, rstd, sq):
            for blk in range(4):
                tok = slice(blk * 512, (blk + 1) * 512)
                for c in range(NCH):
                    g.op("act", lambda e, c=c, tok=tok: e.activation(out=sq[:, c, :], in_=xT[:, c, tok], func=AF.Square),
                         reads=[("xT", c, blk)], writes=[("sq", c)])
                for c in range(NCH):
                    g.op("pe", lambda e, c=c: e.matmul(ps[6][:, :], lhsT=onesb[:, :], rhs=sq[:, c, :],
                                                      start=(c == 0), stop=(c == NCH - 1)),
                         reads=["onesb", ("sq", c)], writes=[("ps", 6)])
                g.op("act", lambda e, tok=tok: e.activation(out=rstd[:, tok], in_=ps[6][:, :], func=AF.Ln,
                                                            bias=EPS, scale=1.0 / D),
                     reads=[("ps", 6)], writes=[("rstd", blk)])
            for blk in range(4):
                tok = slice(blk * 512, (blk + 1) * 512)
                g.op("act", lambda e, tok=tok: e.activation(out=rstd[:, tok], in_=rstd[:, tok], func=AF.Exp, scale=-0.5),
                     reads=[("rstd", blk)], writes=[("rstd", blk)])

        def norm_mod(b, i, rstd, tmp, dst_fn, blk, keyfn):
            tok = slice(blk * 512, (blk + 1) * 512)
            for c in range(NCH):
                tb = tmp[c % 2]
                g.op("dve", lambda e, c=c, tb=tb: e.tensor_tensor(out=tb, in0=xT[:, c, tok], in1=rstd[:, tok], op=ALU.mult),
                     reads=[("xT", c, blk), ("rstd", blk)], writes=[("tmp", c % 2)])
                g.op("act", lambda e, c=c, tb=tb: e.activation(out=dst_fn(c), in_=tb, func=AF.Identity,
                                                               bias=shift_col(b, i, c), scale=geff[:, b, i, c:c + 1]),
                     reads=[("tmp", c % 2), ("geff", b, i), ("modT", 0 if i == 0 else 1, b)], writes=[keyfn(c)])

        def ffn(b, i, which):
            wgu_v = wgu_d[which].rearrange("(c p) n -> p c n", p=128)
            wdn_v = wdn_d[which].rearrange("(k p) n -> p k n", p=128)
            m0 = ar.mark()
            rstd = ar.alloc(S, F32)
            sq = ar.alloc(NCH * 512, BF16).rearrange("p (c n) -> p c n", c=NCH)
            tmp = [ar.alloc(512, F32) for _ in range(2)]
            hT = ar.alloc(NCH * 1024, BF16).rearrange("p (c n) -> p c n", c=NCH)
            actT = ar.alloc(NF * 1024, BF16).rearrange("p (c n) -> p c n", c=NF)
            sg = [ar.alloc(512, BF16) for _ in range(2)]
            rms_rstd(b, rstd, sq)
            cnt = 0
            for sb in range(2):
                for lb in range(2):
                    blk = sb * 2 + lb
                    norm_mod(b, i, rstd, tmp, lambda c, lb=lb: hT[:, c, lb * 512:(lb + 1) * 512], blk,
                             lambda c, lb=lb: ("hT", c, lb))
                for grp in range(11):
                    c0 = grp * 256
                    sgw = wload([wgu_v[:, :, c0:c0 + 256], wgu_v[:, :, DFF + c0:DFF + c0 + 256]],
                                [lambda sl, q=q: sl.rearrange("p (c n) -> p c n", c=NCH)[:, :, q * 256:(q + 1) * 256]
                                 for q in range(2)])
                    wgv = ring[:, sgw, :].rearrange("p (c n) -> p c n", c=NCH)
                    for j in range(2):
                        f = grp * 2 + j
                        for lb in range(2):
                            pg = cnt % 2
                            cnt += 1
                            hs = slice(lb * 512, (lb + 1) * 512)
                            for k in range(NCH):
                                g.op("pe", lambda e, k=k, j=j, wgv=wgv, pg=pg, hs=hs: e.matmul(
                                    ps[pg][:, :], lhsT=wgv[:, k, j * 128:(j + 1) * 128], rhs=hT[:, k, hs],
                                    start=(k == 0), stop=(k == NCH - 1)),
                                    reads=[("ring", sgw), ("hT", k, lb)], writes=[("ps", pg)])
                            for k in range(NCH):
                                g.op("pe", lambda e, k=k, j=j, wgv=wgv, pg=pg, hs=hs: e.matmul(
                                    ps[2 + pg][:, :], lhsT=wgv[:, k, 256 + j * 128:256 + (j + 1) * 128], rhs=hT[:, k, hs],
                                    start=(k == 0), stop=(k == NCH - 1)),
                                    reads=[("ring", sgw), ("hT", k, lb)], writes=[("ps", 2 + pg)])
                            g.op("act", lambda e, pg=pg: e.activation(out=sg[pg], in_=ps[pg][:, :], func=AF.Silu),
                                 reads=[("ps", pg)], writes=[("sg", pg)])
                            g.op("dve", lambda e, pg=pg, f=f, hs=hs: e.tensor_tensor(
                                out=actT[:, f, hs], in0=ps[2 + pg][:, :], in1=sg[pg], op=ALU.mult),
                                reads=[("ps", 2 + pg), ("sg", pg)], writes=[("actT", f, lb)])
                for dc in range(NCH):
                    sw = wload([wdn_v[:, :, dc * 128:(dc + 1) * 128]],
                               [lambda sl: sl[:, 0:NF * 128].rearrange("p (k n) -> p k n", k=NF)])
                    wd = ring[:, sw, 0:NF * 128].rearrange("p (k n) -> p k n", k=NF)
                    for lb in range(2):
                        blk = sb * 2 + lb
                        pd = 4 + (cnt % 2)
                        cnt += 1
                        hs = slice(lb * 512, (lb + 1) * 512)
                        for k in range(NF):
                            g.op("pe", lambda e, k=k, wd=wd, pd=pd, hs=hs: e.matmul(
                                ps[pd][:, :], lhsT=wd[:, k, :], rhs=actT[:, k, hs],
                                start=(k == 0), stop=(k == NF - 1)),
                                reads=[("ring", sw), ("actT", k, lb)], writes=[("ps", pd)])
                        tok = slice(blk * 512, (blk + 1) * 512)
                        g.op("dve", lambda e, pd=pd, dc=dc, tok=tok: e.scalar_tensor_tensor(
                            out=xT[:, dc, tok], in0=ps[pd][:, :], scalar=gate[:, b, i, dc:dc + 1], in1=xT[:, dc, tok],
                            op0=ALU.mult, op1=ALU.add),
                            reads=[("ps", pd), ("gate", b, i), ("xT", dc, blk)], writes=[("xT", dc, blk)])
            ar.release(m0)
            g.barrier()

        def mixer(b):
            i = 1
            win_v = win_d.rearrange("(c p) n -> p c n", p=128)
            wout_v = wout_d.rearrange("(k p) n -> p k n", p=128)
            m0 = ar.mark()
            h2T = ar.alloc(NCH * S, BF16).rearrange("p (c n) -> p c n", c=NCH)
            mixh = ar.alloc(4 * S, BF16).rearrange("p (c n) -> p c n", c=4)
            m1 = ar.mark()
            rstd = ar.alloc(S, F32)
            sq = ar.alloc(NCH * 512, BF16).rearrange("p (c n) -> p c n", c=NCH)
            tmp = [ar.alloc(512, F32) for _ in range(2)]
            rms_rstd(b, rstd, sq)
            for blk in range(4):
                norm_mod(b, i, rstd, tmp, lambda c, blk=blk: h2T[:, c, blk * 512:(blk + 1) * 512], blk,
                         lambda c, blk=blk: ("h2T", c, blk))
            dbg("h2T", h2T, [128, NCH, S], BF16, [("h2T", c, blk) for c in range(NCH) for blk in range(4)], b)
            ar.release(m1)
            g.barrier()

            def wout_half(half):
                cnt = 0
                for dcp in range(2):
                    sw = wload([wout_v[:, half * 4:half * 4 + 4, dcp * 512:(dcp + 1) * 512]],
                               [lambda sl: sl[:, 0:4 * 512].rearrange("p (k n) -> p k n", k=4)])
                    wo = ring[:, sw, 0:4 * 512].rearrange("p (k n) -> p k n", k=4)
                    for dj in range(4):
                        dc = dcp * 4 + dj
                        for blk in range(4):
                            pd = 4 + (cnt % 2)
                            cnt += 1
                            tok = slice(blk * 512, (blk + 1) * 512)
                            for k in range(4):
                                g.op("pe", lambda e, k=k, wo=wo, pd=pd, dj=dj, tok=tok: e.matmul(
                                    ps[pd][:, :], lhsT=wo[:, k, dj * 128:(dj + 1) * 128], rhs=mixh[:, k, tok],
                                    start=(k == 0), stop=(k == 3)),
                                    reads=[("ring", sw), ("mixh", k, blk)], writes=[("ps", pd)])
                            g.op("dve", lambda e, pd=pd, dc=dc, tok=tok: e.scalar_tensor_tensor(
                                out=xT[:, dc, tok], in0=ps[pd][:, :], scalar=gate[:, b, i, dc:dc + 1], in1=xT[:, dc, tok],
                                op0=ALU.mult, op1=ALU.add),
                                reads=[("ps", pd), ("gate", b, i), ("xT", dc, blk)], writes=[("xT", dc, blk)])

            m2 = ar.mark()
            aqT = [ar.alloc(S, BF16) for _ in range(2)]
            akT = [ar.alloc(S, BF16) for _ in range(2)]
            av2 = [ar.alloc(16 * 2 * 128, BF16).rearrange("p (t h n) -> p t h n", t=16, h=2) for _ in range(2)]
            ytm4 = ar.alloc(1024, F32)
            ysq4 = ar.alloc(1024, F32)
            yb4 = ar.alloc(1024, BF16)
            st16 = ar.alloc(16, F32)
            rp4 = ar.alloc(4 * 128, F32).rearrange("p (k a n) -> p k a n", k=4, a=16)
            Eb = [ar.alloc(512, BF16) for _ in range(4)]
            Pm = [ar.alloc(512, BF16) for _ in range(4)]
            rd = [ar.alloc(512, F32)] * 2
            SBANK = [0, 1, 2, 4]
            for par in range(2):
                g.op("dve", lambda e, par=par: e.memset(av2[par][:, :, :, 64:128], 1.0), writes=[("av2ones", par)])
            y16 = ytm4.rearrange("p (a n) -> p a n", a=16)
            s16 = ysq4.rearrange("p (a n) -> p a n", a=16)
            yb16 = yb4.rearrange("p (a n) -> p a n", a=16)
            y44 = ytm4.rearrange("p (t a n) -> p t a n", t=4, a=4)
            st_b = st16.rearrange("p (a o) -> p a o", o=1).to_broadcast([128, 16, 64])
            gqk_b = gqk[:].rearrange("p (o a) n -> p o a n", o=1).to_broadcast([128, 4, 4, 64])

            def load_wa(hp):
                sw = wload([win_v[:, :, 1552 + hp * 128:1552 + hp * 128 + 128],
                            win_v[:, :, 2064 + hp * 128:2064 + hp * 128 + 128],
                            win_v[:, :, 2576 + hp * 128:2576 + hp * 128 + 128]],
                           [lambda sl, q=q: sl[:, 0:NCH * 384].rearrange("p (c n) -> p c n", c=NCH)[:, :, q * 128:(q + 1) * 128]
                            for q in range(3)])
                return ring[:, sw, 0:NCH * 384].rearrange("p (c n) -> p c n", c=NCH), sw

            def proj_p1(hp, tg, wa, sw):
                par = hp % 2
                for tl in range(4):
                    tt = tg * 4 + tl
                    ts_ = slice(tt * 128, (tt + 1) * 128)
                    bank = 5 + tl // 2
                    co = (tl % 2) * 256
                    for c in range(NCH):
                        g.op("pe", lambda e, c=c, bank=bank, co=co, ts_=ts_: e.matmul(ps[bank][:, co:co + 256], lhsT=h2T[:, c, ts_], rhs=wa[:, c, 0:256],
                                                          start=(c == 0), stop=(c == NCH - 1), skip_group_check=True),
                             reads=[("ring", sw), ("h2T", c, tg)], writes=[("ps", bank)])
                    for c in range(NCH):
                        g.op("pe", lambda e, c=c, tl=tl, ts_=ts_: e.matmul(ps[7][:, tl * 128:(tl + 1) * 128], lhsT=h2T[:, c, ts_],
                                                          rhs=wa[:, c, 256:384], start=(c == 0), stop=(c == NCH - 1),
                                                          skip_group_check=True),
                             reads=[("ring", sw), ("h2T", c, tg)], writes=[("ps", 7)])
                g.op("act", lambda e: e.copy(out=ytm4[:, 0:512], in_=ps[5][:, :]), reads=[("ps", 5)], writes=["ytm4a"])
                g.op("act", lambda e: e.copy(out=ytm4[:, 512:1024], in_=ps[6][:, :]), reads=[("ps", 6)], writes=["ytm4b"])
                g.op("act", lambda e: e.copy(out=av2[par][:, tg * 4:(tg + 1) * 4, :, 0:64],
                                             in_=ps[7][:, :].rearrange("p (t h n) -> p t h n", t=4, h=2)),
                     reads=[("ps", 7)], writes=[("av2", par, tg)])
                yk = ["ytm4a", "ytm4b"]
                g.op("dve", lambda e: e.tensor_tensor(out=ysq4, in0=ytm4, in1=ytm4, op=ALU.mult), reads=yk, writes=["ysq4"])
                g.op("dve", lambda e: e.tensor_reduce(out=st16, in_=s16, axis=AX.X, op=ALU.add), reads=["ysq4"], writes=["st16"])
                g.op("act", lambda e: e.activation(out=st16, in_=st16, func=AF.Ln, bias=EPS, scale=1.0 / 64),
                     reads=["st16"], writes=["st16"])
                g.op("act", lambda e: e.activation(out=st16, in_=st16, func=AF.Exp, scale=-0.5), reads=["st16"], writes=["st16"])
                g.op("dve", lambda e: e.tensor_tensor(out=y16, in0=y16, in1=st_b, op=ALU.mult), reads=yk + ["st16"], writes=yk)
                g.op("dve", lambda e: e.tensor_tensor(out=y44, in0=y44, in1=gqk_b, op=ALU.mult), reads=yk + ["gqk"], writes=yk)
                g.op("act", lambda e: e.copy(out=yb4, in_=ytm4), reads=yk, writes=["yb4"])
                t1 = y16[:, :, 0:8]
                t2 = y16[:, :, 8:16]
                cs_ = cos4[:, tg * 4:(tg + 1) * 4, :, :].rearrange("p t a n -> p (t a) n")
                sn_ = sin4[:, tg * 4:(tg + 1) * 4, :, :].rearrange("p t a n -> p (t a) n")
                g.op("dve", lambda e: e.tensor_tensor(out=rp4[:, 0], in0=t1, in1=cs_, op=ALU.mult), reads=yk + ["cos4"], writes=[("rp4", 0)])
                g.op("dve", lambda e: e.tensor_tensor(out=rp4[:, 1], in0=t2, in1=sn_, op=ALU.mult), reads=yk + ["sin4"], writes=[("rp4", 1)])
                g.op("dve", lambda e: e.tensor_tensor(out=rp4[:, 2], in0=t2, in1=cs_, op=ALU.mult), reads=yk + ["cos4"], writes=[("rp4", 2)])
                g.op("dve", lambda e: e.tensor_tensor(out=rp4[:, 3], in0=t1, in1=sn_, op=ALU.mult), reads=yk + ["sin4"], writes=[("rp4", 3)])
                g.op("dve", lambda e: e.tensor_tensor(out=yb16[:, :, 0:8], in0=rp4[:, 0], in1=rp4[:, 1], op=ALU.subtract),
                     reads=[("rp4", 0), ("rp4", 1), "yb4"], writes=["yb4"])
                g.op("dve", lambda e: e.tensor_tensor(out=yb16[:, :, 8:16], in0=rp4[:, 2], in1=rp4[:, 3], op=ALU.add),
                     reads=[("rp4", 2), ("rp4", 3), "yb4"], writes=["yb4"])

            def proj_p2(hp, tg):
                par = hp % 2
                for tl in range(4):
                    g.op("pe", lambda e, tl=tl: e.transpose(psT[:, tl * 128:(tl + 1) * 128], yb4[:, tl * 256:tl * 256 + 128],
                                                            identb[:, :]), reads=["yb4", "identb"], writes=[PST])
                    g.op("pe", lambda e, tl=tl: e.transpose(psT[:, 512 + tl * 128:512 + (tl + 1) * 128],
                                                            yb4[:, tl * 256 + 128:tl * 256 + 256], identb[:, :]),
                         reads=["yb4", "identb"], writes=[PST])
                qs = slice(tg * 512, (tg + 1) * 512)
                g.op("act", lambda e: e.copy(out=aqT[par][:, qs], in_=psT[:, 0:512]), reads=[PST], writes=[("aqT", par, tg)])
                g.op("dve", lambda e: e.tensor_copy(out=akT[par][:, qs], in_=psT[:, 512:1024]), reads=[PST], writes=[("akT", par, tg)])

            ecnt = 0
            ocnt = 0
            wa_cur = load_wa(0)
            for tg in range(4):
                proj_p1(0, tg, *wa_cur)
                proj_p2(0, tg)
            for hp in range(4):
                par = hp % 2
                wa_nxt = load_wa(hp + 1) if hp < 3 else None
                parts = []
                if wa_nxt is not None:
                    for tg in range(4):
                        parts.append(lambda tg=tg, wa_nxt=wa_nxt, hp=hp: proj_p1(hp + 1, tg, *wa_nxt))
                        parts.append(lambda tg=tg, hp=hp: proj_p2(hp + 1, tg))
                steps = []
                seg_end = []
                for hl in range(2):
                    for qb in range(4):
                        kts = list(range(max(0, qb * 4 - 8), min(15, qb * 4 + 11) + 1))
                        po = 3
                        ocnt += 1
                        for n_, kt in enumerate(kts):
                            steps.append((hl, qb, kt, n_, n_ == len(kts) - 1, po, (ecnt + len(steps)) % 4))
                        seg_end.append(len(steps) - 1)
                ecnt += len(steps)
                LA = 3

                def emit_score(si, hp=hp, par=par):
                    hl, qb, kt, n_, last, po, pS = steps[si]
                    rows = slice(hl * 64, (hl + 1) * 64)
                    qs = slice(qb * 512, (qb + 1) * 512)
                    ks = slice(kt * 128, (kt + 1) * 128)
                    j0 = MASK_C - (kt * 128 - qb * 512)
                    bk = SBANK[pS]
                    g.op("pe", lambda e: e.matmul(ps[bk][:, :], lhsT=akT[par][rows, ks], rhs=aqT[par][rows, qs], start=True, stop=True),
                         reads=[("akT", par, kt // 4), ("aqT", par, qb)], writes=[("ps", bk)])
                    g.op("act", lambda e: e.activation(out=Eb[pS], in_=ps[bk][:, :], func=AF.Exp),
                         reads=[("ps", bk)], writes=[("Eb", pS)])
                    g.op("dve", lambda e: e.tensor_tensor(out=Pm[pS], in0=Eb[pS], in1=maskT[:, j0:j0 + 512], op=ALU.mult),
                         reads=[("Eb", pS), "maskT"], writes=[("Pm", pS)])

                def emit_pv(si, hp=hp, par=par):
                    hl, qb, kt, n_, last, po, pS = steps[si]
                    rows = slice(hl * 64, (hl + 1) * 64)
                    qs = slice(qb * 512, (qb + 1) * 512)
                    g.op("pe", lambda e: e.matmul(ps[po][:, :], lhsT=av2[par][:, kt, hl, :], rhs=Pm[pS], start=(n_ == 0), stop=last),
                         reads=[("av2", par, kt // 4), ("av2ones", par), ("Pm", pS)], writes=[("ps", po)])
                    if last:
                        rr = rd[0]
                        g.op("dve", lambda e: e.reciprocal(out=rr[0:64, :], in_=ps[po][64:128, :]),
                             reads=[("ps", po)], writes=[("rd", 0)])
                        g.op("dve", lambda e: e.tensor_tensor(out=mixh[rows, hp, qs], in0=ps[po][0:64, :], in1=rr[0:64, :],
                                                              op=ALU.mult),
                             reads=[("ps", po), ("rd", 0)], writes=[("mixh", hp, qb)])
                for si in range(len(steps) + LA):
                    if si < len(steps):
                        emit_score(si)
                    if si >= LA:
                        emit_pv(si - LA)
                    if si in seg_end and parts:
                        parts.pop(0)()
                while parts:
                    parts.pop(0)()
            dbg("mixa", mixh, [128, 4, S], BF16, [("mixh", k_, q_) for k_ in range(4) for q_ in range(4)], b)
            wout_half(1)
            ar.release(m2)
            g.barrier()

            m3 = ar.mark()
            G = ar.alloc(16 * 16, F32).rearrange("p (t n) -> p t n", t=16)
            G4 = G.rearrange("p t (a h) -> p t a h", a=4)
            LF = ar.alloc(16 * 8, F32).rearrange("p (t n) -> p t n", t=16)
            LF4 = LF.rearrange("p t (a h) -> p t a h", a=2)
            sB = ar.alloc(16 * 16, F32).rearrange("p (t n) -> p t n", t=16)
            T1 = ar.alloc(16 * 8, F32).rearrange("p (t n) -> p t n", t=16)
            T2 = T1
            Call = ar.alloc(16 * 8, F32).rearrange("p (t n) -> p t n", t=16)
            Aall = ar.alloc(16 * 8, F32).rearrange("p (t n) -> p t n", t=16)
            EBa = ar.alloc(16 * 8, F32).rearrange("p (t n) -> p t n", t=16)
            EBH = ar.alloc(16 * 8, F32).rearrange("p (t n) -> p t n", t=16)
            mqT = ar.alloc(S, BF16)
            mkT = ar.alloc(S, BF16)
            ktok = ar.alloc(16 * 128, BF16).rearrange("p (t n) -> p t n", t=16)
            V1 = ar.alloc(16 * 2 * 130, BF16).rearrange("p (t h n) -> p t h n", t=16, h=2)
            sgb = [ar.alloc(256, BF16) for _ in range(2)]
            hacc = ar.alloc(16 * 256, F32).rearrange("p (t h n) -> p t h n", t=16, h=2)
            etm = [ar.alloc(256, F32) for _ in range(2)]
            Cst = ar.alloc(2 * 130, F32).rearrange("p (d n) -> p d n", d=2)
            Cbf = ar.alloc(2 * 130, BF16).rearrange("p (d n) -> p d n", d=2)
            Sm = [ar.alloc(128, BF16) for _ in range(8)]
            vw = [ar.alloc(130, BF16) for _ in range(8)]
            dn = [ar.alloc(2, F32) for _ in range(8)]
            hsq = [ar.alloc(256, F32)] * 2
            hst = [ar.alloc(2, F32) for _ in range(2)]
            hy = [ar.alloc(256, F32) for _ in range(2)]
            hyb = [ar.alloc(256, BF16) for _ in range(2)]
            g.op("dve", lambda e: e.memset(V1[:, :, :, 128:130], 1.0), writes=["V1ones"])

            for mp in range(2):
                sw = wload([win_v[:, :, mp * 128:(mp + 1) * 128], win_v[:, :, 256 + mp * 128:256 + (mp + 1) * 128]],
                           [lambda sl, q=q: sl[:, 0:NCH * 256].rearrange("p (c n) -> p c n", c=NCH)[:, :, q * 128:(q + 1) * 128]
                            for q in range(2)])
                wq = ring[:, sw, 0:NCH * 256].rearrange("p (c n) -> p c n", c=NCH)
                cnt = 0
                for q in range(2):
                    for blk in range(4):
                        pp = 5 + (cnt % 2)
                        cnt += 1
                        tok = slice(blk * 512, (blk + 1) * 512)
                        for c in range(NCH):
                            g.op("pe", lambda e, c=c, q=q, pp=pp, tok=tok, wq=wq: e.matmul(
                                ps[pp][:, :], lhsT=wq[:, c, q * 128:(q + 1) * 128], rhs=h2T[:, c, tok],
                                start=(c == 0), stop=(c == NCH - 1)),
                                reads=[("ring", sw), ("h2T", c, blk)], writes=[("ps", pp)])
                        if q == 0:
                            g.op("act", lambda e, pp=pp, tok=tok: e.copy(out=mqT[:, tok], in_=ps[pp][:, :]),
                                 reads=[("ps", pp)], writes=[("mqT", blk)])
                        else:
                            g.op("act", lambda e, pp=pp, tok=tok: e.mul(out=mkT[:, tok], in_=ps[pp][:, :], mul=0.125),
                                 reads=[("ps", pp)], writes=[("mkT", blk)])
                s1 = wload([win_v[:, :, 256 + mp * 128:256 + (mp + 1) * 128], win_v[:, :, 512 + mp * 256:512 + (mp + 1) * 256],
                            win_v[:, :, 1536:1552]],
                           [lambda sl: sl[:, 0:NCH * 400].rearrange("p (c n) -> p c n", c=NCH)[:, :, 0:128],
                            lambda sl: sl[:, 0:NCH * 400].rearrange("p (c n) -> p c n", c=NCH)[:, :, 128:384],
                            lambda sl: sl[:, 0:NCH * 400].rearrange("p (c n) -> p c n", c=NCH)[:, :, 384:400]])
                w1 = ring[:, s1, 0:NCH * 400].rearrange("p (c n) -> p c n", c=NCH)
                for tt in range(16):
                    ts_ = slice(tt * 128, (tt + 1) * 128)
                    blk = tt // 4
                    pp = 5 + (tt % 2)
                    for c in range(NCH):
                        g.op("pe", lambda e, c=c, ts_=ts_, w1=w1, pp=pp: e.matmul(
                            ps[pp][:, 0:400], lhsT=h2T[:, c, ts_], rhs=w1[:, c, :], start=(c == 0), stop=(c == NCH - 1)),
                            reads=[("ring", s1), ("h2T", c, blk)], writes=[("ps", pp)])
                    g.op("act", lambda e, tt=tt, pp=pp: e.mul(out=ktok[:, tt, :], in_=ps[pp][:, 0:128], mul=0.125),
                         reads=[("ps", pp)], writes=[("ktok", tt)])
                    g.op("act", lambda e, tt=tt, pp=pp: e.copy(out=V1[:, tt, :, 0:128],
                                                               in_=ps[pp][:, 128:384].rearrange("p (h n) -> p h n", h=2)),
                         reads=[("ps", pp)], writes=[("V1", tt)])
                    if mp == 0:
                        g.op("dve", lambda e, tt=tt, pp=pp: e.tensor_tensor(out=G[:, tt, :], in0=ps[pp][:, 384:400],
                                                                           in1=gbias[:, :], op=ALU.add),
                             reads=[("ps", pp), "gbias"], writes=[("G", tt)])
                if mp == 0:
                    allG = [("G", tt) for tt in range(16)]
                    g.op("act", lambda e: e.activation(out=LF4, in_=G4[:, :, 1:4:2, :], func=AF.Exp, scale=-1.0),
                         reads=allG, writes=["LF"])
                    g.op("act", lambda e: e.activation(out=LF, in_=LF, func=AF.Ln, bias=1.0, scale=1.0),
                         reads=["LF"], writes=["LF"])
                    g.op("dve", lambda e: e.tensor_scalar(out=LF, in0=LF, scalar1=-1.0, scalar2=None, op0=ALU.mult),
                         reads=["LF"], writes=["LF"])
                    for tt in range(16):
                        o = tt * 16
                        g.op("pe", lambda e, tt=tt, o=o: e.matmul(ps[4][:, o:o + 4], lhsT=trif[:, :], rhs=LF4[:, tt, 0, :],
                                                                  start=True, stop=True, skip_group_check=True),
                             reads=["LF", "trif"], writes=[("ps", 4)])
                        g.op("pe", lambda e, tt=tt, o=o: e.matmul(ps[4][:, o + 4:o + 8], lhsT=trib[:, :], rhs=LF4[:, tt, 1, :],
                                                                  start=True, stop=True, skip_group_check=True),
                             reads=["LF", "trib"], writes=[("ps", 4)])
                        g.op("pe", lambda e, tt=tt, o=o: e.matmul(ps[4][:, o + 8:o + 16], lhsT=onesf[:, :], rhs=LF[:, tt, :],
                                                                  start=True, stop=True, skip_group_check=True),
                             reads=["LF", "onesf"], writes=[("ps", 4)])
                    g.op("act", lambda e: e.copy(out=sB, in_=ps[4][:, 0:256].rearrange("p (t n) -> p t n", t=16)),
                         reads=[("ps", 4)], writes=["sB"])
                    LI = G4[:, :, 0:4:2, :]
                    T1v = T1.rearrange("p t (a h) -> p t a h", a=2)
                    bc4 = sB[:, :, 0:8].rearrange("p t (a h) -> p t a h", a=2)
                    g.op("dve", lambda e: e.tensor_tensor(out=T1v, in0=LI, in1=bc4, op=ALU.subtract),
                         reads=allG + ["sB"], writes=["T1"])
                    g.op("dve", lambda e: e.scalar_tensor_tensor(out=T1, in0=sB[:, :, 8:16], scalar=0.5, in1=T1,
                                                                 op0=ALU.mult, op1=ALU.add), reads=["sB", "T1"], writes=["T1"])
                    g.op("act", lambda e: e.activation(out=Call, in_=T1, func=AF.Exp), reads=["T1"], writes=["Call"])
                    g.op("dve", lambda e: e.scalar_tensor_tensor(out=T2, in0=sB[:, :, 8:16], scalar=-0.5, in1=sB[:, :, 0:8],
                                                                 op0=ALU.mult, op1=ALU.add), reads=["sB"], writes=["T1"])
                    g.op("act", lambda e: e.activation(out=Aall, in_=T2, func=AF.Exp), reads=["T1"], writes=["Aall"])
                    g.op("act", lambda e: e.activation(out=EBa, in_=sB[:, :, 8:16], func=AF.Exp), reads=["sB"], writes=["EBa"])
                    g.op("act", lambda e: e.activation(out=EBH, in_=sB[:, :, 8:16], func=AF.Exp, scale=0.5),
                         reads=["sB"], writes=["EBH"])
                g.op("dve", lambda e: e.memset(Cst, 0.0), writes=[("Cst", d_, hl_) for d_ in range(2) for hl_ in range(2)])
                g.op("dve", lambda e: e.memset(Cbf, 0.0), writes=[("Cbf", d_, hl_) for d_ in range(2) for hl_ in range(2)])
                qcnt = [0]

                def chain_a(step, hl, dr, mp=mp):
                    head = mp * 2 + hl
                    rows = slice(hl * 64, (hl + 1) * 64)
                    tt = step if dr == 0 else 15 - step
                    ts_ = slice(tt * 128, (tt + 1) * 128)
                    j = dr * 4 + head
                    z = (hl * 2 + dr) * 2 + (step % 2)
                    pq = qcnt[0] % 2
                    qcnt[0] += 1
                    g.op("pe", lambda e: e.matmul(ps[pq][:, 0:128], lhsT=mkT[rows, ts_], rhs=mqT[rows, ts_], start=True, stop=True),
                         reads=[("mkT", tt // 4), ("mqT", tt // 4)], writes=[("ps", pq)])
                    mk_ = mkf if dr == 0 else mkb
                    g.op("dve", lambda e: e.tensor_tensor(out=Sm[z], in0=ps[pq][:, 0:128], in1=mk_[:, :], op=ALU.mult),
                         reads=[("ps", pq), "mkf", "mkb"], writes=[("Sm", z)])
                    g.op("act", lambda e: e.activation(out=vw[z][:, 0:129], in_=V1[:, tt, hl, 0:129], func=AF.Identity,
                                                       scale=Call[:, tt, j:j + 1]),
                         reads=[("V1", tt), "V1ones", "Call"], writes=[("vw", z)])

                def chain_b(step, hl, dr, mp=mp):
                    head = mp * 2 + hl
                    rows = slice(hl * 64, (hl + 1) * 64)
                    tt = step if dr == 0 else 15 - step
                    ts_ = slice(tt * 128, (tt + 1) * 128)
                    j = dr * 4 + head
                    c_ = hl * 2 + dr
                    z = c_ * 2 + (step % 2)
                    po_ = 2 + (qcnt[0] % 2)
                    qcnt[0] += 1
                    pc = 4 + c_
                    g.op("pe", lambda e: e.matmul(ps[po_][:, 0:129], lhsT=Sm[z], rhs=vw[z][:, 0:129], start=True, stop=False),
                         reads=[("Sm", z), ("vw", z)], writes=[("ps", po_)])
                    g.op("pe", lambda e: e.matmul(ps[po_][:, 0:129], lhsT=mqT[rows, ts_], rhs=Cbf[rows, dr, 0:129], start=False, stop=True),
                         reads=[("mqT", tt // 4), ("Cbf", dr, hl)], writes=[("ps", po_)])
                    g.op("pe", lambda e: e.matmul(ps[pc][rows, 0:129], lhsT=ktok[:, tt, rows], rhs=vw[z][:, 0:129], start=True, stop=True),
                         reads=[("ktok", tt), ("vw", z)], writes=[("ps", pc)])
                    g.op("dve", lambda e: e.tensor_scalar(out=Cst[rows, dr, 0:129], in0=Cst[rows, dr, 0:129],
                                                          scalar1=EBa[rows, tt, j:j + 1], scalar2=None, op0=ALU.mult),
                         reads=[("Cst", dr, hl), "EBa"], writes=[("Cst", dr, hl)])
                    g.op("dve", lambda e: e.scalar_tensor_tensor(out=Cst[rows, dr, 0:129], in0=ps[pc][rows, 0:129],
                                                                 scalar=EBH[rows, tt, j:j + 1], in1=Cst[rows, dr, 0:129],
                                                                 op0=ALU.mult, op1=ALU.add),
                         reads=[("ps", pc), ("Cst", dr, hl), "EBH"], writes=[("Cst", dr, hl)])
                    if step < 15:
                        tn = tt + 1 if dr == 0 else tt - 1
                        g.op("act", lambda e: e.activation(out=Cbf[rows, dr, 0:129], in_=Cst[rows, dr, 0:129], func=AF.Identity,
                                                           scale=EBH[rows, tn, j:j + 1]),
                             reads=[("Cst", dr, hl), "EBH"], writes=[("Cbf", dr, hl)])
                    acol = Aall[:, tt, j:j + 1]
                    g.op("act", lambda e: e.activation(out=dn[z][:, 0:1], in_=ps[po_][:, 128:129], func=AF.Abs, scale=acol),
                         reads=[("ps", po_), "Aall"], writes=[("dn", z)])
                    g.op("dve", lambda e: e.tensor_scalar(out=dn[z][:, 0:1], in0=dn[z][:, 0:1], scalar1=1.0, scalar2=None, op0=ALU.max),
                         reads=[("dn", z)], writes=[("dn", z)])
                    g.op("dve", lambda e: e.reciprocal(out=dn[z][:, 0:1], in_=dn[z][:, 0:1]), reads=[("dn", z)], writes=[("dn", z)])
                    g.op("dve", lambda e: e.tensor_tensor(out=dn[z][:, 1:2], in0=dn[z][:, 0:1], in1=acol, op=ALU.mult),
                         reads=[("dn", z), "Aall"], writes=[("dn", z)])
                    is_first = (dr == 0 and tt <= 15 - tt) or (dr == 1 and (15 - tt) < tt)
                    if is_first:
                        g.op("act", lambda e: e.activation(out=hacc[:, tt, hl, :], in_=ps[po_][:, 0:128], func=AF.Identity,
                                                           scale=dn[z][:, 1:2]),
                             reads=[("ps", po_), ("dn", z)], writes=[("hacc", tt, hl)])
                    else:
                        g.op("dve", lambda e: e.scalar_tensor_tensor(out=hacc[:, tt, hl, :], in0=ps[po_][:, 0:128], scalar=dn[z][:, 1:2],
                                                                     in1=hacc[:, tt, hl, :], op0=ALU.mult, op1=ALU.add),
                             reads=[("ps", po_), ("dn", z), ("hacc", tt, hl)], writes=[("hacc", tt, hl)])

                chains = [(hl_, dr_) for hl_ in range(2) for dr_ in range(2)]
                for hl_, dr_ in chains:
                    chain_a(0, hl_, dr_)
                for step in range(16):
                    if step < 15:
                        for hl_, dr_ in chains:
                            chain_a(step + 1, hl_, dr_)
                    for hl_, dr_ in chains:
                        chain_b(step, hl_, dr_)
                s3 = wload([win_v[:, :, 1024 + mp * 256:1024 + (mp + 1) * 256]],
                           [lambda sl: sl[:, 0:NCH * 256].rearrange("p (c n) -> p c n", c=NCH)])
                w3 = ring[:, s3, 0:NCH * 256].rearrange("p (c n) -> p c n", c=NCH)
                for tt in range(16):
                    u = tt % 2
                    ts_ = slice(tt * 128, (tt + 1) * 128)
                    pp = 5 + (tt % 2)
                    for c in range(NCH):
                        g.op("pe", lambda e, c=c, ts_=ts_, w3=w3, pp=pp: e.matmul(
                            ps[pp][:, 0:256], lhsT=h2T[:, c, ts_], rhs=w3[:, c, :], start=(c == 0), stop=(c == NCH - 1)),
                            reads=[("ring", s3), ("h2T", c, tt // 4)], writes=[("ps", pp)])
                    g.op("act", lambda e, u=u, pp=pp: e.activation(out=etm[u], in_=ps[pp][:, 0:256], func=AF.Exp, scale=-1.0),
                         reads=[("ps", pp)], writes=[("etm", u)])
                    g.op("dve", lambda e, u=u: e.tensor_scalar(out=etm[u], in0=etm[u], scalar1=1.0, scalar2=None, op0=ALU.add),
                         reads=[("etm", u)], writes=[("etm", u)])
                    g.op("dve", lambda e, u=u: e.reciprocal(out=etm[u], in_=etm[u]),
                         reads=[("etm", u)], writes=[("etm", u)])
                    hk = [("hacc", tt, 0), ("hacc", tt, 1)]
                    hv = hacc[:, tt, :, :]
                    h3 = hsq[u].rearrange("p (h n) -> p h n", h=2)
                    y3 = hy[u].rearrange("p (h n) -> p h n", h=2)
                    g.op("dve", lambda e, hv=hv, h3=h3: e.tensor_tensor(out=h3, in0=hv, in1=hv, op=ALU.mult),
                         reads=hk, writes=["hsq"])
                    g.op("dve", lambda e, u=u, h3=h3: e.tensor_reduce(out=hst[u], in_=h3, axis=AX.X, op=ALU.add),
                         reads=["hsq"], writes=[("hst", u)])
                    g.op("act", lambda e, u=u: e.activation(out=hst[u], in_=hst[u], func=AF.Ln, bias=EPS, scale=1.0 / 128),
                         reads=[("hst", u)], writes=[("hst", u)])
                    g.op("act", lambda e, u=u: e.activation(out=hst[u], in_=hst[u], func=AF.Exp, scale=-0.5),
                         reads=[("hst", u)], writes=[("hst", u)])
                    for hl in range(2):
                        head = mp * 2 + hl
                        g.op("dve", lambda e, u=u, hl=hl, head=head, hv=hv, y3=y3: e.scalar_tensor_tensor(
                            out=y3[:, hl, :], in0=hv[:, hl, :], scalar=hst[u][:, hl:hl + 1], in1=gmh[:, head * 128:(head + 1) * 128],
                            op0=ALU.mult, op1=ALU.mult), reads=hk + [("hst", u), "gmh"], writes=[("hy", u, hl)])
                    g.op("dve", lambda e, u=u, tt=tt: e.tensor_tensor(out=hyb[u], in0=hy[u], in1=etm[u], op=ALU.mult),
                         reads=[("hy", u, 0), ("hy", u, 1), ("etm", u)], writes=[("hyb", u)])
                    def fin_tr(tt, mp=mp):
                        u = tt % 2
                        ts_ = slice(tt * 128, (tt + 1) * 128)
                        for hl in range(2):
                            g.op("pe", lambda e, hl=hl: e.transpose(psT[:, 256 + hl * 128:256 + (hl + 1) * 128],
                                                                    hyb[u][:, hl * 128:(hl + 1) * 128], identb[:, :]),
                                 reads=[("hyb", u), "identb"], writes=[PST])
                        g.op("act", lambda e: e.copy(
                            out=mixh[:, mp * 2:mp * 2 + 2, ts_], in_=psT[:, 256:512].rearrange("p (h n) -> p h n", h=2)),
                            reads=[PST], writes=[("mixh", mp * 2, tt // 4), ("mixh", mp * 2 + 1, tt // 4)])
                    if tt >= 1:
                        fin_tr(tt - 1)
                    if tt == 15:
                        fin_tr(15)
            dbg("mixm", mixh, [128, 4, S], BF16, [("mixh", k_, q_) for k_ in range(4) for q_ in range(4)], b)
            dbg("G", G, [128, 16, 16], F32, [("G", t_) for t_ in range(16)], b)
            dbg("LF", LF, [128, 16, 8], F32, ["LF"], b)
            dbg("sB", sB, [128, 16, 16], F32, ["sB"], b)
            dbg("Call", Call, [128, 16, 8], F32, ["Call"], b)
            dbg("Aall", Aall, [128, 16, 8], F32, ["Aall"], b)
            dbg("hacc", hacc, [128, 16, 2, 128], F32, [("hacc", t_, h_) for t_ in range(16) for h_ in range(2)], b)
            wout_half(0)
            ar.release(m0)
            g.barrier()

        def final(b, raw=False):
            m0 = ar.mark()
            outs = []
            if raw:
                for c in range(NCH):
                    outs.append(g.dma("sp", out_d[b, c * 128:(c + 1) * 128, :], xT[:, c, :],
                                      reads=[("xT", c, blk) for blk in range(4)]))
                return outs
            rstd = ar.alloc(S, F32)
            sq = ar.alloc(NCH * 512, BF16).rearrange("p (c n) -> p c n", c=NCH)
            ob = [ar.alloc(512, F32) for _ in range(4)]
            rms_rstd(b, rstd, sq)
            n = 0
            for blk in range(4):
                tok = slice(blk * 512, (blk + 1) * 512)
                for c in range(NCH):
                    o_ = ob[n % 4]
                    g.op("dve", lambda e, c=c, tok=tok, o_=o_: e.scalar_tensor_tensor(
                        out=o_, in0=xT[:, c, tok], scalar=g4[:, 3, c:c + 1], in1=rstd[:, tok], op0=ALU.mult, op1=ALU.mult),
                        reads=[("xT", c, blk), ("rstd", blk), "g4"], writes=[("ob", n % 4)])
                    outs.append(g.dma("sp", out_d[b, c * 128:(c + 1) * 128, tok], o_, reads=[("ob", n % 4)]))
                    n += 1
            ar.release(m0)
            g.barrier()
            return outs

        outs = []
        adaln(list(range(0, 6)))
        derive(0)
        for b in range(2):
            load_x(b)
            ffn(b, 0, 0)
            if b == 0:
                adaln(list(range(6, 18)))
                derive(1)
                derive(2)
            if stage >= 2:
                mixer(b)
            if stage >= 3:
                ffn(b, 2, 1)
            outs += final(b, raw=(stage < 3))
        g.emit(final_wait_ops=outs + dbg_outs)
    return nc


def _consts():
    p = np.arange(128)[:, None]
    j = np.arange(MASK_W)[None, :]
    dlt = p - j + MASK_C
    a = np.abs(dlt)
    m = (a <= 64).astype(np.float32) + ((dlt % 4 == 0) & (a <= 256)) + ((dlt % 16 == 0) & (a <= 1024))
    maskT = m.astype(ml_dtypes.bfloat16)
    identb = np.eye(128, dtype=np.float32).astype(ml_dtypes.bfloat16)
    u = np.arange(128)[:, None]
    t = np.arange(128)[None, :]
    trif = (u <= t).astype(np.float32)
    trib = (u >= t).astype(np.float32)
    maskf = (u <= t).astype(np.float32).astype(ml_dtypes.bfloat16)
    maskb = (u >= t).astype(np.float32).astype(ml_dtypes.bfloat16)
    half = 8
    inv_freq = (500000.0 ** (-2.0 * np.arange(half, dtype=np.float32) / 16.0)).astype(np.float32)
    pos = np.arange(S, dtype=np.float32)
    ang = (pos[:, None] * inv_freq[None, :]).astype(np.float32)
    cos = np.cos(ang).astype(np.float32).reshape(16, 128, 8).transpose(1, 0, 2)
    sin = np.sin(ang).astype(np.float32).reshape(16, 128, 8).transpose(1, 0, 2)
    cos4 = np.ascontiguousarray(np.broadcast_to(cos[:, :, None, :], (128, 16, 4, 8))).astype(np.float32)
    sin4 = np.ascontiguousarray(np.broadcast_to(sin[:, :, None, :], (128, 16, 4, 8))).astype(np.float32)
    return dict(maskT=maskT, identb=identb, trif=trif, trib=trib, maskf=maskf, maskb=maskb, cos4=cos4, sin4=sin4)


def _cols(v):
    return np.ascontiguousarray(np.asarray(v, np.float32).reshape(-1, 128).T)


_NC_CACHE = {}


def kernel(x, c, w_ada, b_ada, g_ffn1, w_gu1, w_down1, g_mix, w_in, gate_bias, g_q, g_k, g_mh,
           w_out, g_ffn2, w_gu2, w_down2, g_final):
    stage = int(os.environ.get("MK_STAGE", "3"))
    x = np.asarray(x, np.float32)
    c = np.asarray(c, np.float32)
    if stage not in _NC_CACHE:
        _NC_CACHE[stage] = build_program(stage)
    nc = _NC_CACHE[stage]
    consts = _consts()
    shared = dict(
        w_ada=np.ascontiguousarray(np.asarray(w_ada, np.float32)[0]),
        b_adaT=_cols(np.asarray(b_ada)[0]),
        g4=np.ascontiguousarray(np.stack([_cols(np.asarray(v)[0]) for v in (g_ffn1, g_mix, g_ffn2, g_final)], axis=1)),
        w_gu1=np.ascontiguousarray(np.asarray(w_gu1, np.float32)[0]),
        w_gu2=np.ascontiguousarray(np.asarray(w_gu2, np.float32)[0]),
        w_down1=np.ascontiguousarray(np.asarray(w_down1, np.float32)[0]),
        w_down2=np.ascontiguousarray(np.asarray(w_down2, np.float32)[0]),
        w_in=np.ascontiguousarray(np.asarray(w_in, np.float32)[0]),
        w_out=np.ascontiguousarray(np.asarray(w_out, np.float32)[0]),
        gbias_rep=np.ascontiguousarray(np.broadcast_to(np.asarray(gate_bias, np.float32)[0].reshape(1, 16), (128, 16))),
        gqk_rep=np.ascontiguousarray(np.broadcast_to(
            np.stack([np.asarray(g_q, np.float32)[0]] * 2 + [np.asarray(g_k, np.float32)[0]] * 2)[None], (128, 4, 64))),
        gmh_rep=np.ascontiguousarray(np.broadcast_to(np.asarray(g_mh, np.float32)[0][None, :], (128, 512))),
        **consts,
    )
    in_maps = []
    for i in range(8):
        m = dict(shared)
        m["xT"] = np.ascontiguousarray(x[2 * i:2 * i + 2].transpose(0, 2, 1))
        m["cT"] = np.ascontiguousarray(c[2 * i:2 * i + 2].reshape(2, NCH, 128).transpose(2, 1, 0))
        in_maps.append(m)
    res = run_bass_kernel_spmd(nc, in_maps, core_ids=list(range(8)))
    out = np.empty((16, S, D), np.float32)
    for i in range(8):
        out[2 * i:2 * i + 2] = res.results[i]["outT"].transpose(0, 2, 1)
    return out
```

```python
import os
import contextlib
import numpy as np
import ml_dtypes
import concourse.bass as bass
import concourse.mybir as mybir
from concourse.bass_utils import run_bass_kernel_spmd

F32 = mybir.dt.float32
BF16 = mybir.dt.bfloat16
AF = mybir.ActivationFunctionType
ALU = mybir.AluOpType
AX = mybir.AxisListType

ENG_NAMES = ("pe", "act", "dve", "pool", "sp")
D = 1024
S = 2048
DFF = 2816
NCH = 8
NF = 22
EPS = 1e-6
MASK_C = 1408
MASK_W = 2944
NSLOT = 3
SLOT_ELEMS = 4096


class Op:
    __slots__ = ("eng", "fn", "deps", "is_dma", "sem", "val", "has_dep", "idx", "name", "prewait")

    def __init__(self, eng, fn, name=""):
        self.eng = eng
        self.fn = fn
        self.deps = []
        self.is_dma = False
        self.sem = None
        self.val = 0
        self.has_dep = False
        self.idx = 0
        self.name = name
        self.prewait = None


class Graph:
    def __init__(self, nc, n_dma_sems=16):
        self.nc = nc
        self.ops = {e: [] for e in ENG_NAMES}
        self.last_writer = {}
        self.readers = {}
        self.n_dma_sems = n_dma_sems
        self.dma_count = {e: 0 for e in ENG_NAMES}
        self.dma_ops = {e: [] for e in ENG_NAMES}
        self.barrier_deps = {}
        self.sp_since_barrier = []

    def _link(self, op, deps):
        latest = {}
        keep = []
        for d in deps:
            if d is op:
                continue
            if d.is_dma:
                keep.append(d)
                continue
            if d.eng == "pe" and op.eng == "pe":
                continue
            cur = latest.get(d.eng)
            if cur is None or d.idx > cur.idx:
                latest[d.eng] = d
        keep.extend(latest.values())
        seen = set(id(d) for d in op.deps)
        for d in keep:
            if id(d) in seen:
                continue
            seen.add(id(d))
            op.deps.append(d)
            d.has_dep = True

    def _add_deps(self, op, reads, writes):
        deps = []
        for r in reads:
            w = self.last_writer.get(r)
            if w is not None:
                deps.append(w)
            if (isinstance(r, tuple) and r[0] == "ps") or (isinstance(r, str) and r.startswith("psT")):
                deps.extend(x for x in self.readers.get(r, ()) if x.eng != op.eng)
        for w_ in writes:
            w = self.last_writer.get(w_)
            if w is not None:
                deps.append(w)
            deps.extend(self.readers.get(w_, ()))
        b = self.barrier_deps.pop(op.eng, None)
        if b:
            deps.extend(b)
        self._link(op, deps)
        for r in reads:
            self.readers.setdefault(r, []).append(op)
        for w_ in writes:
            self.last_writer[w_] = op
            self.readers[w_] = []

    def op(self, eng, fn, reads=(), writes=(), name=""):
        o = Op(eng, fn, name)
        o.idx = len(self.ops[eng])
        self._add_deps(o, reads, writes)
        self.ops[eng].append(o)
        return o

    def dma(self, eng, out, in_, reads=(), writes=(), name=""):
        def fn(e, out=out, in_=in_):
            return e.dma_start(out=out, in_=in_)
        o = Op(eng, fn, name)
        o.is_dma = True
        i = self.dma_count[eng]
        self.dma_count[eng] += 1
        o.idx = i
        o.val = 16 * (i // self.n_dma_sems + 1)
        if i >= self.n_dma_sems:
            o.prewait = self.dma_ops[eng][i - self.n_dma_sems]
        self.dma_ops[eng].append(o)
        self._add_deps(o, reads, writes)
        self.ops[eng].append(o)
        if eng == "sp":
            self.sp_since_barrier.append(o)
        return o

    def barrier(self):
        b = []
        for e in ("pe", "act", "dve"):
            for o in reversed(self.ops[e]):
                b.append(o)
                break
        b.extend(self.sp_since_barrier)
        self.sp_since_barrier = []
        for e in ("pe", "act", "dve", "sp"):
            self.barrier_deps[e] = list(b) + list(self.barrier_deps.get(e, ()))

    def emit(self, final_wait_ops=()):
        nc = self.nc
        with contextlib.ExitStack() as st:
            esem = {e: st.enter_context(nc.semaphore("s_" + e)) for e in ENG_NAMES}
            dsem = {}
            for e in ENG_NAMES:
                if self.dma_count[e]:
                    dsem[e] = [st.enter_context(nc.semaphore("d_%s_%d" % (e, i)))
                               for i in range(min(self.n_dma_sems, self.dma_count[e]))]
            for e in ENG_NAMES:
                m = 0
                for o in self.ops[e]:
                    if o.is_dma:
                        o.sem = dsem[e][o.idx % self.n_dma_sems]
                    elif o.has_dep:
                        m += 1
                        o.sem = esem[e]
                        o.val = m
            block = st.enter_context(nc.Block())
            handles = {"pe": block.tensor, "act": block.scalar, "dve": block.vector,
                       "pool": block.gpsimd, "sp": block.sync}

            def make(e):
                ops = self.ops[e]

                def body(eng):
                    waited = {}

                    def wait(d):
                        key = id(d.sem)
                        if waited.get(key, 0) >= d.val:
                            return
                        waited[key] = d.val
                        eng.wait_ge(d.sem, d.val)
                    for o in ops:
                        if o.prewait is not None:
                            wait(o.prewait)
                        for d in o.deps:
                            wait(d)
                        inst = o.fn(eng)
                        if o.is_dma:
                            inst.then_inc(o.sem, 16)
                        elif o.has_dep:
                            inst.then_inc(o.sem, 1)
                    if e == "sp":
                        for d in final_wait_ops:
                            wait(d)
                return body
            for e in ENG_NAMES:
                if self.ops[e] or (e == "sp" and final_wait_ops):
                    handles[e](make(e))


class Arena:
    def __init__(self, ap2d, nelem_bf16):
        self.ap = ap2d
        self.cap = nelem_bf16
        self.off = 0

    def mark(self):
        return self.off

    def release(self, m):
        self.off = m

    def alloc(self, nelem, dt):
        n16 = nelem * (2 if dt == F32 else 1)
        self.off = (self.off + 1) // 2 * 2
        assert self.off + n16 <= self.cap, ("arena overflow", self.off, n16, self.cap)
        a = self.ap[:, self.off:self.off + n16]
        self.off += n16
        if dt == F32:
            a = a.bitcast(F32)
        return a


def build_program(stage=3):
    nc = bass.Bass("TRN2", target_bir_lowering=False)

    def din(name, shape, dt=F32):
        return nc.dram_tensor(name, list(shape), dt, kind="ExternalInput").ap()
    xT_d = din("xT", [2, D, S])
    cT_d = din("cT", [128, NCH, 2])
    wada_d = din("w_ada", [D, 9 * D])
    bada_d = din("b_adaT", [128, 72])
    g4_d = din("g4", [128, 4, NCH])
    wgu_d = [din("w_gu1", [D, 2 * DFF]), din("w_gu2", [D, 2 * DFF])]
    wdn_d = [din("w_down1", [DFF, D]), din("w_down2", [DFF, D])]
    win_d = din("w_in", [D, 3088])
    wout_d = din("w_out", [D, D])
    gbias_d = din("gbias_rep", [128, 16])
    gqk_d = din("gqk_rep", [128, 4, 64])
    gmh_d = din("gmh_rep", [128, 512])
    mask_d = din("maskT", [128, MASK_W], BF16)
    identb_d = din("identb", [128, 128], BF16)
    trif_d = din("trif", [128, 128])
    trib_d = din("trib", [128, 128])
    mkf_d = din("maskf", [128, 128], BF16)
    mkb_d = din("maskb", [128, 128], BF16)
    cos_d = din("cos4", [128, 16, 4, 8])
    sin_d = din("sin4", [128, 16, 4, 8])
    out_d = nc.dram_tensor("outT", [2, D, S], F32, kind="ExternalOutput").ap()

    st = contextlib.ExitStack()
    with st:
        def sbt(name, shape, dt):
            return st.enter_context(nc.sbuf_tensor(name, shape, dt))
        xT = sbt("xT_sb", [128, NCH, S], F32)
        ring = sbt("ring", [128, NSLOT, SLOT_ELEMS], BF16)
        maskT = sbt("maskT_sb", [128, MASK_W], BF16)
        identb = sbt("identb_sb", [128, 128], BF16)
        onesb = sbt("onesb", [128, 128], BF16)
        onesf = sbt("onesf", [128, 128], F32)
        trif = sbt("trif_sb", [128, 128], F32)
        trib = sbt("trib_sb", [128, 128], F32)
        mkf = sbt("mkf_sb", [128, 128], BF16)
        mkb = sbt("mkb_sb", [128, 128], BF16)
        cos4 = sbt("cos4_sb", [128, 16, 4, 8], F32)
        sin4 = sbt("sin4_sb", [128, 16, 4, 8], F32)
        gqk = sbt("gqk_sb", [128, 4, 64], F32)
        gmh = sbt("gmh_sb", [128, 512], F32)
        gbias = sbt("gbias_sb", [128, 16], F32)
        g4 = sbt("g4_sb", [128, 4, NCH], F32)
        badaT = sbt("bada_sb", [128, 72], F32)
        cT = sbt("cT_sb", [128, NCH, 2], F32)
        csT = sbt("csT_sb", [128, NCH, 2], BF16)
        modT = sbt("modT", [128, 72, 2], F32)
        geff = sbt("geff", [128, 2, 3, NCH], F32)
        gate = sbt("gate", [128, 2, 3, NCH], F32)
        ARENA_N = 52736
        arena_t = sbt("arena", [128, ARENA_N], BF16)
        ar = Arena(arena_t[:, :], ARENA_N)
        ps = [st.enter_context(nc.psum_tensor("ps%d" % i, [128, 512], F32)) for i in range(8)]
        psT = ps[7][:, :].bitcast(BF16)
        PST = ("ps", 7)

        g = Graph(nc)
        ring_ctr = [0]
        DBG = os.environ.get("MK_DEBUG") == "1"
        dbg_outs = []

        def dbg(name, ap, shape, dt, reads, b=0):
            if not DBG or b != 0:
                return
            t = nc.dram_tensor("dbg_" + name, list(shape), dt, kind="ExternalOutput").ap()
            dbg_outs.append(g.dma("sp", t, ap, reads=reads))

        def wload(src_aps, views, name=""):
            s = ring_ctr[0] % NSLOT
            ring_ctr[0] += 1
            for src, vw in zip(src_aps, views):
                g.dma("pool", vw(ring[:, s, :]), src, writes=[("ring", s)], name=name)
            return s

        for dst, src, key in ((maskT[:, :], mask_d, "maskT"), (identb[:, :], identb_d, "identb"),
                              (trif[:, :], trif_d, "trif"), (trib[:, :], trib_d, "trib"),
                              (mkf[:, :], mkf_d, "mkf"), (mkb[:, :], mkb_d, "mkb"),
                              (cos4[:], cos_d, "cos4"), (sin4[:], sin_d, "sin4"),
                              (gqk[:], gqk_d, "gqk"), (gmh[:, :], gmh_d, "gmh"), (gbias[:, :], gbias_d, "gbias"),
                              (g4[:], g4_d, "g4"), (badaT[:, :], bada_d, "badaT"), (cT[:], cT_d, "cT")):
            g.dma("sp", dst, src, writes=[key])
        g.op("dve", lambda e: e.memset(onesb[:, :], 1.0), writes=["onesb"])
        g.op("dve", lambda e: e.memset(onesf[:, :], 1.0), writes=["onesf"])
        g.op("dve", lambda e: e.tensor_scalar(out=gqk[:, 0:2, :], in0=gqk[:, 0:2, :], scalar1=0.125, scalar2=None,
                                              op0=ALU.mult), reads=["gqk"], writes=["gqk"])

        def load_x(b):
            for c in range(NCH):
                g.dma("sp", xT[:, c, :], xT_d[b, c * 128:(c + 1) * 128, :],
                      writes=[("xT", c, blk) for blk in range(4)])

        g.op("act", lambda e: e.activation(out=csT[:], in_=cT[:], func=AF.Silu), reads=["cT"], writes=["csT"])
        wada_v = wada_d.rearrange("(c p) n -> p c n", p=128)

        def adaln(groups):
            for grp in groups:
                s = wload([wada_v[:, :, grp * 512:(grp + 1) * 512]],
                          [lambda sl: sl.rearrange("p (c n) -> p c n", c=NCH)])
                wv = ring[:, s, :].rearrange("p (c n) -> p c n", c=NCH)
                for j in range(4):
                    n = grp * 4 + j
                    for k in range(NCH):
                        g.op("pe", lambda e, n=n, k=k, j=j, wv=wv: e.matmul(
                            ps[6][:, 2 * n:2 * n + 2], lhsT=wv[:, k, j * 128:(j + 1) * 128], rhs=csT[:, k, :],
                            start=(k == 0), stop=(k == NCH - 1), skip_group_check=True),
                            reads=[("ring", s), "csT"], writes=[("ps", 6)])
            n0, n1 = groups[0] * 4, groups[-1] * 4 + 4
            part = 0 if n0 == 0 else 1
            for b in range(2):
                g.op("dve", lambda e, b=b: e.tensor_tensor(
                    out=modT[:, n0:n1, b], in0=ps[6][:, 2 * n0 + b:2 * n1 + b:2], in1=badaT[:, n0:n1], op=ALU.add),
                    reads=[("ps", 6), "badaT"], writes=[("modT", part, b)])

        def derive(i):
            n0 = 3 * i * 8
            part = 0 if i == 0 else 1
            for b in range(2):
                g.op("dve", lambda e, b=b: e.scalar_tensor_tensor(
                    out=geff[:, b, i, :], in0=modT[:, n0 + 8:n0 + 16, b], scalar=1.0, in1=g4[:, i, :],
                    op0=ALU.add, op1=ALU.mult), reads=[("modT", part, b), "g4"], writes=[("geff", b, i)])
                g.op("dve", lambda e, b=b: e.tensor_scalar(
                    out=gate[:, b, i, :], in0=modT[:, n0 + 16:n0 + 24, b], scalar1=(1.0 if i == 1 else 0.5),
                    scalar2=None, op0=ALU.mult), reads=[("modT", part, b)], writes=[("gate", b, i)])

        def shift_col(b, i, c):
            return modT[:, 3 * i * 8 + c, b:b + 1]

        def rms_rstd(b, rstd, sq):
            for blk in range(4):
                tok = slice(blk * 512, (blk + 1) * 512)
                for c in range(NCH):
                    g.op("act", lambda e, c=c, tok=tok: e.activation(out=sq[:, c, :], in_=xT[:, c, tok], func=AF.Square),
                         reads=[("xT", c, blk)], writes=[("sq", c)])
                for c in range(NCH):
                    g.op("pe", lambda e, c=c: e.matmul(ps[6][:, :], lhsT=onesb[:, :], rhs=sq[:, c, :],
                                                      start=(c == 0), stop=(c == NCH - 1)),
                         reads=["onesb", ("sq", c)], writes=[("ps", 6)])
                g.op("act", lambda e, tok=tok: e.activation(out=rstd[:, tok], in_=ps[6][:, :], func=AF.Ln,
                                                            bias=EPS, scale=1.0 / D),
                     reads=[("ps", 6)], writes=[("rstd", blk)])
            for blk in range(4):
                tok = slice(blk * 512, (blk + 1) * 512)
                g.op("act", lambda e, tok=tok: e.activation(out=rstd[:, tok], in_=rstd[:, tok], func=AF.Exp, scale=-0.5),
                     reads=[("rstd", blk)], writes=[("rstd", blk)])

        def norm_mod(b, i, rstd, tmp, dst_fn, blk, keyfn):
            tok = slice(blk * 512, (blk + 1) * 512)
            for c in range(NCH):
                tb = tmp[c % 2]
                g.op("dve", lambda e, c=c, tb=tb: e.tensor_tensor(out=tb, in0=xT[:, c, tok], in1=rstd[:, tok], op=ALU.mult),
                     reads=[("xT", c, blk), ("rstd", blk)], writes=[("tmp", c % 2)])
                g.op("act", lambda e, c=c, tb=tb: e.activation(out=dst_fn(c), in_=tb, func=AF.Identity,
                                                               bias=shift_col(b, i, c), scale=geff[:, b, i, c:c + 1]),
                     reads=[("tmp", c % 2), ("geff", b, i), ("modT", 0 if i == 0 else 1, b)], writes=[keyfn(c)])

        def ffn(b, i, which):
            wgu_v = wgu_d[which].rearrange("(c p) n -> p c n", p=128)
            wdn_v = wdn_d[which].rearrange("(k p) n -> p k n", p=128)
            m0 = ar.mark()
            rstd = ar.alloc(S, F32)
            sq = ar.alloc(NCH * 512, BF16).rearrange("p (c n) -> p c n", c=NCH)
            tmp = [ar.alloc(512, F32) for _ in range(2)]
            hT = ar.alloc(NCH * 1024, BF16).rearrange("p (c n) -> p c n", c=NCH)
            actT = ar.alloc(NF * 1024, BF16).rearrange("p (c n) -> p c n", c=NF)
            sg = [ar.alloc(512, BF16) for _ in range(2)]
            rms_rstd(b, rstd, sq)
            cnt = 0
            for sb in range(2):
                for lb in range(2):
                    blk = sb * 2 + lb
                    norm_mod(b, i, rstd, tmp, lambda c, lb=lb: hT[:, c, lb * 512:(lb + 1) * 512], blk,
                             lambda c, lb=lb: ("hT", c, lb))
                for grp in range(11):
                    c0 = grp * 256
                    sgw = wload([wgu_v[:, :, c0:c0 + 256], wgu_v[:, :, DFF + c0:DFF + c0 + 256]],
                                [lambda sl, q=q: sl.rearrange("p (c n) -> p c n", c=NCH)[:, :, q * 256:(q + 1) * 256]
                                 for q in range(2)])
                    wgv = ring[:, sgw, :].rearrange("p (c n) -> p c n", c=NCH)
                    for j in range(2):
                        f = grp * 2 + j
                        for lb in range(2):
                            pg = cnt % 2
                            cnt += 1
                            hs = slice(lb * 512, (lb + 1) * 512)
                            for k in range(NCH):
                                g.op("pe", lambda e, k=k, j=j, wgv=wgv, pg=pg, hs=hs: e.matmul(
                                    ps[pg][:, :], lhsT=wgv[:, k, j * 128:(j + 1) * 128], rhs=hT[:, k, hs],
                                    start=(k == 0), stop=(k == NCH - 1)),
                                    reads=[("ring", sgw), ("hT", k, lb)], writes=[("ps", pg)])
                            for k in range(NCH):
                                g.op("pe", lambda e, k=k, j=j, wgv=wgv, pg=pg, hs=hs: e.matmul(
                                    ps[2 + pg][:, :], lhsT=wgv[:, k, 256 + j * 128:256 + (j + 1) * 128], rhs=hT[:, k, hs],
                                    start=(k == 0), stop=(k == NCH - 1)),
                                    reads=[("ring", sgw), ("hT", k, lb)], writes=[("ps", 2 + pg)])
                            g.op("act", lambda e, pg=pg: e.activation(out=sg[pg], in_=ps[pg][:, :], func=AF.Silu),
                                 reads=[("ps", pg)], writes=[("sg", pg)])
                            g.op("dve", lambda e, pg=pg, f=f, hs=hs: e.tensor_tensor(
                                out=actT[:, f, hs], in0=ps[2 + pg][:, :], in1=sg[pg], op=ALU.mult),
                                reads=[("ps", 2 + pg), ("sg", pg)], writes=[("actT", f, lb)])
                for dc in range(NCH):
                    sw = wload([wdn_v[:, :, dc * 128:(dc + 1) * 128]],
                               [lambda sl: sl[:, 0:NF * 128].rearrange("p (k n) -> p k n", k=NF)])
                    wd = ring[:, sw, 0:NF * 128].rearrange("p (k n) -> p k n", k=NF)
                    for lb in range(2):
                        blk = sb * 2 + lb
                        pd = 4 + (cnt % 2)
                        cnt += 1
                        hs = slice(lb * 512, (lb + 1) * 512)
                        for k in range(NF):
                            g.op("pe", lambda e, k=k, wd=wd, pd=pd, hs=hs: e.matmul(
                                ps[pd][:, :], lhsT=wd[:, k, :], rhs=actT[:, k, hs],
                                start=(k == 0), stop=(k == NF - 1)),
                                reads=[("ring", sw), ("actT", k, lb)], writes=[("ps", pd)])
                        tok = slice(blk * 512, (blk + 1) * 512)
                        g.op("dve", lambda e, pd=pd, dc=dc, tok=tok: e.scalar_tensor_tensor(
                            out=xT[:, dc, tok], in0=ps[pd][:, :], scalar=gate[:, b, i, dc:dc + 1], in1=xT[:, dc, tok],
                            op0=ALU.mult, op1=ALU.add),
                            reads=[("ps", pd), ("gate", b, i), ("xT", dc, blk)], writes=[("xT", dc, blk)])
            ar.release(m0)
            g.barrier()

        def mixer(b):
            i = 1
            win_v = win_d.rearrange("(c p) n -> p c n", p=128)
            wout_v = wout_d.rearrange("(k p) n -> p k n", p=128)
            m0 = ar.mark()
            h2T = ar.alloc(NCH * S, BF16).rearrange("p (c n) -> p c n", c=NCH)
            mixh = ar.alloc(4 * S, BF16).rearrange("p (c n) -> p c n", c=4)
            m1 = ar.mark()
            rstd = ar.alloc(S, F32)
            sq = ar.alloc(NCH * 512, BF16).rearrange("p (c n) -> p c n", c=NCH)
            tmp = [ar.alloc(512, F32) for _ in range(2)]
            rms_rstd(b, rstd, sq)
            for blk in range(4):
                norm_mod(b, i, rstd, tmp, lambda c, blk=blk: h2T[:, c, blk * 512:(blk + 1) * 512], blk,
                         lambda c, blk=blk: ("h2T", c, blk))
            dbg("h2T", h2T, [128, NCH, S], BF16, [("h2T", c, blk) for c in range(NCH) for blk in range(4)], b)
            ar.release(m1)
            g.barrier()

            def wout_half(half):
                cnt = 0
                for dcp in range(2):
                    sw = wload([wout_v[:, half * 4:half * 4 + 4, dcp * 512:(dcp + 1) * 512]],
                               [lambda sl: sl[:, 0:4 * 512].rearrange("p (k n) -> p k n", k=4)])
                    wo = ring[:, sw, 0:4 * 512].rearrange("p (k n) -> p k n", k=4)
                    for dj in range(4):
                        dc = dcp * 4 + dj
                        for blk in range(4):
                            pd = 4 + (cnt % 2)
                            cnt += 1
                            tok = slice(blk * 512, (blk + 1) * 512)
                            for k in range(4):
                                g.op("pe", lambda e, k=k, wo=wo, pd=pd, dj=dj, tok=tok: e.matmul(
                                    ps[pd][:, :], lhsT=wo[:, k, dj * 128:(dj + 1) * 128], rhs=mixh[:, k, tok],
                                    start=(k == 0), stop=(k == 3)),
                                    reads=[("ring", sw), ("mixh", k, blk)], writes=[("ps", pd)])
                            g.op("dve", lambda e, pd=pd, dc=dc, tok=tok: e.scalar_tensor_tensor(
                                out=xT[:, dc, tok], in0=ps[pd][:, :], scalar=gate[:, b, i, dc:dc + 1], in1=xT[:, dc, tok],
                                op0=ALU.mult, op1=ALU.add),
                                reads=[("ps", pd), ("gate", b, i), ("xT", dc, blk)], writes=[("xT", dc, blk)])

            m2 = ar.mark()
            aqT = [ar.alloc(S, BF16) for _ in range(2)]
            akT = [ar.alloc(S, BF16) for _ in range(2)]
            av2 = [ar.alloc(16 * 2 * 128, BF16).rearrange("p (t h n) -> p t h n", t=16, h=2) for _ in range(2)]
            ytm4 = ar.alloc(1024, F32)
            ysq4 = ar.alloc(1024, F32)
            yb4 = ar.alloc(1024, BF16)
            st16 = ar.alloc(16, F32)
            rp4 = ar.alloc(4 * 128, F32).rearrange("p (k a n) -> p k a n", k=4, a=16)
            Eb = [ar.alloc(512, BF16) for _ in range(3)]
            Pm = [ar.alloc(512, BF16) for _ in range(3)]
            rd = [ar.alloc(512, F32) for _ in range(2)]
            SBANK = [0, 1, 2]
            for par in range(2):
                g.op("dve", lambda e, par=par: e.memset(av2[par][:, :, :, 64:128], 1.0), writes=[("av2ones", par)])
            y16 = ytm4.rearrange("p (a n) -> p a n", a=16)
            s16 = ysq4.rearrange("p (a n) -> p a n", a=16)
            yb16 = yb4.rearrange("p (a n) -> p a n", a=16)
            y44 = ytm4.rearrange("p (t a n) -> p t a n", t=4, a=4)
            st_b = st16.rearrange("p (a o) -> p a o", o=1).to_broadcast([128, 16, 64])
            gqk_b = gqk[:].rearrange("p (o a) n -> p o a n", o=1).to_broadcast([128, 4, 4, 64])

            def load_wa(hp):
                sw = wload([win_v[:, :, 1552 + hp * 128:1552 + hp * 128 + 128],
                            win_v[:, :, 2064 + hp * 128:2064 + hp * 128 + 128],
                            win_v[:, :, 2576 + hp * 128:2576 + hp * 128 + 128]],
                           [lambda sl, q=q: sl[:, 0:NCH * 384].rearrange("p (c n) -> p c n", c=NCH)[:, :, q * 128:(q + 1) * 128]
                            for q in range(3)])
                return ring[:, sw, 0:NCH * 384].rearrange("p (c n) -> p c n", c=NCH), sw

            def proj_p1(hp, tg, wa, sw):
                par = hp % 2
                for tl in range(4):
                    tt = tg * 4 + tl
                    ts_ = slice(tt * 128, (tt + 1) * 128)
                    bank = 5 + tl // 2
                    co = (tl % 2) * 256
                    for c in range(NCH):
                        g.op("pe", lambda e, c=c, bank=bank, co=co, ts_=ts_: e.matmul(ps[bank][:, co:co + 256], lhsT=h2T[:, c, ts_], rhs=wa[:, c, 0:256],
                                                          start=(c == 0), stop=(c == NCH - 1), skip_group_check=True),
                             reads=[("ring", sw), ("h2T", c, tg)], writes=[("ps", bank)])
                    for c in range(NCH):
                        g.op("pe", lambda e, c=c, tl=tl, ts_=ts_: e.matmul(ps[7][:, tl * 128:(tl + 1) * 128], lhsT=h2T[:, c, ts_],
                                                          rhs=wa[:, c, 256:384], start=(c == 0), stop=(c == NCH - 1),
                                                          skip_group_check=True),
                             reads=[("ring", sw), ("h2T", c, tg)], writes=[("ps", 7)])
                g.op("act", lambda e: e.copy(out=ytm4[:, 0:512], in_=ps[5][:, :]), reads=[("ps", 5)], writes=["ytm4a"])
                g.op("act", lambda e: e.copy(out=ytm4[:, 512:1024], in_=ps[6][:, :]), reads=[("ps", 6)], writes=["ytm4b"])
                g.op("act", lambda e: e.copy(out=av2[par][:, tg * 4:(tg + 1) * 4, :, 0:64],
                                             in_=ps[7][:, :].rearrange("p (t h n) -> p t h n", t=4, h=2)),
                     reads=[("ps", 7)], writes=[("av2", par, tg)])
                yk = ["ytm4a", "ytm4b"]
                g.op("pool", lambda e: e.tensor_tensor(out=ysq4, in0=ytm4, in1=ytm4, op=ALU.mult), reads=yk, writes=["ysq4"])
                g.op("dve", lambda e: e.tensor_reduce(out=st16, in_=s16, axis=AX.X, op=ALU.add), reads=["ysq4"], writes=["st16"])
                g.op("act", lambda e: e.activation(out=st16, in_=st16, func=AF.Ln, bias=EPS, scale=1.0 / 64),
                     reads=["st16"], writes=["st16"])
                g.op("act", lambda e: e.activation(out=st16, in_=st16, func=AF.Exp, scale=-0.5), reads=["st16"], writes=["st16"])
                g.op("pool", lambda e: e.tensor_tensor(out=y16, in0=y16, in1=st_b, op=ALU.mult), reads=yk + ["st16"], writes=yk)
                g.op("pool", lambda e: e.tensor_tensor(out=y44, in0=y44, in1=gqk_b, op=ALU.mult), reads=yk + ["gqk"], writes=yk)
                g.op("act", lambda e: e.copy(out=yb4, in_=ytm4), reads=yk, writes=["yb4"])
                t1 = y16[:, :, 0:8]
                t2 = y16[:, :, 8:16]
                cs_ = cos4[:, tg * 4:(tg + 1) * 4, :, :].rearrange("p t a n -> p (t a) n")
                sn_ = sin4[:, tg * 4:(tg + 1) * 4, :, :].rearrange("p t a n -> p (t a) n")
                g.op("pool", lambda e: e.tensor_tensor(out=rp4[:, 0], in0=t1, in1=cs_, op=ALU.mult), reads=yk + ["cos4"], writes=[("rp4", 0)])
                g.op("pool", lambda e: e.tensor_tensor(out=rp4[:, 1], in0=t2, in1=sn_, op=ALU.mult), reads=yk + ["sin4"], writes=[("rp4", 1)])
                g.op("pool", lambda e: e.tensor_tensor(out=rp4[:, 2], in0=t2, in1=cs_, op=ALU.mult), reads=yk + ["cos4"], writes=[("rp4", 2)])
                g.op("pool", lambda e: e.tensor_tensor(out=rp4[:, 3], in0=t1, in1=sn_, op=ALU.mult), reads=yk + ["sin4"], writes=[("rp4", 3)])
                g.op("pool", lambda e: e.tensor_tensor(out=yb16[:, :, 0:8], in0=rp4[:, 0], in1=rp4[:, 1], op=ALU.subtract),
                     reads=[("rp4", 0), ("rp4", 1), "yb4"], writes=["yb4"])
                g.op("pool", lambda e: e.tensor_tensor(out=yb16[:, :, 8:16], in0=rp4[:, 2], in1=rp4[:, 3], op=ALU.add),
                     reads=[("rp4", 2), ("rp4", 3), "yb4"], writes=["yb4"])

            def proj_p2(hp, tg):
                par = hp % 2
                for tl in range(4):
                    g.op("pe", lambda e, tl=tl: e.transpose(psT[:, tl * 128:(tl + 1) * 128], yb4[:, tl * 256:tl * 256 + 128],
                                                            identb[:, :]), reads=["yb4", "identb"], writes=[PST])
                    g.op("pe", lambda e, tl=tl: e.transpose(psT[:, 512 + tl * 128:512 + (tl + 1) * 128],
                                                            yb4[:, tl * 256 + 128:tl * 256 + 256], identb[:, :]),
                         reads=["yb4", "identb"], writes=[PST])
                qs = slice(tg * 512, (tg + 1) * 512)
                g.op("act", lambda e: e.copy(out=aqT[par][:, qs], in_=psT[:, 0:512]), reads=[PST], writes=[("aqT", par, tg)])
                g.op("dve", lambda e: e.tensor_copy(out=akT[par][:, qs], in_=psT[:, 512:1024]), reads=[PST], writes=[("akT", par, tg)])

            ecnt = 0
            ocnt = 0
            wa_cur = load_wa(0)
            for tg in range(4):
                proj_p1(0, tg, *wa_cur)
                proj_p2(0, tg)
            for hp in range(4):
                par = hp % 2
                wa_nxt = load_wa(hp + 1) if hp < 3 else None
                parts = []
                if wa_nxt is not None:
                    for tg in range(4):
                        parts.append(lambda tg=tg, wa_nxt=wa_nxt, hp=hp: proj_p1(hp + 1, tg, *wa_nxt))
                        parts.append(lambda tg=tg, hp=hp: proj_p2(hp + 1, tg))
                steps = []
                seg_end = []
                for hl in range(2):
                    for qb in range(4):
                        kts = list(range(max(0, qb * 4 - 8), min(15, qb * 4 + 11) + 1))
                        po = 3 + (ocnt % 2)
                        ocnt += 1
                        for n_, kt in enumerate(kts):
                            steps.append((hl, qb, kt, n_, n_ == len(kts) - 1, po, (ecnt + len(steps)) % 3))
                        seg_end.append(len(steps) - 1)
                ecnt += len(steps)
                LA = 2

                def emit_score(si, hp=hp, par=par):
                    hl, qb, kt, n_, last, po, pS = steps[si]
                    rows = slice(hl * 64, (hl + 1) * 64)
                    qs = slice(qb * 512, (qb + 1) * 512)
                    ks = slice(kt * 128, (kt + 1) * 128)
                    j0 = MASK_C - (kt * 128 - qb * 512)
                    bk = SBANK[pS]
                    g.op("pe", lambda e: e.matmul(ps[bk][:, :], lhsT=akT[par][rows, ks], rhs=aqT[par][rows, qs], start=True, stop=True),
                         reads=[("akT", par, kt // 4), ("aqT", par, qb)], writes=[("ps", bk)])
                    g.op("act", lambda e: e.activation(out=Eb[pS], in_=ps[bk][:, :], func=AF.Exp),
                         reads=[("ps", bk)], writes=[("Eb", pS)])
                    g.op("dve", lambda e: e.tensor_tensor(out=Pm[pS], in0=Eb[pS], in1=maskT[:, j0:j0 + 512], op=ALU.mult),
                         reads=[("Eb", pS), "maskT"], writes=[("Pm", pS)])

                def emit_pv(si, hp=hp, par=par):
                    hl, qb, kt, n_, last, po, pS = steps[si]
                    rows = slice(hl * 64, (hl + 1) * 64)
                    qs = slice(qb * 512, (qb + 1) * 512)
                    g.op("pe", lambda e: e.matmul(ps[po][:, :], lhsT=av2[par][:, kt, hl, :], rhs=Pm[pS], start=(n_ == 0), stop=last),
                         reads=[("av2", par, kt // 4), ("av2ones", par), ("Pm", pS)], writes=[("ps", po)])
                    if last:
                        rr = Pm[pS]
                        g.op("act", lambda e: e.activation(out=rd[po % 2][0:64, :], in_=ps[po][64:128, :], func=AF.Ln),
                             reads=[("ps", po)], writes=[("rd", po % 2)])
                        g.op("act", lambda e: e.activation(out=rd[po % 2][0:64, :], in_=rd[po % 2][0:64, :], func=AF.Exp, scale=-1.0),
                             reads=[("rd", po % 2)], writes=[("rd", po % 2)])
                        g.op("dve", lambda e: e.tensor_tensor(out=mixh[rows, hp, qs], in0=ps[po][0:64, :], in1=rd[po % 2][0:64, :],
                                                              op=ALU.mult),
                             reads=[("ps", po), ("rd", po % 2)], writes=[("mixh", hp, qb)])
                for si in range(len(steps) + LA):
                    if si < len(steps):
                        emit_score(si)
                    if si >= LA:
                        emit_pv(si - LA)
                    if si in seg_end and parts:
                        parts.pop(0)()
                while parts:
                    parts.pop(0)()
            dbg("mixa", mixh, [128, 4, S], BF16, [("mixh", k_, q_) for k_ in range(4) for q_ in range(4)], b)
            wout_half(1)
            ar.release(m2)
            g.barrier()

            m3 = ar.mark()
            G = ar.alloc(16 * 16, F32).rearrange("p (t n) -> p t n", t=16)
            G4 = G.rearrange("p t (a h) -> p t a h", a=4)
            LF = ar.alloc(16 * 8, F32).rearrange("p (t n) -> p t n", t=16)
            LF4 = LF.rearrange("p t (a h) -> p t a h", a=2)
            sB = ar.alloc(16 * 16, F32).rearrange("p (t n) -> p t n", t=16)
            T1 = ar.alloc(16 * 8, F32).rearrange("p (t n) -> p t n", t=16)
            T2 = T1
            Call = ar.alloc(16 * 8, F32).rearrange("p (t n) -> p t n", t=16)
            Aall = ar.alloc(16 * 8, F32).rearrange("p (t n) -> p t n", t=16)
            EBa = ar.alloc(16 * 8, F32).rearrange("p (t n) -> p t n", t=16)
            EBH = ar.alloc(16 * 8, F32).rearrange("p (t n) -> p t n", t=16)
            mqT = ar.alloc(S, BF16)
            mkT = ar.alloc(S, BF16)
            ktok = ar.alloc(16 * 128, BF16).rearrange("p (t n) -> p t n", t=16)
            V1 = ar.alloc(16 * 2 * 130, BF16).rearrange("p (t h n) -> p t h n", t=16, h=2)
            sgb = [ar.alloc(256, BF16) for _ in range(2)]
            hacc = ar.alloc(16 * 256, F32).rearrange("p (t h n) -> p t h n", t=16, h=2)
            etm = [ar.alloc(256, F32) for _ in range(2)]
            Cst = ar.alloc(2 * 130, F32).rearrange("p (d n) -> p d n", d=2)
            Cbf = ar.alloc(2 * 130, BF16).rearrange("p (d n) -> p d n", d=2)
            Sm = [ar.alloc(128, BF16) for _ in range(8)]
            vw = [ar.alloc(130, BF16) for _ in range(8)]
            dn = [ar.alloc(2, F32) for _ in range(8)]
            hsq = [ar.alloc(256, F32)] * 2
            hst = [ar.alloc(2, F32) for _ in range(2)]
            hy = [ar.alloc(256, F32) for _ in range(2)]
            hyb = [ar.alloc(256, BF16) for _ in range(2)]
            g.op("dve", lambda e: e.memset(V1[:, :, :, 128:130], 1.0), writes=["V1ones"])

            for mp in range(2):
                sw = wload([win_v[:, :, mp * 128:(mp + 1) * 128], win_v[:, :, 256 + mp * 128:256 + (mp + 1) * 128]],
                           [lambda sl, q=q: sl[:, 0:NCH * 256].rearrange("p (c n) -> p c n", c=NCH)[:, :, q * 128:(q + 1) * 128]
                            for q in range(2)])
                wq = ring[:, sw, 0:NCH * 256].rearrange("p (c n) -> p c n", c=NCH)
                cnt = 0
                for q in range(2):
                    for blk in range(4):
                        pp = 5 + (cnt % 2)
                        cnt += 1
                        tok = slice(blk * 512, (blk + 1) * 512)
                        for c in range(NCH):
                            g.op("pe", lambda e, c=c, q=q, pp=pp, tok=tok, wq=wq: e.matmul(
                                ps[pp][:, :], lhsT=wq[:, c, q * 128:(q + 1) * 128], rhs=h2T[:, c, tok],
                                start=(c == 0), stop=(c == NCH - 1)),
                                reads=[("ring", sw), ("h2T", c, blk)], writes=[("ps", pp)])
                        if q == 0:
                            g.op("act", lambda e, pp=pp, tok=tok: e.copy(out=mqT[:, tok], in_=ps[pp][:, :]),
                                 reads=[("ps", pp)], writes=[("mqT", blk)])
                        else:
                            g.op("act", lambda e, pp=pp, tok=tok: e.mul(out=mkT[:, tok], in_=ps[pp][:, :], mul=0.125),
                                 reads=[("ps", pp)], writes=[("mkT", blk)])
                s1 = wload([win_v[:, :, 256 + mp * 128:256 + (mp + 1) * 128], win_v[:, :, 512 + mp * 256:512 + (mp + 1) * 256],
                            win_v[:, :, 1536:1552]],
                           [lambda sl: sl[:, 0:NCH * 400].rearrange("p (c n) -> p c n", c=NCH)[:, :, 0:128],
                            lambda sl: sl[:, 0:NCH * 400].rearrange("p (c n) -> p c n", c=NCH)[:, :, 128:384],
                            lambda sl: sl[:, 0:NCH * 400].rearrange("p (c n) -> p c n", c=NCH)[:, :, 384:400]])
                w1 = ring[:, s1, 0:NCH * 400].rearrange("p (c n) -> p c n", c=NCH)
                for tt in range(16):
                    ts_ = slice(tt * 128, (tt + 1) * 128)
                    blk = tt // 4
                    pp = 5 + (tt % 2)
                    for c in range(NCH):
                        g.op("pe", lambda e, c=c, ts_=ts_, w1=w1, pp=pp: e.matmul(
                            ps[pp][:, 0:400], lhsT=h2T[:, c, ts_], rhs=w1[:, c, :], start=(c == 0), stop=(c == NCH - 1)),
                            reads=[("ring", s1), ("h2T", c, blk)], writes=[("ps", pp)])
                    g.op("act", lambda e, tt=tt, pp=pp: e.mul(out=ktok[:, tt, :], in_=ps[pp][:, 0:128], mul=0.125),
                         reads=[("ps", pp)], writes=[("ktok", tt)])
                    g.op("act", lambda e, tt=tt, pp=pp: e.copy(out=V1[:, tt, :, 0:128],
                                                               in_=ps[pp][:, 128:384].rearrange("p (h n) -> p h n", h=2)),
                         reads=[("ps", pp)], writes=[("V1", tt)])
                    if mp == 0:
                        g.op("dve", lambda e, tt=tt, pp=pp: e.tensor_tensor(out=G[:, tt, :], in0=ps[pp][:, 384:400],
                                                                           in1=gbias[:, :], op=ALU.add),
                             reads=[("ps", pp), "gbias"], writes=[("G", tt)])
                if mp == 0:
                    allG = [("G", tt) for tt in range(16)]
                    g.op("act", lambda e: e.activation(out=LF4, in_=G4[:, :, 1:4:2, :], func=AF.Exp, scale=-1.0),
                         reads=allG, writes=["LF"])
                    g.op("act", lambda e: e.activation(out=LF, in_=LF, func=AF.Ln, bias=1.0, scale=1.0),
                         reads=["LF"], writes=["LF"])
                    g.op("dve", lambda e: e.tensor_scalar(out=LF, in0=LF, scalar1=-1.0, scalar2=None, op0=ALU.mult),
                         reads=["LF"], writes=["LF"])
                    for tt in range(16):
                        o = tt * 16
                        g.op("pe", lambda e, tt=tt, o=o: e.matmul(ps[4][:, o:o + 4], lhsT=trif[:, :], rhs=LF4[:, tt, 0, :],
                                                                  start=True, stop=True, skip_group_check=True),
                             reads=["LF", "trif"], writes=[("ps", 4)])
                        g.op("pe", lambda e, tt=tt, o=o: e.matmul(ps[4][:, o + 4:o + 8], lhsT=trib[:, :], rhs=LF4[:, tt, 1, :],
                                                                  start=True, stop=True, skip_group_check=True),
                             reads=["LF", "trib"], writes=[("ps", 4)])
                        g.op("pe", lambda e, tt=tt, o=o: e.matmul(ps[4][:, o + 8:o + 16], lhsT=onesf[:, :], rhs=LF[:, tt, :],
                                                                  start=True, stop=True, skip_group_check=True),
                             reads=["LF", "onesf"], writes=[("ps", 4)])
                    g.op("act", lambda e: e.copy(out=sB, in_=ps[4][:, 0:256].rearrange("p (t n) -> p t n", t=16)),
                         reads=[("ps", 4)], writes=["sB"])
                    LI = G4[:, :, 0:4:2, :]
                    T1v = T1.rearrange("p t (a h) -> p t a h", a=2)
                    bc4 = sB[:, :, 0:8].rearrange("p t (a h) -> p t a h", a=2)
                    g.op("dve", lambda e: e.tensor_tensor(out=T1v, in0=LI, in1=bc4, op=ALU.subtract),
                         reads=allG + ["sB"], writes=["T1"])
                    g.op("dve", lambda e: e.scalar_tensor_tensor(out=T1, in0=sB[:, :, 8:16], scalar=0.5, in1=T1,
                                                                 op0=ALU.mult, op1=ALU.add), reads=["sB", "T1"], writes=["T1"])
                    g.op("act", lambda e: e.activation(out=Call, in_=T1, func=AF.Exp), reads=["T1"], writes=["Call"])
                    g.op("dve", lambda e: e.scalar_tensor_tensor(out=T2, in0=sB[:, :, 8:16], scalar=-0.5, in1=sB[:, :, 0:8],
                                                                 op0=ALU.mult, op1=ALU.add), reads=["sB"], writes=["T1"])
                    g.op("act", lambda e: e.activation(out=Aall, in_=T2, func=AF.Exp), reads=["T1"], writes=["Aall"])
                    g.op("act", lambda e: e.activation(out=EBa, in_=sB[:, :, 8:16], func=AF.Exp), reads=["sB"], writes=["EBa"])
                    g.op("act", lambda e: e.activation(out=EBH, in_=sB[:, :, 8:16], func=AF.Exp, scale=0.5),
                         reads=["sB"], writes=["EBH"])
                g.op("dve", lambda e: e.memset(Cst, 0.0), writes=[("Cst", d_, hl_) for d_ in range(2) for hl_ in range(2)])
                g.op("dve", lambda e: e.memset(Cbf, 0.0), writes=[("Cbf", d_, hl_) for d_ in range(2) for hl_ in range(2)])
                qcnt = [0]

                def chain_a(step, hl, dr, mp=mp):
                    head = mp * 2 + hl
                    rows = slice(hl * 64, (hl + 1) * 64)
                    tt = step if dr == 0 else 15 - step
                    ts_ = slice(tt * 128, (tt + 1) * 128)
                    j = dr * 4 + head
                    c_ = hl * 2 + dr
                    z = c_ * 2 + (step % 2)
                    pq = c_
                    g.op("pe", lambda e: e.matmul(ps[pq][:, 0:128], lhsT=mkT[rows, ts_], rhs=mqT[rows, ts_], start=True, stop=True),
                         reads=[("mkT", tt // 4), ("mqT", tt // 4)], writes=[("ps", pq)])
                    g.op("act", lambda e: e.activation(out=vw[z][:, 0:129], in_=V1[:, tt, hl, 0:129], func=AF.Identity,
                                                       scale=Call[:, tt, j:j + 1]),
                         reads=[("V1", tt), "V1ones", "Call"], writes=[("vw", z)])
                    yield
                    mk_ = mkf if dr == 0 else mkb
                    g.op("dve", lambda e: e.tensor_tensor(out=Sm[z], in0=ps[pq][:, 0:128], in1=mk_[:, :], op=ALU.mult),
                         reads=[("ps", pq), "mkf", "mkb"], writes=[("Sm", z)])
                    yield

                def chain_b(step, hl, dr, mp=mp):
                    head = mp * 2 + hl
                    rows = slice(hl * 64, (hl + 1) * 64)
                    tt = step if dr == 0 else 15 - step
                    ts_ = slice(tt * 128, (tt + 1) * 128)
                    j = dr * 4 + head
                    c_ = hl * 2 + dr
                    z = c_ * 2 + (step % 2)
                    pc = 4 + c_
                    pso = ps[pc][:, 256:385]
                    g.op("pe", lambda e: e.matmul(pso, lhsT=Sm[z], rhs=vw[z][:, 0:129], start=True, stop=False),
                         reads=[("Sm", z), ("vw", z)], writes=[("ps", pc)])
                    g.op("pe", lambda e: e.matmul(pso, lhsT=mqT[rows, ts_], rhs=Cbf[rows, dr, 0:129], start=False, stop=True),
                         reads=[("mqT", tt // 4), ("Cbf", dr, hl)], writes=[("ps", pc)])
                    g.op("pe", lambda e: e.matmul(ps[pc][rows, 0:129], lhsT=ktok[:, tt, rows], rhs=vw[z][:, 0:129], start=True, stop=True,
                                                  skip_group_check=True),
                         reads=[("ktok", tt), ("vw", z)], writes=[("ps", pc)])
                    yield
                    g.op("dve", lambda e: e.tensor_scalar(out=Cst[rows, dr, 0:129], in0=Cst[rows, dr, 0:129],
                                                          scalar1=EBa[rows, tt, j:j + 1], scalar2=None, op0=ALU.mult),
                         reads=[("Cst", dr, hl), "EBa"], writes=[("Cst", dr, hl)])
                    acol = Aall[:, tt, j:j + 1]
                    g.op("act", lambda e: e.activation(out=dn[z][:, 0:1], in_=ps[pc][:, 384:385], func=AF.Abs, scale=acol),
                         reads=[("ps", pc), "Aall"], writes=[("dn", z)])
                    yield
                    g.op("dve", lambda e: e.scalar_tensor_tensor(out=Cst[rows, dr, 0:129], in0=ps[pc][rows, 0:129],
                                                                 scalar=EBH[rows, tt, j:j + 1], in1=Cst[rows, dr, 0:129],
                                                                 op0=ALU.mult, op1=ALU.add),
                         reads=[("ps", pc), ("Cst", dr, hl), "EBH"], writes=[("Cst", dr, hl)])
                    yield
                    if step < 15:
                        tn = tt + 1 if dr == 0 else tt - 1
                        g.op("act", lambda e: e.activation(out=Cbf[rows, dr, 0:129], in_=Cst[rows, dr, 0:129], func=AF.Identity,
                                                           scale=EBH[rows, tn, j:j + 1]),
                             reads=[("Cst", dr, hl), "EBH"], writes=[("Cbf", dr, hl)])
                    g.op("dve", lambda e: e.tensor_scalar(out=dn[z][:, 0:1], in0=dn[z][:, 0:1], scalar1=1.0, scalar2=None, op0=ALU.max),
                         reads=[("dn", z)], writes=[("dn", z)])
                    yield
                    g.op("dve", lambda e: e.reciprocal(out=dn[z][:, 0:1], in_=dn[z][:, 0:1]), reads=[("dn", z)], writes=[("dn", z)])
                    yield
                    g.op("dve", lambda e: e.tensor_tensor(out=dn[z][:, 1:2], in0=dn[z][:, 0:1], in1=acol, op=ALU.mult),
                         reads=[("dn", z), "Aall"], writes=[("dn", z)])
                    yield
                    is_first = (dr == 0 and tt <= 15 - tt) or (dr == 1 and (15 - tt) < tt)
                    if is_first:
                        g.op("act", lambda e: e.activation(out=hacc[:, tt, hl, :], in_=ps[pc][:, 256:384], func=AF.Identity,
                                                           scale=dn[z][:, 1:2]),
                             reads=[("ps", pc), ("dn", z)], writes=[("hacc", tt, hl)])
                    else:
                        g.op("dve", lambda e: e.scalar_tensor_tensor(out=hacc[:, tt, hl, :], in0=ps[pc][:, 256:384], scalar=dn[z][:, 1:2],
                                                                     in1=hacc[:, tt, hl, :], op0=ALU.mult, op1=ALU.add),
                             reads=[("ps", pc), ("dn", z), ("hacc", tt, hl)], writes=[("hacc", tt, hl)])
                    yield

                def round_robin(gens):
                    gens = list(gens)
                    while gens:
                        alive = []
                        for ge in gens:
                            try:
                                next(ge)
                                alive.append(ge)
                            except StopIteration:
                                pass
                        gens = alive

                chains = [(hl_, dr_) for hl_ in range(2) for dr_ in range(2)]
                round_robin([chain_a(0, hl_, dr_) for hl_, dr_ in chains])
                for step in range(16):
                    if step < 15:
                        round_robin([chain_a(step + 1, hl_, dr_) for hl_, dr_ in chains])
                    round_robin([chain_b(step, hl_, dr_) for hl_, dr_ in chains])
                s3 = wload([win_v[:, :, 1024 + mp * 256:1024 + (mp + 1) * 256]],
                           [lambda sl: sl[:, 0:NCH * 256].rearrange("p (c n) -> p c n", c=NCH)])
                w3 = ring[:, s3, 0:NCH * 256].rearrange("p (c n) -> p c n", c=NCH)
                for tt in range(16):
                    u = tt % 2
                    ts_ = slice(tt * 128, (tt + 1) * 128)
                    pp = 5 + (tt % 2)
                    for c in range(NCH):
                        g.op("pe", lambda e, c=c, ts_=ts_, w3=w3, pp=pp: e.matmul(
                            ps[pp][:, 0:256], lhsT=h2T[:, c, ts_], rhs=w3[:, c, :], start=(c == 0), stop=(c == NCH - 1)),
                            reads=[("ring", s3), ("h2T", c, tt // 4)], writes=[("ps", pp)])
                    g.op("act", lambda e, u=u, pp=pp: e.activation(out=etm[u], in_=ps[pp][:, 0:256], func=AF.Exp, scale=-1.0),
                         reads=[("ps", pp)], writes=[("etm", u)])
                    g.op("dve", lambda e, u=u: e.tensor_scalar(out=etm[u], in0=etm[u], scalar1=1.0, scalar2=None, op0=ALU.add),
                         reads=[("etm", u)], writes=[("etm", u)])
                    g.op("dve", lambda e, u=u: e.reciprocal(out=etm[u], in_=etm[u]),
                         reads=[("etm", u)], writes=[("etm", u)])
                    hk = [("hacc", tt, 0), ("hacc", tt, 1)]
                    hv = hacc[:, tt, :, :]
                    h3 = hsq[u].rearrange("p (h n) -> p h n", h=2)
                    y3 = hy[u].rearrange("p (h n) -> p h n", h=2)
                    g.op("dve", lambda e, hv=hv, h3=h3: e.tensor_tensor(out=h3, in0=hv, in1=hv, op=ALU.mult),
                         reads=hk, writes=["hsq"])
                    g.op("dve", lambda e, u=u, h3=h3: e.tensor_reduce(out=hst[u], in_=h3, axis=AX.X, op=ALU.add),
                         reads=["hsq"], writes=[("hst", u)])
                    g.op("act", lambda e, u=u: e.activation(out=hst[u], in_=hst[u], func=AF.Ln, bias=EPS, scale=1.0 / 128),
                         reads=[("hst", u)], writes=[("hst", u)])
                    g.op("act", lambda e, u=u: e.activation(out=hst[u], in_=hst[u], func=AF.Exp, scale=-0.5),
                         reads=[("hst", u)], writes=[("hst", u)])
                    for hl in range(2):
                        head = mp * 2 + hl
                        g.op("dve", lambda e, u=u, hl=hl, head=head, hv=hv, y3=y3: e.scalar_tensor_tensor(
                            out=y3[:, hl, :], in0=hv[:, hl, :], scalar=hst[u][:, hl:hl + 1], in1=gmh[:, head * 128:(head + 1) * 128],
                            op0=ALU.mult, op1=ALU.mult), reads=hk + [("hst", u), "gmh"], writes=[("hy", u, hl)])
                    g.op("dve", lambda e, u=u, tt=tt: e.tensor_tensor(out=hyb[u], in0=hy[u], in1=etm[u], op=ALU.mult),
                         reads=[("hy", u, 0), ("hy", u, 1), ("etm", u)], writes=[("hyb", u)])
                    def fin_tr(tt, mp=mp):
                        u = tt % 2
                        ts_ = slice(tt * 128, (tt + 1) * 128)
                        for hl in range(2):
                            g.op("pe", lambda e, hl=hl: e.transpose(psT[:, 256 + hl * 128:256 + (hl + 1) * 128],
                                                                    hyb[u][:, hl * 128:(hl + 1) * 128], identb[:, :]),
                                 reads=[("hyb", u), "identb"], writes=[PST])
                        g.op("act", lambda e: e.copy(
                            out=mixh[:, mp * 2:mp * 2 + 2, ts_], in_=psT[:, 256:512].rearrange("p (h n) -> p h n", h=2)),
                            reads=[PST], writes=[("mixh", mp * 2, tt // 4), ("mixh", mp * 2 + 1, tt // 4)])
                    if tt >= 1:
                        fin_tr(tt - 1)
                    if tt == 15:
                        fin_tr(15)
            dbg("mixm", mixh, [128, 4, S], BF16, [("mixh", k_, q_) for k_ in range(4) for q_ in range(4)], b)
            dbg("G", G, [128, 16, 16], F32, [("G", t_) for t_ in range(16)], b)
            dbg("LF", LF, [128, 16, 8], F32, ["LF"], b)
            dbg("sB", sB, [128, 16, 16], F32, ["sB"], b)
            dbg("Call", Call, [128, 16, 8], F32, ["Call"], b)
            dbg("Aall", Aall, [128, 16, 8], F32, ["Aall"], b)
            dbg("hacc", hacc, [128, 16, 2, 128], F32, [("hacc", t_, h_) for t_ in range(16) for h_ in range(2)], b)
            wout_half(0)
            ar.release(m0)
            g.barrier()

        def final(b, raw=False):
            m0 = ar.mark()
            outs = []
            if raw:
                for c in range(NCH):
                    outs.append(g.dma("sp", out_d[b, c * 128:(c + 1) * 128, :], xT[:, c, :],
                                      reads=[("xT", c, blk) for blk in range(4)]))
                return outs
            rstd = ar.alloc(S, F32)
            sq = ar.alloc(NCH * 512, BF16).rearrange("p (c n) -> p c n", c=NCH)
            ob = [ar.alloc(512, F32) for _ in range(4)]
            rms_rstd(b, rstd, sq)
            n = 0
            for blk in range(4):
                tok = slice(blk * 512, (blk + 1) * 512)
                for c in range(NCH):
                    o_ = ob[n % 4]
                    g.op("dve", lambda e, c=c, tok=tok, o_=o_: e.scalar_tensor_tensor(
                        out=o_, in0=xT[:, c, tok], scalar=g4[:, 3, c:c + 1], in1=rstd[:, tok], op0=ALU.mult, op1=ALU.mult),
                        reads=[("xT", c, blk), ("rstd", blk), "g4"], writes=[("ob", n % 4)])
                    outs.append(g.dma("sp", out_d[b, c * 128:(c + 1) * 128, tok], o_, reads=[("ob", n % 4)]))
                    n += 1
            ar.release(m0)
            g.barrier()
            return outs

        outs = []
        adaln(list(range(0, 6)))
        derive(0)
        for b in range(2):
            load_x(b)
            ffn(b, 0, 0)
            if b == 0:
                adaln(list(range(6, 18)))
                derive(1)
                derive(2)
            if stage >= 2:
                mixer(b)
            if stage >= 3:
                ffn(b, 2, 1)
            outs += final(b, raw=(stage < 3))
        g.emit(final_wait_ops=outs + dbg_outs)
    return nc


def _consts():
    p = np.arange(128)[:, None]
    j = np.arange(MASK_W)[None, :]
    dlt = p - j + MASK_C
    a = np.abs(dlt)
    m = (a <= 64).astype(np.float32) + ((dlt % 4 == 0) & (a <= 256)) + ((dlt % 16 == 0) & (a <= 1024))
    maskT = m.astype(ml_dtypes.bfloat16)
    identb = np.eye(128, dtype=np.float32).astype(ml_dtypes.bfloat16)
    u = np.arange(128)[:, None]
    t = np.arange(128)[None, :]
    trif = (u <= t).astype(np.float32)
    trib = (u >= t).astype(np.float32)
    maskf = (u <= t).astype(np.float32).astype(ml_dtypes.bfloat16)
    maskb = (u >= t).astype(np.float32).astype(ml_dtypes.bfloat16)
    half = 8
    inv_freq = (500000.0 ** (-2.0 * np.arange(half, dtype=np.float32) / 16.0)).astype(np.float32)
    pos = np.arange(S, dtype=np.float32)
    ang = (pos[:, None] * inv_freq[None, :]).astype(np.float32)
    cos = np.cos(ang).astype(np.float32).reshape(16, 128, 8).transpose(1, 0, 2)
    sin = np.sin(ang).astype(np.float32).reshape(16, 128, 8).transpose(1, 0, 2)
    cos4 = np.ascontiguousarray(np.broadcast_to(cos[:, :, None, :], (128, 16, 4, 8))).astype(np.float32)
    sin4 = np.ascontiguousarray(np.broadcast_to(sin[:, :, None, :], (128, 16, 4, 8))).astype(np.float32)
    return dict(maskT=maskT, identb=identb, trif=trif, trib=trib, maskf=maskf, maskb=maskb, cos4=cos4, sin4=sin4)


def _cols(v):
    return np.ascontiguousarray(np.asarray(v, np.float32).reshape(-1, 128).T)


_NC_CACHE = {}


def kernel(x, c, w_ada, b_ada, g_ffn1, w_gu1, w_down1, g_mix, w_in, gate_bias, g_q, g_k, g_mh,
           w_out, g_ffn2, w_gu2, w_down2, g_final):
    stage = int(os.environ.get("MK_STAGE", "3"))
    x = np.asarray(x, np.float32)
    c = np.asarray(c, np.float32)
    if stage not in _NC_CACHE:
        _NC_CACHE[stage] = build_program(stage)
    nc = _NC_CACHE[stage]
    consts = _consts()
    shared = dict(
        w_ada=np.ascontiguousarray(np.asarray(w_ada, np.float32)[0]),
        b_adaT=_cols(np.asarray(b_ada)[0]),
        g4=np.ascontiguousarray(np.stack([_cols(np.asarray(v)[0]) for v in (g_ffn1, g_mix, g_ffn2, g_final)], axis=1)),
        w_gu1=np.ascontiguousarray(np.asarray(w_gu1, np.float32)[0]),
        w_gu2=np.ascontiguousarray(np.asarray(w_gu2, np.float32)[0]),
        w_down1=np.ascontiguousarray(np.asarray(w_down1, np.float32)[0]),
        w_down2=np.ascontiguousarray(np.asarray(w_down2, np.float32)[0]),
        w_in=np.ascontiguousarray(np.asarray(w_in, np.float32)[0]),
        w_out=np.ascontiguousarray(np.asarray(w_out, np.float32)[0]),
        gbias_rep=np.ascontiguousarray(np.broadcast_to(np.asarray(gate_bias, np.float32)[0].reshape(1, 16), (128, 16))),
        gqk_rep=np.ascontiguousarray(np.broadcast_to(
            np.stack([np.asarray(g_q, np.float32)[0]] * 2 + [np.asarray(g_k, np.float32)[0]] * 2)[None], (128, 4, 64))),
        gmh_rep=np.ascontiguousarray(np.broadcast_to(np.asarray(g_mh, np.float32)[0][None, :], (128, 512))),
        **consts,
    )
    in_maps = []
    for i in range(8):
        m = dict(shared)
        m["xT"] = np.ascontiguousarray(x[2 * i:2 * i + 2].transpose(0, 2, 1))
        m["cT"] = np.ascontiguousarray(c[2 * i:2 * i + 2].reshape(2, NCH, 128).transpose(2, 1, 0))
        in_maps.append(m)
    res = run_bass_kernel_spmd(nc, in_maps, core_ids=list(range(8)))
    out = np.empty((16, S, D), np.float32)
    for i in range(8):
        out[2 * i:2 * i + 2] = res.results[i]["outT"].transpose(0, 2, 1)
    return out
```

```python
import os
import contextlib
import numpy as np
import ml_dtypes
import concourse.bass as bass
import concourse.mybir as mybir
from concourse.bass_utils import run_bass_kernel_spmd

F32 = mybir.dt.float32
BF16 = mybir.dt.bfloat16
AF = mybir.ActivationFunctionType
ALU = mybir.AluOpType
AX = mybir.AxisListType

ENG_NAMES = ("pe", "act", "dve", "pool", "sp")
D = 1024
S = 2048
DFF = 2816
NCH = 8
NF = 22
EPS = 1e-6
MASK_C = 1408
MASK_W = 2944
NSLOT = 3
SLOT_ELEMS = 4096


class Op:
    __slots__ = ("eng", "fn", "deps", "is_dma", "sem", "val", "has_dep", "idx", "name", "prewait")

    def __init__(self, eng, fn, name=""):
        self.eng = eng
        self.fn = fn
        self.deps = []
        self.is_dma = False
        self.sem = None
        self.val = 0
        self.has_dep = False
        self.idx = 0
        self.name = name
        self.prewait = None


class Graph:
    def __init__(self, nc, n_dma_sems=16):
        self.nc = nc
        self.ops = {e: [] for e in ENG_NAMES}
        self.last_writer = {}
        self.readers = {}
        self.n_dma_sems = n_dma_sems
        self.dma_count = {e: 0 for e in ENG_NAMES}
        self.dma_ops = {e: [] for e in ENG_NAMES}
        self.barrier_deps = {}
        self.sp_since_barrier = []

    def _link(self, op, deps):
        latest = {}
        keep = []
        for d in deps:
            if d is op:
                continue
            if d.is_dma:
                keep.append(d)
                continue
            if d.eng == "pe" and op.eng == "pe":
                continue
            cur = latest.get(d.eng)
            if cur is None or d.idx > cur.idx:
                latest[d.eng] = d
        keep.extend(latest.values())
        seen = set(id(d) for d in op.deps)
        for d in keep:
            if id(d) in seen:
                continue
            seen.add(id(d))
            op.deps.append(d)
            d.has_dep = True

    def _add_deps(self, op, reads, writes):
        deps = []
        for r in reads:
            w = self.last_writer.get(r)
            if w is not None:
                deps.append(w)
            if (isinstance(r, tuple) and r[0] == "ps") or (isinstance(r, str) and r.startswith("psT")):
                deps.extend(x for x in self.readers.get(r, ()) if x.eng != op.eng)
        for w_ in writes:
            w = self.last_writer.get(w_)
            if w is not None:
                deps.append(w)
            deps.extend(self.readers.get(w_, ()))
        b = self.barrier_deps.pop(op.eng, None)
        if b:
            deps.extend(b)
        self._link(op, deps)
        for r in reads:
            self.readers.setdefault(r, []).append(op)
        for w_ in writes:
            self.last_writer[w_] = op
            self.readers[w_] = []

    def op(self, eng, fn, reads=(), writes=(), name=""):
        o = Op(eng, fn, name)
        o.idx = len(self.ops[eng])
        self._add_deps(o, reads, writes)
        self.ops[eng].append(o)
        return o

    def dma(self, eng, out, in_, reads=(), writes=(), name=""):
        def fn(e, out=out, in_=in_):
            return e.dma_start(out=out, in_=in_)
        o = Op(eng, fn, name)
        o.is_dma = True
        i = self.dma_count[eng]
        self.dma_count[eng] += 1
        o.idx = i
        o.val = 16 * (i // self.n_dma_sems + 1)
        if i >= self.n_dma_sems:
            o.prewait = self.dma_ops[eng][i - self.n_dma_sems]
        self.dma_ops[eng].append(o)
        self._add_deps(o, reads, writes)
        self.ops[eng].append(o)
        if eng == "sp":
            self.sp_since_barrier.append(o)
        return o

    def barrier(self):
        b = []
        for e in ("pe", "act", "dve"):
            for o in reversed(self.ops[e]):
                b.append(o)
                break
        b.extend(self.sp_since_barrier)
        self.sp_since_barrier = []
        for e in ("pe", "act", "dve", "sp"):
            self.barrier_deps[e] = list(b) + list(self.barrier_deps.get(e, ()))

    def emit(self, final_wait_ops=()):
        nc = self.nc
        with contextlib.ExitStack() as st:
            esem = {e: st.enter_context(nc.semaphore("s_" + e)) for e in ENG_NAMES}
            dsem = {}
            for e in ENG_NAMES:
                if self.dma_count[e]:
                    dsem[e] = [st.enter_context(nc.semaphore("d_%s_%d" % (e, i)))
                               for i in range(min(self.n_dma_sems, self.dma_count[e]))]
            for e in ENG_NAMES:
                m = 0
                for o in self.ops[e]:
                    if o.is_dma:
                        o.sem = dsem[e][o.idx % self.n_dma_sems]
                    elif o.has_dep:
                        m += 1
                        o.sem = esem[e]
                        o.val = m
            block = st.enter_context(nc.Block())
            handles = {"pe": block.tensor, "act": block.scalar, "dve": block.vector,
                       "pool": block.gpsimd, "sp": block.sync}

            def make(e):
                ops = self.ops[e]

                def body(eng):
                    waited = {}

                    def wait(d):
                        key = id(d.sem)
                        if waited.get(key, 0) >= d.val:
                            return
                        waited[key] = d.val
                        eng.wait_ge(d.sem, d.val)
                    for o in ops:
                        if o.prewait is not None:
                            wait(o.prewait)
                        for d in o.deps:
                            wait(d)
                        inst = o.fn(eng)
                        if o.is_dma:
                            inst.then_inc(o.sem, 16)
                        elif o.has_dep:
                            inst.then_inc(o.sem, 1)
                    if e == "sp":
                        for d in final_wait_ops:
                            wait(d)
                return body
            for e in ENG_NAMES:
                if self.ops[e] or (e == "sp" and final_wait_ops):
                    handles[e](make(e))


class Arena:
    def __init__(self, ap2d, nelem_bf16):
        self.ap = ap2d
        self.cap = nelem_bf16
        self.off = 0

    def mark(self):
        return self.off

    def release(self, m):
        self.off = m

    def alloc(self, nelem, dt):
        n16 = nelem * (2 if dt == F32 else 1)
        self.off = (self.off + 1) // 2 * 2
        assert self.off + n16 <= self.cap, ("arena overflow", self.off, n16, self.cap)
        a = self.ap[:, self.off:self.off + n16]
        self.off += n16
        if dt == F32:
            a = a.bitcast(F32)
        return a


def build_program(stage=3):
    nc = bass.Bass("TRN2", target_bir_lowering=False)

    def din(name, shape, dt=F32):
        return nc.dram_tensor(name, list(shape), dt, kind="ExternalInput").ap()
    xT_d = din("xT", [2, D, S])
    cT_d = din("cT", [128, NCH, 2])
    wada_d = din("w_ada", [D, 9 * D])
    bada_d = din("b_adaT", [128, 72])
    g4_d = din("g4", [128, 4, NCH])
    wgu_d = [din("w_gu1", [D, 2 * DFF]), din("w_gu2", [D, 2 * DFF])]
    wdn_d = [din("w_down1", [DFF, D]), din("w_down2", [DFF, D])]
    win_d = din("w_in", [D, 3088])
    wout_d = din("w_out", [D, D])
    gbias_d = din("gbias_rep", [128, 16])
    gqk_d = din("gqk_rep", [128, 4, 64])
    gmh_d = din("gmh_rep", [128, 512])
    mask_d = din("maskT", [128, MASK_W], BF16)
    identb_d = din("identb", [128, 128], BF16)
    trif_d = din("trif", [128, 128])
    trib_d = din("trib", [128, 128])
    mkf_d = din("maskf", [128, 128], BF16)
    mkb_d = din("maskb", [128, 128], BF16)
    cos_d = din("cos4", [128, 16, 4, 8])
    sin_d = din("sin4", [128, 16, 4, 8])
    out_d = nc.dram_tensor("outT", [2, D, S], F32, kind="ExternalOutput").ap()

    st = contextlib.ExitStack()
    with st:
        def sbt(name, shape, dt):
            return st.enter_context(nc.sbuf_tensor(name, shape, dt))
        xT = sbt("xT_sb", [128, NCH, S], F32)
        ring = sbt("ring", [128, NSLOT, SLOT_ELEMS], BF16)
        maskT = sbt("maskT_sb", [128, MASK_W], BF16)
        identb = sbt("identb_sb", [128, 128], BF16)
        onesb = sbt("onesb", [128, 128], BF16)
        onesf = sbt("onesf", [128, 128], F32)
        trif = sbt("trif_sb", [128, 128], F32)
        trib = sbt("trib_sb", [128, 128], F32)
        mkf = sbt("mkf_sb", [128, 128], BF16)
        mkb = sbt("mkb_sb", [128, 128], BF16)
        cos4 = sbt("cos4_sb", [128, 16, 4, 8], F32)
        sin4 = sbt("sin4_sb", [128, 16, 4, 8], F32)
        gqk = sbt("gqk_sb", [128, 4, 64], F32)
        gmh = sbt("gmh_sb", [128, 512], F32)
        gbias = sbt("gbias_sb", [128, 16], F32)
        g4 = sbt("g4_sb", [128, 4, NCH], F32)
        badaT = sbt("bada_sb", [128, 72], F32)
        cT = sbt("cT_sb", [128, NCH, 2], F32)
        csT = sbt("csT_sb", [128, NCH, 2], BF16)
        modT = sbt("modT", [128, 72, 2], F32)
        geff = sbt("geff", [128, 2, 3, NCH], F32)
        gate = sbt("gate", [128, 2, 3, NCH], F32)
        ARENA_N = 52736
        arena_t = sbt("arena", [128, ARENA_N], BF16)
        ar = Arena(arena_t[:, :], ARENA_N)
        ps = [st.enter_context(nc.psum_tensor("ps%d" % i, [128, 512], F32)) for i in range(8)]
        psT = ps[7][:, :].bitcast(BF16)
        PST = ("ps", 7)

        g = Graph(nc)
        ring_ctr = [0]
        DBG = os.environ.get("MK_DEBUG") == "1"
        dbg_outs = []

        def dbg(name, ap, shape, dt, reads, b=0):
            if not DBG or b != 0:
                return
            t = nc.dram_tensor("dbg_" + name, list(shape), dt, kind="ExternalOutput").ap()
            dbg_outs.append(g.dma("sp", t, ap, reads=reads))

        def wload(src_aps, views, name=""):
            s = ring_ctr[0] % NSLOT
            ring_ctr[0] += 1
            for src, vw in zip(src_aps, views):
                g.dma("pool", vw(ring[:, s, :]), src, writes=[("ring", s)], name=name)
            return s

        for dst, src, key in ((maskT[:, :], mask_d, "maskT"), (identb[:, :], identb_d, "identb"),
                              (trif[:, :], trif_d, "trif"), (trib[:, :], trib_d, "trib"),
                              (mkf[:, :], mkf_d, "mkf"), (mkb[:, :], mkb_d, "mkb"),
                              (cos4[:], cos_d, "cos4"), (sin4[:], sin_d, "sin4"),
                              (gqk[:], gqk_d, "gqk"), (gmh[:, :], gmh_d, "gmh"), (gbias[:, :], gbias_d, "gbias"),
                              (g4[:], g4_d, "g4"), (badaT[:, :], bada_d, "badaT"), (cT[:], cT_d, "cT")):
            g.dma("sp", dst, src, writes=[key])
        g.op("dve", lambda e: e.memset(onesb[:, :], 1.0), writes=["onesb"])
        g.op("dve", lambda e: e.memset(onesf[:, :], 1.0), writes=["onesf"])
        g.op("dve", lambda e: e.tensor_scalar(out=gqk[:, 0:2, :], in0=gqk[:, 0:2, :], scalar1=0.125, scalar2=None,
                                              op0=ALU.mult), reads=["gqk"], writes=["gqk"])

        def load_x(b):
            for c in range(NCH):
                g.dma("sp", xT[:, c, :], xT_d[b, c * 128:(c + 1) * 128, :],
                      writes=[("xT", c, blk) for blk in range(4)])

        g.op("act", lambda e: e.activation(out=csT[:], in_=cT[:], func=AF.Silu), reads=["cT"], writes=["csT"])
        wada_v = wada_d.rearrange("(c p) n -> p c n", p=128)

        def adaln(groups):
            for grp in groups:
                s = wload([wada_v[:, :, grp * 512:(grp + 1) * 512]],
                          [lambda sl: sl.rearrange("p (c n) -> p c n", c=NCH)])
                wv = ring[:, s, :].rearrange("p (c n) -> p c n", c=NCH)
                for j in range(4):
                    n = grp * 4 + j
                    for k in range(NCH):
                        g.op("pe", lambda e, n=n, k=k, j=j, wv=wv: e.matmul(
                            ps[6][:, 2 * n:2 * n + 2], lhsT=wv[:, k, j * 128:(j + 1) * 128], rhs=csT[:, k, :],
                            start=(k == 0), stop=(k == NCH - 1), skip_group_check=True),
                            reads=[("ring", s), "csT"], writes=[("ps", 6)])
            n0, n1 = groups[0] * 4, groups[-1] * 4 + 4
            part = 0 if n0 == 0 else 1
            for b in range(2):
                g.op("dve", lambda e, b=b: e.tensor_tensor(
                    out=modT[:, n0:n1, b], in0=ps[6][:, 2 * n0 + b:2 * n1 + b:2], in1=badaT[:, n0:n1], op=ALU.add),
                    reads=[("ps", 6), "badaT"], writes=[("modT", part, b)])

        def derive(i):
            n0 = 3 * i * 8
            part = 0 if i == 0 else 1
            for b in range(2):
                g.op("dve", lambda e, b=b: e.scalar_tensor_tensor(
                    out=geff[:, b, i, :], in0=modT[:, n0 + 8:n0 + 16, b], scalar=1.0, in1=g4[:, i, :],
                    op0=ALU.add, op1=ALU.mult), reads=[("modT", part, b), "g4"], writes=[("geff", b, i)])
                g.op("dve", lambda e, b=b: e.tensor_scalar(
                    out=gate[:, b, i, :], in0=modT[:, n0 + 16:n0 + 24, b], scalar1=(1.0 if i == 1 else 0.5),
                    scalar2=None, op0=ALU.mult), reads=[("modT", part, b)], writes=[("gate", b, i)])

        def shift_col(b, i, c):
            return modT[:, 3 * i * 8 + c, b:b + 1]

        def rms_rstd(b, rstd, sq):
            for blk in range(4):
                tok = slice(blk * 512, (blk + 1) * 512)
                for c in range(NCH):
                    g.op("act", lambda e, c=c, tok=tok: e.activation(out=sq[:, c, :], in_=xT[:, c, tok], func=AF.Square),
                         reads=[("xT", c, blk)], writes=[("sq", c)])
                for c in range(NCH):
                    g.op("pe", lambda e, c=c: e.matmul(ps[6][:, :], lhsT=onesb[:, :], rhs=sq[:, c, :],
                                                      start=(c == 0), stop=(c == NCH - 1)),
                         reads=["onesb", ("sq", c)], writes=[("ps", 6)])
                g.op("act", lambda e, tok=tok: e.activation(out=rstd[:, tok], in_=ps[6][:, :], func=AF.Ln,
                                                            bias=EPS, scale=1.0 / D),
                     reads=[("ps", 6)], writes=[("rstd", blk)])
            for blk in range(4):
                tok = slice(blk * 512, (blk + 1) * 512)
                g.op("act", lambda e, tok=tok: e.activation(out=rstd[:, tok], in_=rstd[:, tok], func=AF.Exp, scale=-0.5),
                     reads=[("rstd", blk)], writes=[("rstd", blk)])

        def norm_mod(b, i, rstd, tmp, dst_fn, blk, keyfn):
            tok = slice(blk * 512, (blk + 1) * 512)
            for c in range(NCH):
                tb = tmp[c % 2]
                g.op("dve", lambda e, c=c, tb=tb: e.tensor_tensor(out=tb, in0=xT[:, c, tok], in1=rstd[:, tok], op=ALU.mult),
                     reads=[("xT", c, blk), ("rstd", blk)], writes=[("tmp", c % 2)])
                g.op("act", lambda e, c=c, tb=tb: e.activation(out=dst_fn(c), in_=tb, func=AF.Identity,
                                                               bias=shift_col(b, i, c), scale=geff[:, b, i, c:c + 1]),
                     reads=[("tmp", c % 2), ("geff", b, i), ("modT", 0 if i == 0 else 1, b)], writes=[keyfn(c)])

        def ffn(b, i, which):
            wgu_v = wgu_d[which].rearrange("(c p) n -> p c n", p=128)
            wdn_v = wdn_d[which].rearrange("(k p) n -> p k n", p=128)
            m0 = ar.mark()
            rstd = ar.alloc(S, F32)
            sq = ar.alloc(NCH * 512, BF16).rearrange("p (c n) -> p c n", c=NCH)
            tmp = [ar.alloc(512, F32) for _ in range(2)]
            hT = ar.alloc(NCH * 1024, BF16).rearrange("p (c n) -> p c n", c=NCH)
            actT = ar.alloc(NF * 1024, BF16).rearrange("p (c n) -> p c n", c=NF)
            sg = [ar.alloc(512, BF16) for _ in range(2)]
            rms_rstd(b, rstd, sq)
            cnt = 0
            for sb in range(2):
                for lb in range(2):
                    blk = sb * 2 + lb
                    norm_mod(b, i, rstd, tmp, lambda c, lb=lb: hT[:, c, lb * 512:(lb + 1) * 512], blk,
                             lambda c, lb=lb: ("hT", c, lb))
                for grp in range(11):
                    c0 = grp * 256
                    sgw = wload([wgu_v[:, :, c0:c0 + 256], wgu_v[:, :, DFF + c0:DFF + c0 + 256]],
                                [lambda sl, q=q: sl.rearrange("p (c n) -> p c n", c=NCH)[:, :, q * 256:(q + 1) * 256]
                                 for q in range(2)])
                    wgv = ring[:, sgw, :].rearrange("p (c n) -> p c n", c=NCH)
                    for j in range(2):
                        f = grp * 2 + j
                        for lb in range(2):
                            pg = cnt % 2
                            cnt += 1
                            hs = slice(lb * 512, (lb + 1) * 512)
                            for k in range(NCH):
                                g.op("pe", lambda e, k=k, j=j, wgv=wgv, pg=pg, hs=hs: e.matmul(
                                    ps[pg][:, :], lhsT=wgv[:, k, j * 128:(j + 1) * 128], rhs=hT[:, k, hs],
                                    start=(k == 0), stop=(k == NCH - 1)),
                                    reads=[("ring", sgw), ("hT", k, lb)], writes=[("ps", pg)])
                            for k in range(NCH):
                                g.op("pe", lambda e, k=k, j=j, wgv=wgv, pg=pg, hs=hs: e.matmul(
                                    ps[2 + pg][:, :], lhsT=wgv[:, k, 256 + j * 128:256 + (j + 1) * 128], rhs=hT[:, k, hs],
                                    start=(k == 0), stop=(k == NCH - 1)),
                                    reads=[("ring", sgw), ("hT", k, lb)], writes=[("ps", 2 + pg)])
                            g.op("act", lambda e, pg=pg: e.activation(out=sg[pg], in_=ps[pg][:, :], func=AF.Silu),
                                 reads=[("ps", pg)], writes=[("sg", pg)])
                            g.op("dve", lambda e, pg=pg, f=f, hs=hs: e.tensor_tensor(
                                out=actT[:, f, hs], in0=ps[2 + pg][:, :], in1=sg[pg], op=ALU.mult),
                                reads=[("ps", 2 + pg), ("sg", pg)], writes=[("actT", f, lb)])
                for dc in range(NCH):
                    sw = wload([wdn_v[:, :, dc * 128:(dc + 1) * 128]],
                               [lambda sl: sl[:, 0:NF * 128].rearrange("p (k n) -> p k n", k=NF)])
                    wd = ring[:, sw, 0:NF * 128].rearrange("p (k n) -> p k n", k=NF)
                    for lb in range(2):
                        blk = sb * 2 + lb
                        pd = 4 + (cnt % 2)
                        cnt += 1
                        hs = slice(lb * 512, (lb + 1) * 512)
                        for k in range(NF):
                            g.op("pe", lambda e, k=k, wd=wd, pd=pd, hs=hs: e.matmul(
                                ps[pd][:, :], lhsT=wd[:, k, :], rhs=actT[:, k, hs],
                                start=(k == 0), stop=(k == NF - 1)),
                                reads=[("ring", sw), ("actT", k, lb)], writes=[("ps", pd)])
                        tok = slice(blk * 512, (blk + 1) * 512)
                        g.op("dve", lambda e, pd=pd, dc=dc, tok=tok: e.scalar_tensor_tensor(
                            out=xT[:, dc, tok], in0=ps[pd][:, :], scalar=gate[:, b, i, dc:dc + 1], in1=xT[:, dc, tok],
                            op0=ALU.mult, op1=ALU.add),
                            reads=[("ps", pd), ("gate", b, i), ("xT", dc, blk)], writes=[("xT", dc, blk)])
            ar.release(m0)
            g.barrier()

        def mixer(b):
            i = 1
            win_v = win_d.rearrange("(c p) n -> p c n", p=128)
            wout_v = wout_d.rearrange("(k p) n -> p k n", p=128)
            m0 = ar.mark()
            h2T = ar.alloc(NCH * S, BF16).rearrange("p (c n) -> p c n", c=NCH)
            mixh = ar.alloc(4 * S, BF16).rearrange("p (c n) -> p c n", c=4)
            m1 = ar.mark()
            rstd = ar.alloc(S, F32)
            sq = ar.alloc(NCH * 512, BF16).rearrange("p (c n) -> p c n", c=NCH)
            tmp = [ar.alloc(512, F32) for _ in range(2)]
            rms_rstd(b, rstd, sq)
            for blk in range(4):
                norm_mod(b, i, rstd, tmp, lambda c, blk=blk: h2T[:, c, blk * 512:(blk + 1) * 512], blk,
                         lambda c, blk=blk: ("h2T", c, blk))
            dbg("h2T", h2T, [128, NCH, S], BF16, [("h2T", c, blk) for c in range(NCH) for blk in range(4)], b)
            ar.release(m1)
            g.barrier()

            def wout_half(half):
                cnt = 0
                for dcp in range(2):
                    sw = wload([wout_v[:, half * 4:half * 4 + 4, dcp * 512:(dcp + 1) * 512]],
                               [lambda sl: sl[:, 0:4 * 512].rearrange("p (k n) -> p k n", k=4)])
                    wo = ring[:, sw, 0:4 * 512].rearrange("p (k n) -> p k n", k=4)
                    for dj in range(4):
                        dc = dcp * 4 + dj
                        for blk in range(4):
                            pd = 4 + (cnt % 2)
                            cnt += 1
                            tok = slice(blk * 512, (blk + 1) * 512)
                            for k in range(4):
                                g.op("pe", lambda e, k=k, wo=wo, pd=pd, dj=dj, tok=tok: e.matmul(
                                    ps[pd][:, :], lhsT=wo[:, k, dj * 128:(dj + 1) * 128], rhs=mixh[:, k, tok],
                                    start=(k == 0), stop=(k == 3)),
                                    reads=[("ring", sw), ("mixh", k, blk)], writes=[("ps", pd)])
                            g.op("dve", lambda e, pd=pd, dc=dc, tok=tok: e.scalar_tensor_tensor(
                                out=xT[:, dc, tok], in0=ps[pd][:, :], scalar=gate[:, b, i, dc:dc + 1], in1=xT[:, dc, tok],
                                op0=ALU.mult, op1=ALU.add),
                                reads=[("ps", pd), ("gate", b, i), ("xT", dc, blk)], writes=[("xT", dc, blk)])

            m2 = ar.mark()
            aqT = [ar.alloc(S, BF16) for _ in range(2)]
            akT = [ar.alloc(S, BF16) for _ in range(2)]
            av2 = [ar.alloc(16 * 2 * 128, BF16).rearrange("p (t h n) -> p t h n", t=16, h=2) for _ in range(2)]
            ytm4 = ar.alloc(1024, F32)
            ysq4 = ar.alloc(1024, F32)
            yb4 = ar.alloc(1024, BF16)
            st16 = ar.alloc(16, F32)
            rp4 = ar.alloc(4 * 128, F32).rearrange("p (k a n) -> p k a n", k=4, a=16)
            Eb = [ar.alloc(512, BF16) for _ in range(3)]
            Pm = [ar.alloc(512, BF16) for _ in range(3)]
            rd = [ar.alloc(512, F32) for _ in range(2)]
            SBANK = [0, 1, 2]
            for par in range(2):
                g.op("dve", lambda e, par=par: e.memset(av2[par][:, :, :, 64:128], 1.0), writes=[("av2ones", par)])
            y16 = ytm4.rearrange("p (a n) -> p a n", a=16)
            s16 = ysq4.rearrange("p (a n) -> p a n", a=16)
            yb16 = yb4.rearrange("p (a n) -> p a n", a=16)
            y44 = ytm4.rearrange("p (t a n) -> p t a n", t=4, a=4)
            st_b = st16.rearrange("p (a o) -> p a o", o=1).to_broadcast([128, 16, 64])
            gqk_b = gqk[:].rearrange("p (o a) n -> p o a n", o=1).to_broadcast([128, 4, 4, 64])

            def load_wa(hp):
                sw = wload([win_v[:, :, 1552 + hp * 128:1552 + hp * 128 + 128],
                            win_v[:, :, 2064 + hp * 128:2064 + hp * 128 + 128],
                            win_v[:, :, 2576 + hp * 128:2576 + hp * 128 + 128]],
                           [lambda sl, q=q: sl[:, 0:NCH * 384].rearrange("p (c n) -> p c n", c=NCH)[:, :, q * 128:(q + 1) * 128]
                            for q in range(3)])
                return ring[:, sw, 0:NCH * 384].rearrange("p (c n) -> p c n", c=NCH), sw

            def proj_p1(hp, tg, wa, sw):
                par = hp % 2
                for tl in range(4):
                    tt = tg * 4 + tl
                    ts_ = slice(tt * 128, (tt + 1) * 128)
                    bank = 5 + tl // 2
                    co = (tl % 2) * 256
                    for c in range(NCH):
                        g.op("pe", lambda e, c=c, bank=bank, co=co, ts_=ts_: e.matmul(ps[bank][:, co:co + 256], lhsT=h2T[:, c, ts_], rhs=wa[:, c, 0:256],
                                                          start=(c == 0), stop=(c == NCH - 1), skip_group_check=True),
                             reads=[("ring", sw), ("h2T", c, tg)], writes=[("ps", bank)])
                    for c in range(NCH):
                        g.op("pe", lambda e, c=c, tl=tl, ts_=ts_: e.matmul(ps[7][:, tl * 128:(tl + 1) * 128], lhsT=h2T[:, c, ts_],
                                                          rhs=wa[:, c, 256:384], start=(c == 0), stop=(c == NCH - 1),
                                                          skip_group_check=True),
                             reads=[("ring", sw), ("h2T", c, tg)], writes=[("ps", 7)])
                g.op("act", lambda e: e.copy(out=ytm4[:, 0:512], in_=ps[5][:, :]), reads=[("ps", 5)], writes=["ytm4a"])
                g.op("act", lambda e: e.copy(out=ytm4[:, 512:1024], in_=ps[6][:, :]), reads=[("ps", 6)], writes=["ytm4b"])
                g.op("act", lambda e: e.copy(out=av2[par][:, tg * 4:(tg + 1) * 4, :, 0:64],
                                             in_=ps[7][:, :].rearrange("p (t h n) -> p t h n", t=4, h=2)),
                     reads=[("ps", 7)], writes=[("av2", par, tg)])
                yk = ["ytm4a", "ytm4b"]
                g.op("pool", lambda e: e.tensor_tensor(out=ysq4, in0=ytm4, in1=ytm4, op=ALU.mult), reads=yk, writes=["ysq4"])
                g.op("dve", lambda e: e.tensor_reduce(out=st16, in_=s16, axis=AX.X, op=ALU.add), reads=["ysq4"], writes=["st16"])
                g.op("act", lambda e: e.activation(out=st16, in_=st16, func=AF.Ln, bias=EPS, scale=1.0 / 64),
                     reads=["st16"], writes=["st16"])
                g.op("act", lambda e: e.activation(out=st16, in_=st16, func=AF.Exp, scale=-0.5), reads=["st16"], writes=["st16"])
                g.op("pool", lambda e: e.tensor_tensor(out=y16, in0=y16, in1=st_b, op=ALU.mult), reads=yk + ["st16"], writes=yk)
                g.op("pool", lambda e: e.tensor_tensor(out=y44, in0=y44, in1=gqk_b, op=ALU.mult), reads=yk + ["gqk"], writes=yk)
                g.op("act", lambda e: e.copy(out=yb4, in_=ytm4), reads=yk, writes=["yb4"])
                t1 = y16[:, :, 0:8]
                t2 = y16[:, :, 8:16]
                cs_ = cos4[:, tg * 4:(tg + 1) * 4, :, :].rearrange("p t a n -> p (t a) n")
                sn_ = sin4[:, tg * 4:(tg + 1) * 4, :, :].rearrange("p t a n -> p (t a) n")
                g.op("pool", lambda e: e.tensor_tensor(out=rp4[:, 0], in0=t1, in1=cs_, op=ALU.mult), reads=yk + ["cos4"], writes=[("rp4", 0)])
                g.op("pool", lambda e: e.tensor_tensor(out=rp4[:, 1], in0=t2, in1=sn_, op=ALU.mult), reads=yk + ["sin4"], writes=[("rp4", 1)])
                g.op("pool", lambda e: e.tensor_tensor(out=rp4[:, 2], in0=t2, in1=cs_, op=ALU.mult), reads=yk + ["cos4"], writes=[("rp4", 2)])
                g.op("pool", lambda e: e.tensor_tensor(out=rp4[:, 3], in0=t1, in1=sn_, op=ALU.mult), reads=yk + ["sin4"], writes=[("rp4", 3)])
                g.op("pool", lambda e: e.tensor_tensor(out=yb16[:, :, 0:8], in0=rp4[:, 0], in1=rp4[:, 1], op=ALU.subtract),
                     reads=[("rp4", 0), ("rp4", 1), "yb4"], writes=["yb4"])
                g.op("pool", lambda e: e.tensor_tensor(out=yb16[:, :, 8:16], in0=rp4[:, 2], in1=rp4[:, 3], op=ALU.add),
                     reads=[("rp4", 2), ("rp4", 3), "yb4"], writes=["yb4"])

            def proj_p2(hp, tg):
                par = hp % 2
                for tl in range(4):
                    g.op("pe", lambda e, tl=tl: e.transpose(psT[:, tl * 128:(tl + 1) * 128], yb4[:, tl * 256:tl * 256 + 128],
                                                            identb[:, :]), reads=["yb4", "identb"], writes=[PST])
                    g.op("pe", lambda e, tl=tl: e.transpose(psT[:, 512 + tl * 128:512 + (tl + 1) * 128],
                                                            yb4[:, tl * 256 + 128:tl * 256 + 256], identb[:, :]),
                         reads=["yb4", "identb"], writes=[PST])
                qs = slice(tg * 512, (tg + 1) * 512)
                g.op("act", lambda e: e.copy(out=aqT[par][:, qs], in_=psT[:, 0:512]), reads=[PST], writes=[("aqT", par, tg)])
                g.op("dve", lambda e: e.tensor_copy(out=akT[par][:, qs], in_=psT[:, 512:1024]), reads=[PST], writes=[("akT", par, tg)])

            ecnt = 0
            ocnt = 0
            wa_cur = load_wa(0)
            for tg in range(4):
                proj_p1(0, tg, *wa_cur)
                proj_p2(0, tg)
            for hp in range(4):
                par = hp % 2
                wa_nxt = load_wa(hp + 1) if hp < 3 else None
                parts = []
                if wa_nxt is not None:
                    for tg in range(4):
                        parts.append(lambda tg=tg, wa_nxt=wa_nxt, hp=hp: proj_p1(hp + 1, tg, *wa_nxt))
                        parts.append(lambda tg=tg, hp=hp: proj_p2(hp + 1, tg))
                steps = []
                seg_end = []
                for hl in range(2):
                    for qb in range(4):
                        kts = list(range(max(0, qb * 4 - 8), min(15, qb * 4 + 11) + 1))
                        po = 3 + (ocnt % 2)
                        ocnt += 1
                        for n_, kt in enumerate(kts):
                            steps.append((hl, qb, kt, n_, n_ == len(kts) - 1, po, (ecnt + len(steps)) % 3))
                        seg_end.append(len(steps) - 1)
                ecnt += len(steps)
                LA = 2

                def emit_score(si, hp=hp, par=par):
                    hl, qb, kt, n_, last, po, pS = steps[si]
                    rows = slice(hl * 64, (hl + 1) * 64)
                    qs = slice(qb * 512, (qb + 1) * 512)
                    ks = slice(kt * 128, (kt + 1) * 128)
                    j0 = MASK_C - (kt * 128 - qb * 512)
                    bk = SBANK[pS]
                    g.op("pe", lambda e: e.matmul(ps[bk][:, :], lhsT=akT[par][rows, ks], rhs=aqT[par][rows, qs], start=True, stop=True),
                         reads=[("akT", par, kt // 4), ("aqT", par, qb)], writes=[("ps", bk)])
                    g.op("act", lambda e: e.activation(out=Eb[pS], in_=ps[bk][:, :], func=AF.Exp),
                         reads=[("ps", bk)], writes=[("Eb", pS)])
                    g.op("dve", lambda e: e.tensor_tensor(out=Pm[pS], in0=Eb[pS], in1=maskT[:, j0:j0 + 512], op=ALU.mult),
                         reads=[("Eb", pS), "maskT"], writes=[("Pm", pS)])

                def emit_pv(si, hp=hp, par=par):
                    hl, qb, kt, n_, last, po, pS = steps[si]
                    rows = slice(hl * 64, (hl + 1) * 64)
                    qs = slice(qb * 512, (qb + 1) * 512)
                    g.op("pe", lambda e: e.matmul(ps[po][:, :], lhsT=av2[par][:, kt, hl, :], rhs=Pm[pS], start=(n_ == 0), stop=last),
                         reads=[("av2", par, kt // 4), ("av2ones", par), ("Pm", pS)], writes=[("ps", po)])
                    if last:
                        rr = Pm[pS]
                        g.op("act", lambda e: e.activation(out=rd[po % 2][0:64, :], in_=ps[po][64:128, :], func=AF.Ln),
                             reads=[("ps", po)], writes=[("rd", po % 2)])
                        g.op("act", lambda e: e.activation(out=rd[po % 2][0:64, :], in_=rd[po % 2][0:64, :], func=AF.Exp, scale=-1.0),
                             reads=[("rd", po % 2)], writes=[("rd", po % 2)])
                        g.op("dve", lambda e: e.tensor_tensor(out=mixh[rows, hp, qs], in0=ps[po][0:64, :], in1=rd[po % 2][0:64, :],
                                                              op=ALU.mult),
                             reads=[("ps", po), ("rd", po % 2)], writes=[("mixh", hp, qb)])
                for si in range(len(steps) + LA):
                    if si < len(steps):
                        emit_score(si)
                    if si >= LA:
                        emit_pv(si - LA)
                    if si in seg_end and parts:
                        parts.pop(0)()
                while parts:
                    parts.pop(0)()
            dbg("mixa", mixh, [128, 4, S], BF16, [("mixh", k_, q_) for k_ in range(4) for q_ in range(4)], b)
            wout_half(1)
            ar.release(m2)
            g.barrier()

            m3 = ar.mark()
            G = ar.alloc(16 * 16, F32).rearrange("p (t n) -> p t n", t=16)
            G4 = G.rearrange("p t (a h) -> p t a h", a=4)
            LF = ar.alloc(16 * 8, F32).rearrange("p (t n) -> p t n", t=16)
            LF4 = LF.rearrange("p t (a h) -> p t a h", a=2)
            sB = ar.alloc(16 * 16, F32).rearrange("p (t n) -> p t n", t=16)
            T1 = ar.alloc(16 * 8, F32).rearrange("p (t n) -> p t n", t=16)
            T2 = T1
            Call = ar.alloc(16 * 8, F32).rearrange("p (t n) -> p t n", t=16)
            Aall = ar.alloc(16 * 8, F32).rearrange("p (t n) -> p t n", t=16)
            EBa = ar.alloc(16 * 8, F32).rearrange("p (t n) -> p t n", t=16)
            EBH = ar.alloc(16 * 8, F32).rearrange("p (t n) -> p t n", t=16)
            mqT = ar.alloc(S, BF16)
            mkT = ar.alloc(S, BF16)
            ktok = ar.alloc(16 * 128, BF16).rearrange("p (t n) -> p t n", t=16)
            V1 = ar.alloc(16 * 2 * 130, BF16).rearrange("p (t h n) -> p t h n", t=16, h=2)
            sgb = [ar.alloc(256, BF16) for _ in range(2)]
            hacc = ar.alloc(16 * 256, F32).rearrange("p (t h n) -> p t h n", t=16, h=2)
            etm = [ar.alloc(256, F32) for _ in range(2)]
            Cst = ar.alloc(2 * 130, F32).rearrange("p (d n) -> p d n", d=2)
            Cbf = ar.alloc(2 * 130, BF16).rearrange("p (d n) -> p d n", d=2)
            Sm = [ar.alloc(128, BF16) for _ in range(8)]
            vw = [ar.alloc(130, BF16) for _ in range(8)]
            dn = [ar.alloc(2, F32) for _ in range(8)]
            hsq = [ar.alloc(256, F32)] * 2
            hst = [ar.alloc(2, F32) for _ in range(2)]
            hy = [ar.alloc(256, F32) for _ in range(2)]
            hyb = [ar.alloc(256, BF16) for _ in range(2)]
            g.op("dve", lambda e: e.memset(V1[:, :, :, 128:130], 1.0), writes=["V1ones"])

            for mp in range(2):
                sw = wload([win_v[:, :, mp * 128:(mp + 1) * 128], win_v[:, :, 256 + mp * 128:256 + (mp + 1) * 128]],
                           [lambda sl, q=q: sl[:, 0:NCH * 256].rearrange("p (c n) -> p c n", c=NCH)[:, :, q * 128:(q + 1) * 128]
                            for q in range(2)])
                wq = ring[:, sw, 0:NCH * 256].rearrange("p (c n) -> p c n", c=NCH)
                cnt = 0
                for q in range(2):
                    for blk in range(4):
                        pp = 5 + (cnt % 2)
                        cnt += 1
                        tok = slice(blk * 512, (blk + 1) * 512)
                        for c in range(NCH):
                            g.op("pe", lambda e, c=c, q=q, pp=pp, tok=tok, wq=wq: e.matmul(
                                ps[pp][:, :], lhsT=wq[:, c, q * 128:(q + 1) * 128], rhs=h2T[:, c, tok],
                                start=(c == 0), stop=(c == NCH - 1)),
                                reads=[("ring", sw), ("h2T", c, blk)], writes=[("ps", pp)])
                        if q == 0:
                            g.op("act", lambda e, pp=pp, tok=tok: e.copy(out=mqT[:, tok], in_=ps[pp][:, :]),
                                 reads=[("ps", pp)], writes=[("mqT", blk)])
                        else:
                            g.op("act", lambda e, pp=pp, tok=tok: e.mul(out=mkT[:, tok], in_=ps[pp][:, :], mul=0.125),
                                 reads=[("ps", pp)], writes=[("mkT", blk)])
                s1 = wload([win_v[:, :, 256 + mp * 128:256 + (mp + 1) * 128], win_v[:, :, 512 + mp * 256:512 + (mp + 1) * 256],
                            win_v[:, :, 1536:1552]],
                           [lambda sl: sl[:, 0:NCH * 400].rearrange("p (c n) -> p c n", c=NCH)[:, :, 0:128],
                            lambda sl: sl[:, 0:NCH * 400].rearrange("p (c n) -> p c n", c=NCH)[:, :, 128:384],
                            lambda sl: sl[:, 0:NCH * 400].rearrange("p (c n) -> p c n", c=NCH)[:, :, 384:400]])
                w1 = ring[:, s1, 0:NCH * 400].rearrange("p (c n) -> p c n", c=NCH)
                for tt in range(16):
                    ts_ = slice(tt * 128, (tt + 1) * 128)
                    blk = tt // 4
                    pp = 5 + (tt % 2)
                    for c in range(NCH):
                        g.op("pe", lambda e, c=c, ts_=ts_, w1=w1, pp=pp: e.matmul(
                            ps[pp][:, 0:400], lhsT=h2T[:, c, ts_], rhs=w1[:, c, :], start=(c == 0), stop=(c == NCH - 1)),
                            reads=[("ring", s1), ("h2T", c, blk)], writes=[("ps", pp)])
                    g.op("act", lambda e, tt=tt, pp=pp: e.mul(out=ktok[:, tt, :], in_=ps[pp][:, 0:128], mul=0.125),
                         reads=[("ps", pp)], writes=[("ktok", tt)])
                    g.op("act", lambda e, tt=tt, pp=pp: e.copy(out=V1[:, tt, :, 0:128],
                                                               in_=ps[pp][:, 128:384].rearrange("p (h n) -> p h n", h=2)),
                         reads=[("ps", pp)], writes=[("V1", tt)])
                    if mp == 0:
                        g.op("dve", lambda e, tt=tt, pp=pp: e.tensor_tensor(out=G[:, tt, :], in0=ps[pp][:, 384:400],
                                                                           in1=gbias[:, :], op=ALU.add),
                             reads=[("ps", pp), "gbias"], writes=[("G", tt)])
                if mp == 0:
                    allG = [("G", tt) for tt in range(16)]
                    g.op("act", lambda e: e.activation(out=LF4, in_=G4[:, :, 1:4:2, :], func=AF.Exp, scale=-1.0),
                         reads=allG, writes=["LF"])
                    g.op("act", lambda e: e.activation(out=LF, in_=LF, func=AF.Ln, bias=1.0, scale=1.0),
                         reads=["LF"], writes=["LF"])
                    g.op("dve", lambda e: e.tensor_scalar(out=LF, in0=LF, scalar1=-1.0, scalar2=None, op0=ALU.mult),
                         reads=["LF"], writes=["LF"])
                    for tt in range(16):
                        o = tt * 16
                        g.op("pe", lambda e, tt=tt, o=o: e.matmul(ps[4][:, o:o + 4], lhsT=trif[:, :], rhs=LF4[:, tt, 0, :],
                                                                  start=True, stop=True, skip_group_check=True),
                             reads=["LF", "trif"], writes=[("ps", 4)])
                        g.op("pe", lambda e, tt=tt, o=o: e.matmul(ps[4][:, o + 4:o + 8], lhsT=trib[:, :], rhs=LF4[:, tt, 1, :],
                                                                  start=True, stop=True, skip_group_check=True),
                             reads=["LF", "trib"], writes=[("ps", 4)])
                        g.op("pe", lambda e, tt=tt, o=o: e.matmul(ps[4][:, o + 8:o + 16], lhsT=onesf[:, :], rhs=LF[:, tt, :],
                                                                  start=True, stop=True, skip_group_check=True),
                             reads=["LF", "onesf"], writes=[("ps", 4)])
                    g.op("act", lambda e: e.copy(out=sB, in_=ps[4][:, 0:256].rearrange("p (t n) -> p t n", t=16)),
                         reads=[("ps", 4)], writes=["sB"])
                    LI = G4[:, :, 0:4:2, :]
                    T1v = T1.rearrange("p t (a h) -> p t a h", a=2)
                    bc4 = sB[:, :, 0:8].rearrange("p t (a h) -> p t a h", a=2)
                    g.op("dve", lambda e: e.tensor_tensor(out=T1v, in0=LI, in1=bc4, op=ALU.subtract),
                         reads=allG + ["sB"], writes=["T1"])
                    g.op("dve", lambda e: e.scalar_tensor_tensor(out=T1, in0=sB[:, :, 8:16], scalar=0.5, in1=T1,
                                                                 op0=ALU.mult, op1=ALU.add), reads=["sB", "T1"], writes=["T1"])
                    g.op("act", lambda e: e.activation(out=Call, in_=T1, func=AF.Exp), reads=["T1"], writes=["Call"])
                    g.op("dve", lambda e: e.scalar_tensor_tensor(out=T2, in0=sB[:, :, 8:16], scalar=-0.5, in1=sB[:, :, 0:8],
                                                                 op0=ALU.mult, op1=ALU.add), reads=["sB"], writes=["T1"])
                    g.op("act", lambda e: e.activation(out=Aall, in_=T2, func=AF.Exp), reads=["T1"], writes=["Aall"])
                    g.op("act", lambda e: e.activation(out=EBa, in_=sB[:, :, 8:16], func=AF.Exp), reads=["sB"], writes=["EBa"])
                    g.op("act", lambda e: e.activation(out=EBH, in_=sB[:, :, 8:16], func=AF.Exp, scale=0.5),
                         reads=["sB"], writes=["EBH"])
                g.op("dve", lambda e: e.memset(Cst, 0.0), writes=[("Cst", d_, hl_) for d_ in range(2) for hl_ in range(2)])
                g.op("dve", lambda e: e.memset(Cbf, 0.0), writes=[("Cbf", d_, hl_) for d_ in range(2) for hl_ in range(2)])
                qcnt = [0]

                def chain_a(step, hl, dr, mp=mp):
                    head = mp * 2 + hl
                    rows = slice(hl * 64, (hl + 1) * 64)
                    tt = step if dr == 0 else 15 - step
                    ts_ = slice(tt * 128, (tt + 1) * 128)
                    j = dr * 4 + head
                    c_ = hl * 2 + dr
                    z = c_ * 2 + (step % 2)
                    pq = c_
                    g.op("pe", lambda e: e.matmul(ps[pq][:, 0:128], lhsT=mkT[rows, ts_], rhs=mqT[rows, ts_], start=True, stop=True),
                         reads=[("mkT", tt // 4), ("mqT", tt // 4)], writes=[("ps", pq)])
                    g.op("act", lambda e: e.activation(out=vw[z][:, 0:129], in_=V1[:, tt, hl, 0:129], func=AF.Identity,
                                                       scale=Call[:, tt, j:j + 1]),
                         reads=[("V1", tt), "V1ones", "Call"], writes=[("vw", z)])
                    yield
                    mk_ = mkf if dr == 0 else mkb
                    g.op("dve", lambda e: e.tensor_tensor(out=Sm[z], in0=ps[pq][:, 0:128], in1=mk_[:, :], op=ALU.mult),
                         reads=[("ps", pq), "mkf", "mkb"], writes=[("Sm", z)])
                    yield

                def chain_b(step, hl, dr, mp=mp):
                    head = mp * 2 + hl
                    rows = slice(hl * 64, (hl + 1) * 64)
                    tt = step if dr == 0 else 15 - step
                    ts_ = slice(tt * 128, (tt + 1) * 128)
                    j = dr * 4 + head
                    c_ = hl * 2 + dr
                    z = c_ * 2 + (step % 2)
                    pc = 4 + c_
                    pso = ps[pc][:, 256:385]
                    g.op("pe", lambda e: e.matmul(pso, lhsT=Sm[z], rhs=vw[z][:, 0:129], start=True, stop=False),
                         reads=[("Sm", z), ("vw", z)], writes=[("ps", pc)])
                    g.op("pe", lambda e: e.matmul(pso, lhsT=mqT[rows, ts_], rhs=Cbf[rows, dr, 0:129], start=False, stop=True),
                         reads=[("mqT", tt // 4), ("Cbf", dr, hl)], writes=[("ps", pc)])
                    g.op("pe", lambda e: e.matmul(ps[pc][rows, 0:129], lhsT=ktok[:, tt, rows], rhs=vw[z][:, 0:129], start=True, stop=True,
                                                  skip_group_check=True),
                         reads=[("ktok", tt), ("vw", z)], writes=[("ps", pc)])
                    yield
                    g.op("dve", lambda e: e.tensor_scalar(out=Cst[rows, dr, 0:129], in0=Cst[rows, dr, 0:129],
                                                          scalar1=EBa[rows, tt, j:j + 1], scalar2=None, op0=ALU.mult),
                         reads=[("Cst", dr, hl), "EBa"], writes=[("Cst", dr, hl)])
                    acol = Aall[:, tt, j:j + 1]
                    g.op("act", lambda e: e.activation(out=dn[z][:, 0:1], in_=ps[pc][:, 384:385], func=AF.Abs),
                         reads=[("ps", pc)], writes=[("dn", z)])
                    yield
                    g.op("dve", lambda e: e.scalar_tensor_tensor(out=Cst[rows, dr, 0:129], in0=ps[pc][rows, 0:129],
                                                                 scalar=EBH[rows, tt, j:j + 1], in1=Cst[rows, dr, 0:129],
                                                                 op0=ALU.mult, op1=ALU.add),
                         reads=[("ps", pc), ("Cst", dr, hl), "EBH"], writes=[("Cst", dr, hl)])
                    yield
                    if step < 15:
                        tn = tt + 1 if dr == 0 else tt - 1
                        g.op("act", lambda e: e.activation(out=Cbf[rows, dr, 0:129], in_=Cst[rows, dr, 0:129], func=AF.Identity,
                                                           scale=EBH[rows, tn, j:j + 1]),
                             reads=[("Cst", dr, hl), "EBH"], writes=[("Cbf", dr, hl)])
                    g.op("dve", lambda e: e.reciprocal(out=dn[z][:, 0:1], in_=dn[z][:, 0:1]), reads=[("dn", z)], writes=[("dn", z)])
                    yield
                    g.op("dve", lambda e: e.tensor_tensor(out=dn[z][:, 1:2], in0=dn[z][:, 0:1], in1=acol, op=ALU.min),
                         reads=[("dn", z), "Aall"], writes=[("dn", z)])
                    yield
                    is_first = (dr == 0 and tt <= 15 - tt) or (dr == 1 and (15 - tt) < tt)
                    if is_first:
                        g.op("act", lambda e: e.activation(out=hacc[:, tt, hl, :], in_=ps[pc][:, 256:384], func=AF.Identity,
                                                           scale=dn[z][:, 1:2]),
                             reads=[("ps", pc), ("dn", z)], writes=[("hacc", tt, hl)])
                    else:
                        g.op("dve", lambda e: e.scalar_tensor_tensor(out=hacc[:, tt, hl, :], in0=ps[pc][:, 256:384], scalar=dn[z][:, 1:2],
                                                                     in1=hacc[:, tt, hl, :], op0=ALU.mult, op1=ALU.add),
                             reads=[("ps", pc), ("dn", z), ("hacc", tt, hl)], writes=[("hacc", tt, hl)])
                    yield

                def round_robin(gens):
                    gens = list(gens)
                    while gens:
                        alive = []
                        for ge in gens:
                            try:
                                next(ge)
                                alive.append(ge)
                            except StopIteration:
                                pass
                        gens = alive

                chains = [(hl_, dr_) for hl_ in range(2) for dr_ in range(2)]
                round_robin([chain_a(0, hl_, dr_) for hl_, dr_ in chains])
                for step in range(16):
                    if step < 15:
                        round_robin([chain_a(step + 1, hl_, dr_) for hl_, dr_ in chains])
                    round_robin([chain_b(step, hl_, dr_) for hl_, dr_ in chains])
                s3 = wload([win_v[:, :, 1024 + mp * 256:1024 + (mp + 1) * 256]],
                           [lambda sl: sl[:, 0:NCH * 256].rearrange("p (c n) -> p c n", c=NCH)])
                w3 = ring[:, s3, 0:NCH * 256].rearrange("p (c n) -> p c n", c=NCH)
                for tt in range(16):
                    u = tt % 2
                    ts_ = slice(tt * 128, (tt + 1) * 128)
                    pp = 5 + (tt % 2)
                    for c in range(NCH):
                        g.op("pe", lambda e, c=c, ts_=ts_, w3=w3, pp=pp: e.matmul(
                            ps[pp][:, 0:256], lhsT=h2T[:, c, ts_], rhs=w3[:, c, :], start=(c == 0), stop=(c == NCH - 1)),
                            reads=[("ring", s3), ("h2T", c, tt // 4)], writes=[("ps", pp)])
                    g.op("act", lambda e, u=u, pp=pp: e.activation(out=etm[u], in_=ps[pp][:, 0:256], func=AF.Exp, scale=-1.0),
                         reads=[("ps", pp)], writes=[("etm", u)])
                    g.op("dve", lambda e, u=u: e.tensor_scalar(out=etm[u], in0=etm[u], scalar1=1.0, scalar2=None, op0=ALU.add),
                         reads=[("etm", u)], writes=[("etm", u)])
                    g.op("dve", lambda e, u=u: e.reciprocal(out=etm[u], in_=etm[u]),
                         reads=[("etm", u)], writes=[("etm", u)])
                    hk = [("hacc", tt, 0), ("hacc", tt, 1)]
                    hv = hacc[:, tt, :, :]
                    h3 = hsq[u].rearrange("p (h n) -> p h n", h=2)
                    y3 = hy[u].rearrange("p (h n) -> p h n", h=2)
                    g.op("dve", lambda e, hv=hv, h3=h3: e.tensor_tensor(out=h3, in0=hv, in1=hv, op=ALU.mult),
                         reads=hk, writes=["hsq"])
                    g.op("dve", lambda e, u=u, h3=h3: e.tensor_reduce(out=hst[u], in_=h3, axis=AX.X, op=ALU.add),
                         reads=["hsq"], writes=[("hst", u)])
                    g.op("act", lambda e, u=u: e.activation(out=hst[u], in_=hst[u], func=AF.Ln, bias=EPS, scale=1.0 / 128),
                         reads=[("hst", u)], writes=[("hst", u)])
                    g.op("act", lambda e, u=u: e.activation(out=hst[u], in_=hst[u], func=AF.Exp, scale=-0.5),
                         reads=[("hst", u)], writes=[("hst", u)])
                    for hl in range(2):
                        head = mp * 2 + hl
                        g.op("dve", lambda e, u=u, hl=hl, head=head, hv=hv, y3=y3: e.scalar_tensor_tensor(
                            out=y3[:, hl, :], in0=hv[:, hl, :], scalar=hst[u][:, hl:hl + 1], in1=gmh[:, head * 128:(head + 1) * 128],
                            op0=ALU.mult, op1=ALU.mult), reads=hk + [("hst", u), "gmh"], writes=[("hy", u, hl)])
                    g.op("dve", lambda e, u=u, tt=tt: e.tensor_tensor(out=hyb[u], in0=hy[u], in1=etm[u], op=ALU.mult),
                         reads=[("hy", u, 0), ("hy", u, 1), ("etm", u)], writes=[("hyb", u)])
                    def fin_tr(tt, mp=mp):
                        u = tt % 2
                        ts_ = slice(tt * 128, (tt + 1) * 128)
                        for hl in range(2):
                            g.op("pe", lambda e, hl=hl: e.transpose(psT[:, 256 + hl * 128:256 + (hl + 1) * 128],
                                                                    hyb[u][:, hl * 128:(hl + 1) * 128], identb[:, :]),
                                 reads=[("hyb", u), "identb"], writes=[PST])
                        g.op("act", lambda e: e.copy(
                            out=mixh[:, mp * 2:mp * 2 + 2, ts_], in_=psT[:, 256:512].rearrange("p (h n) -> p h n", h=2)),
                            reads=[PST], writes=[("mixh", mp * 2, tt // 4), ("mixh", mp * 2 + 1, tt // 4)])
                    if tt >= 1:
                        fin_tr(tt - 1)
                    if tt == 15:
                        fin_tr(15)
            dbg("mixm", mixh, [128, 4, S], BF16, [("mixh", k_, q_) for k_ in range(4) for q_ in range(4)], b)
            dbg("G", G, [128, 16, 16], F32, [("G", t_) for t_ in range(16)], b)
            dbg("LF", LF, [128, 16, 8], F32, ["LF"], b)
            dbg("sB", sB, [128, 16, 16], F32, ["sB"], b)
            dbg("Call", Call, [128, 16, 8], F32, ["Call"], b)
            dbg("Aall", Aall, [128, 16, 8], F32, ["Aall"], b)
            dbg("hacc", hacc, [128, 16, 2, 128], F32, [("hacc", t_, h_) for t_ in range(16) for h_ in range(2)], b)
            wout_half(0)
            ar.release(m0)
            g.barrier()

        def final(b, raw=False):
            m0 = ar.mark()
            outs = []
            if raw:
                for c in range(NCH):
                    outs.append(g.dma("sp", out_d[b, c * 128:(c + 1) * 128, :], xT[:, c, :],
                                      reads=[("xT", c, blk) for blk in range(4)]))
                return outs
            rstd = ar.alloc(S, F32)
            sq = ar.alloc(NCH * 512, BF16).rearrange("p (c n) -> p c n", c=NCH)
            ob = [ar.alloc(512, F32) for _ in range(4)]
            rms_rstd(b, rstd, sq)
            n = 0
            for blk in range(4):
                tok = slice(blk * 512, (blk + 1) * 512)
                for c in range(NCH):
                    o_ = ob[n % 4]
                    g.op("dve", lambda e, c=c, tok=tok, o_=o_: e.scalar_tensor_tensor(
                        out=o_, in0=xT[:, c, tok], scalar=g4[:, 3, c:c + 1], in1=rstd[:, tok], op0=ALU.mult, op1=ALU.mult),
                        reads=[("xT", c, blk), ("rstd", blk), "g4"], writes=[("ob", n % 4)])
                    outs.append(g.dma("sp", out_d[b, c * 128:(c + 1) * 128, tok], o_, reads=[("ob", n % 4)]))
                    n += 1
            ar.release(m0)
            g.barrier()
            return outs

        outs = []
        adaln(list(range(0, 6)))
        derive(0)
        for b in range(2):
            load_x(b)
            ffn(b, 0, 0)
            if b == 0:
                adaln(list(range(6, 18)))
                derive(1)
                derive(2)
            if stage >= 2:
                mixer(b)
            if stage >= 3:
                ffn(b, 2, 1)
            outs += final(b, raw=(stage < 3))
        g.emit(final_wait_ops=outs + dbg_outs)
    return nc


def _consts():
    p = np.arange(128)[:, None]
    j = np.arange(MASK_W)[None, :]
    dlt = p - j + MASK_C
    a = np.abs(dlt)
    m = (a <= 64).astype(np.float32) + ((dlt % 4 == 0) & (a <= 256)) + ((dlt % 16 == 0) & (a <= 1024))
    maskT = m.astype(ml_dtypes.bfloat16)
    identb = np.eye(128, dtype=np.float32).astype(ml_dtypes.bfloat16)
    u = np.arange(128)[:, None]
    t = np.arange(128)[None, :]
    trif = (u <= t).astype(np.float32)
    trib = (u >= t).astype(np.float32)
    maskf = (u <= t).astype(np.float32).astype(ml_dtypes.bfloat16)
    maskb = (u >= t).astype(np.float32).astype(ml_dtypes.bfloat16)
    half = 8
    inv_freq = (500000.0 ** (-2.0 * np.arange(half, dtype=np.float32) / 16.0)).astype(np.float32)
    pos = np.arange(S, dtype=np.float32)
    ang = (pos[:, None] * inv_freq[None, :]).astype(np.float32)
    cos = np.cos(ang).astype(np.float32).reshape(16, 128, 8).transpose(1, 0, 2)
    sin = np.sin(ang).astype(np.float32).reshape(16, 128, 8).transpose(1, 0, 2)
    cos4 = np.ascontiguousarray(np.broadcast_to(cos[:, :, None, :], (128, 16, 4, 8))).astype(np.float32)
    sin4 = np.ascontiguousarray(np.broadcast_to(sin[:, :, None, :], (128, 16, 4, 8))).astype(np.float32)
    return dict(maskT=maskT, identb=identb, trif=trif, trib=trib, maskf=maskf, maskb=maskb, cos4=cos4, sin4=sin4)


def _cols(v):
    return np.ascontiguousarray(np.asarray(v, np.float32).reshape(-1, 128).T)


_NC_CACHE = {}


def kernel(x, c, w_ada, b_ada, g_ffn1, w_gu1, w_down1, g_mix, w_in, gate_bias, g_q, g_k, g_mh,
           w_out, g_ffn2, w_gu2, w_down2, g_final):
    stage = int(os.environ.get("MK_STAGE", "3"))
    x = np.asarray(x, np.float32)
    c = np.asarray(c, np.float32)
    if stage not in _NC_CACHE:
        _NC_CACHE[stage] = build_program(stage)
    nc = _NC_CACHE[stage]
    consts = _consts()
    shared = dict(
        w_ada=np.ascontiguousarray(np.asarray(w_ada, np.float32)[0]),
        b_adaT=_cols(np.asarray(b_ada)[0]),
        g4=np.ascontiguousarray(np.stack([_cols(np.asarray(v)[0]) for v in (g_ffn1, g_mix, g_ffn2, g_final)], axis=1)),
        w_gu1=np.ascontiguousarray(np.asarray(w_gu1, np.float32)[0]),
        w_gu2=np.ascontiguousarray(np.asarray(w_gu2, np.float32)[0]),
        w_down1=np.ascontiguousarray(np.asarray(w_down1, np.float32)[0]),
        w_down2=np.ascontiguousarray(np.asarray(w_down2, np.float32)[0]),
        w_in=np.ascontiguousarray(np.asarray(w_in, np.float32)[0]),
        w_out=np.ascontiguousarray(np.asarray(w_out, np.float32)[0]),
        gbias_rep=np.ascontiguousarray(np.broadcast_to(np.asarray(gate_bias, np.float32)[0].reshape(1, 16), (128, 16))),
        gqk_rep=np.ascontiguousarray(np.broadcast_to(
            np.stack([np.asarray(g_q, np.float32)[0]] * 2 + [np.asarray(g_k, np.float32)[0]] * 2)[None], (128, 4, 64))),
        gmh_rep=np.ascontiguousarray(np.broadcast_to(np.asarray(g_mh, np.float32)[0][None, :], (128, 512))),
        **consts,
    )
    in_maps = []
    for i in range(8):
        m = dict(shared)
        m["xT"] = np.ascontiguousarray(x[2 * i:2 * i + 2].transpose(0, 2, 1))
        m["cT"] = np.ascontiguousarray(c[2 * i:2 * i + 2].reshape(2, NCH, 128).transpose(2, 1, 0))
        in_maps.append(m)
    res = run_bass_kernel_spmd(nc, in_maps, core_ids=list(range(8)))
    out = np.empty((16, S, D), np.float32)
    for i in range(8):
        out[2 * i:2 * i + 2] = res.results[i]["outT"].transpose(0, 2, 1)
    return out
```

```python
import os
import contextlib
import numpy as np
import ml_dtypes
import concourse.bass as bass
import concourse.mybir as mybir
from concourse.bass_utils import run_bass_kernel_spmd

F32 = mybir.dt.float32
BF16 = mybir.dt.bfloat16
AF = mybir.ActivationFunctionType
ALU = mybir.AluOpType
AX = mybir.AxisListType

ENG_NAMES = ("pe", "act", "dve", "pool", "sp")
D = 1024
S = 2048
DFF = 2816
NCH = 8
NF = 22
EPS = 1e-6
MASK_C = 1408
MASK_W = 2944
NSLOT = 3
SLOT_ELEMS = 4096


class Op:
    __slots__ = ("eng", "fn", "deps", "is_dma", "sem", "val", "has_dep", "idx", "name", "prewait")

    def __init__(self, eng, fn, name=""):
        self.eng = eng
        self.fn = fn
        self.deps = []
        self.is_dma = False
        self.sem = None
        self.val = 0
        self.has_dep = False
        self.idx = 0
        self.name = name
        self.prewait = None


class Graph:
    def __init__(self, nc, n_dma_sems=16):
        self.nc = nc
        self.ops = {e: [] for e in ENG_NAMES}
        self.last_writer = {}
        self.readers = {}
        self.n_dma_sems = n_dma_sems
        self.dma_count = {e: 0 for e in ENG_NAMES}
        self.dma_ops = {e: [] for e in ENG_NAMES}
        self.barrier_deps = {}
        self.sp_since_barrier = []

    def _link(self, op, deps):
        latest = {}
        keep = []
        for d in deps:
            if d is op:
                continue
            if d.is_dma:
                keep.append(d)
                continue
            if d.eng == "pe" and op.eng == "pe":
                continue
            cur = latest.get(d.eng)
            if cur is None or d.idx > cur.idx:
                latest[d.eng] = d
        keep.extend(latest.values())
        seen = set(id(d) for d in op.deps)
        for d in keep:
            if id(d) in seen:
                continue
            seen.add(id(d))
            op.deps.append(d)
            d.has_dep = True

    def _add_deps(self, op, reads, writes):
        deps = []
        for r in reads:
            w = self.last_writer.get(r)
            if w is not None:
                deps.append(w)
            if (isinstance(r, tuple) and r[0] == "ps") or (isinstance(r, str) and r.startswith("psT")):
                deps.extend(x for x in self.readers.get(r, ()) if x.eng != op.eng)
        for w_ in writes:
            w = self.last_writer.get(w_)
            if w is not None:
                deps.append(w)
            deps.extend(self.readers.get(w_, ()))
        b = self.barrier_deps.pop(op.eng, None)
        if b:
            deps.extend(b)
        self._link(op, deps)
        for r in reads:
            self.readers.setdefault(r, []).append(op)
        for w_ in writes:
            self.last_writer[w_] = op
            self.readers[w_] = []

    def op(self, eng, fn, reads=(), writes=(), name=""):
        o = Op(eng, fn, name)
        o.idx = len(self.ops[eng])
        self._add_deps(o, reads, writes)
        self.ops[eng].append(o)
        return o

    def dma(self, eng, out, in_, reads=(), writes=(), name=""):
        def fn(e, out=out, in_=in_):
            return e.dma_start(out=out, in_=in_)
        o = Op(eng, fn, name)
        o.is_dma = True
        i = self.dma_count[eng]
        self.dma_count[eng] += 1
        o.idx = i
        o.val = 16 * (i // self.n_dma_sems + 1)
        if i >= self.n_dma_sems:
            o.prewait = self.dma_ops[eng][i - self.n_dma_sems]
        self.dma_ops[eng].append(o)
        self._add_deps(o, reads, writes)
        self.ops[eng].append(o)
        if eng == "sp":
            self.sp_since_barrier.append(o)
        return o

    def barrier(self):
        b = []
        for e in ("pe", "act", "dve"):
            for o in reversed(self.ops[e]):
                b.append(o)
                break
        b.extend(self.sp_since_barrier)
        self.sp_since_barrier = []
        for e in ("pe", "act", "dve", "sp"):
            self.barrier_deps[e] = list(b) + list(self.barrier_deps.get(e, ()))

    def emit(self, final_wait_ops=()):
        nc = self.nc
        with contextlib.ExitStack() as st:
            esem = {e: st.enter_context(nc.semaphore("s_" + e)) for e in ENG_NAMES}
            dsem = {}
            for e in ENG_NAMES:
                if self.dma_count[e]:
                    dsem[e] = [st.enter_context(nc.semaphore("d_%s_%d" % (e, i)))
                               for i in range(min(self.n_dma_sems, self.dma_count[e]))]
            for e in ENG_NAMES:
                m = 0
                for o in self.ops[e]:
                    if o.is_dma:
                        o.sem = dsem[e][o.idx % self.n_dma_sems]
                    elif o.has_dep:
                        m += 1
                        o.sem = esem[e]
                        o.val = m
            block = st.enter_context(nc.Block())
            handles = {"pe": block.tensor, "act": block.scalar, "dve": block.vector,
                       "pool": block.gpsimd, "sp": block.sync}

            def make(e):
                ops = self.ops[e]

                def body(eng):
                    waited = {}

                    def wait(d):
                        key = id(d.sem)
                        if waited.get(key, 0) >= d.val:
                            return
                        waited[key] = d.val
                        eng.wait_ge(d.sem, d.val)
                    for o in ops:
                        if o.prewait is not None:
                            wait(o.prewait)
                        for d in o.deps:
                            wait(d)
                        inst = o.fn(eng)
                        if o.is_dma:
                            inst.then_inc(o.sem, 16)
                        elif o.has_dep:
                            inst.then_inc(o.sem, 1)
                    if e == "sp":
                        for d in final_wait_ops:
                            wait(d)
                return body
            for e in ENG_NAMES:
                if self.ops[e] or (e == "sp" and final_wait_ops):
                    handles[e](make(e))


class Arena:
    def __init__(self, ap2d, nelem_bf16):
        self.ap = ap2d
        self.cap = nelem_bf16
        self.off = 0

    def mark(self):
        return self.off

    def release(self, m):
        self.off = m

    def alloc(self, nelem, dt):
        n16 = nelem * (2 if dt == F32 else 1)
        self.off = (self.off + 1) // 2 * 2
        assert self.off + n16 <= self.cap, ("arena overflow", self.off, n16, self.cap)
        a = self.ap[:, self.off:self.off + n16]
        self.off += n16
        if dt == F32:
            a = a.bitcast(F32)
        return a


def build_program(stage=3):
    nc = bass.Bass("TRN2", target_bir_lowering=False)

    def din(name, shape, dt=F32):
        return nc.dram_tensor(name, list(shape), dt, kind="ExternalInput").ap()
    xT_d = din("xT", [2, D, S])
    cT_d = din("cT", [128, NCH, 2])
    wada_d = din("w_ada", [D, 9 * D])
    bada_d = din("b_adaT", [128, 72])
    g4_d = din("g4", [128, 4, NCH])
    wgu_d = [din("w_gu1", [D, 2 * DFF]), din("w_gu2", [D, 2 * DFF])]
    wdn_d = [din("w_down1", [DFF, D]), din("w_down2", [DFF, D])]
    win_d = din("w_in", [D, 3088])
    wout_d = din("w_out", [D, D])
    gbias_d = din("gbias_rep", [128, 16])
    gqk_d = din("gqk_rep", [128, 4, 64])
    gmh_d = din("gmh_rep", [128, 512])
    mask_d = din("maskT", [128, MASK_W], BF16)
    identb_d = din("identb", [128, 128], BF16)
    trif_d = din("trif", [128, 128])
    trib_d = din("trib", [128, 128])
    mkf_d = din("maskf", [128, 128], BF16)
    mkb_d = din("maskb", [128, 128], BF16)
    cos_d = din("cos4", [128, 16, 4, 8])
    sin_d = din("sin4", [128, 16, 4, 8])
    out_d = nc.dram_tensor("outT", [2, D, S], F32, kind="ExternalOutput").ap()

    st = contextlib.ExitStack()
    with st:
        def sbt(name, shape, dt):
            return st.enter_context(nc.sbuf_tensor(name, shape, dt))
        xT = sbt("xT_sb", [128, NCH, S], F32)
        ring = sbt("ring", [128, NSLOT, SLOT_ELEMS], BF16)
        maskT = sbt("maskT_sb", [128, MASK_W], BF16)
        identb = sbt("identb_sb", [128, 128], BF16)
        onesb = sbt("onesb", [128, 128], BF16)
        onesf = sbt("onesf", [128, 128], F32)
        trif = sbt("trif_sb", [128, 128], F32)
        trib = sbt("trib_sb", [128, 128], F32)
        mkf = sbt("mkf_sb", [128, 128], BF16)
        mkb = sbt("mkb_sb", [128, 128], BF16)
        cos4 = sbt("cos4_sb", [128, 16, 4, 8], F32)
        sin4 = sbt("sin4_sb", [128, 16, 4, 8], F32)
        gqk = sbt("gqk_sb", [128, 4, 64], F32)
        gmh = sbt("gmh_sb", [128, 512], F32)
        gbias = sbt("gbias_sb", [128, 16], F32)
        g4 = sbt("g4_sb", [128, 4, NCH], F32)
        badaT = sbt("bada_sb", [128, 72], F32)
        cT = sbt("cT_sb", [128, NCH, 2], F32)
        csT = sbt("csT_sb", [128, NCH, 2], BF16)
        modT = sbt("modT", [128, 72, 2], F32)
        geff = sbt("geff", [128, 2, 3, NCH], F32)
        gate = sbt("gate", [128, 2, 3, NCH], F32)
        ARENA_N = 52736
        arena_t = sbt("arena", [128, ARENA_N], BF16)
        ar = Arena(arena_t[:, :], ARENA_N)
        ps = [st.enter_context(nc.psum_tensor("ps%d" % i, [128, 512], F32)) for i in range(8)]
        psT = ps[7][:, :].bitcast(BF16)
        PST = ("ps", 7)

        g = Graph(nc)
        ring_ctr = [0]
        DBG = os.environ.get("MK_DEBUG") == "1"
        dbg_outs = []

        def dbg(name, ap, shape, dt, reads, b=0):
            if not DBG or b != 0:
                return
            t = nc.dram_tensor("dbg_" + name, list(shape), dt, kind="ExternalOutput").ap()
            dbg_outs.append(g.dma("sp", t, ap, reads=reads))

        def wload(src_aps, views, name=""):
            s = ring_ctr[0] % NSLOT
            ring_ctr[0] += 1
            for src, vw in zip(src_aps, views):
                g.dma("pool", vw(ring[:, s, :]), src, writes=[("ring", s)], name=name)
            return s

        for dst, src, key in ((maskT[:, :], mask_d, "maskT"), (identb[:, :], identb_d, "identb"),
                              (trif[:, :], trif_d, "trif"), (trib[:, :], trib_d, "trib"),
                              (mkf[:, :], mkf_d, "mkf"), (mkb[:, :], mkb_d, "mkb"),
                              (cos4[:], cos_d, "cos4"), (sin4[:], sin_d, "sin4"),
                              (gqk[:], gqk_d, "gqk"), (gmh[:, :], gmh_d, "gmh"), (gbias[:, :], gbias_d, "gbias"),
                              (g4[:], g4_d, "g4"), (badaT[:, :], bada_d, "badaT"), (cT[:], cT_d, "cT")):
            g.dma("sp", dst, src, writes=[key])
        g.op("dve", lambda e: e.memset(onesb[:, :], 1.0), writes=["onesb"])
        g.op("dve", lambda e: e.memset(onesf[:, :], 1.0), writes=["onesf"])
        g.op("dve", lambda e: e.tensor_scalar(out=gqk[:, 0:2, :], in0=gqk[:, 0:2, :], scalar1=0.125, scalar2=None,
                                              op0=ALU.mult), reads=["gqk"], writes=["gqk"])

        def load_x(b):
            for c in range(NCH):
                g.dma("sp", xT[:, c, :], xT_d[b, c * 128:(c + 1) * 128, :],
                      writes=[("xT", c, blk) for blk in range(4)])

        g.op("act", lambda e: e.activation(out=csT[:], in_=cT[:], func=AF.Silu), reads=["cT"], writes=["csT"])
        wada_v = wada_d.rearrange("(c p) n -> p c n", p=128)

        def adaln(groups):
            for grp in groups:
                s = wload([wada_v[:, :, grp * 512:(grp + 1) * 512]],
                          [lambda sl: sl.rearrange("p (c n) -> p c n", c=NCH)])
                wv = ring[:, s, :].rearrange("p (c n) -> p c n", c=NCH)
                for j in range(4):
                    n = grp * 4 + j
                    for k in range(NCH):
                        g.op("pe", lambda e, n=n, k=k, j=j, wv=wv: e.matmul(
                            ps[6][:, 2 * n:2 * n + 2], lhsT=wv[:, k, j * 128:(j + 1) * 128], rhs=csT[:, k, :],
                            start=(k == 0), stop=(k == NCH - 1), skip_group_check=True),
                            reads=[("ring", s), "csT"], writes=[("ps", 6)])
            n0, n1 = groups[0] * 4, groups[-1] * 4 + 4
            part = 0 if n0 == 0 else 1
            for b in range(2):
                g.op("dve", lambda e, b=b: e.tensor_tensor(
                    out=modT[:, n0:n1, b], in0=ps[6][:, 2 * n0 + b:2 * n1 + b:2], in1=badaT[:, n0:n1], op=ALU.add),
                    reads=[("ps", 6), "badaT"], writes=[("modT", part, b)])

        def derive(i):
            n0 = 3 * i * 8
            part = 0 if i == 0 else 1
            for b in range(2):
                g.op("dve", lambda e, b=b: e.scalar_tensor_tensor(
                    out=geff[:, b, i, :], in0=modT[:, n0 + 8:n0 + 16, b], scalar=1.0, in1=g4[:, i, :],
                    op0=ALU.add, op1=ALU.mult), reads=[("modT", part, b), "g4"], writes=[("geff", b, i)])
                g.op("dve", lambda e, b=b: e.tensor_scalar(
                    out=gate[:, b, i, :], in0=modT[:, n0 + 16:n0 + 24, b], scalar1=(1.0 if i == 1 else 0.5),
                    scalar2=None, op0=ALU.mult), reads=[("modT", part, b)], writes=[("gate", b, i)])

        def shift_col(b, i, c):
            return modT[:, 3 * i * 8 + c, b:b + 1]

        def rms_rstd(b, rstd, sq):
            for blk in range(4):
                tok = slice(blk * 512, (blk + 1) * 512)
                for c in range(NCH):
                    g.op("act", lambda e, c=c, tok=tok: e.activation(out=sq[:, c, :], in_=xT[:, c, tok], func=AF.Square),
                         reads=[("xT", c, blk)], writes=[("sq", c)])
                for c in range(NCH):
                    g.op("pe", lambda e, c=c: e.matmul(ps[6][:, :], lhsT=onesb[:, :], rhs=sq[:, c, :],
                                                      start=(c == 0), stop=(c == NCH - 1)),
                         reads=["onesb", ("sq", c)], writes=[("ps", 6)])
                g.op("act", lambda e, tok=tok: e.activation(out=rstd[:, tok], in_=ps[6][:, :], func=AF.Ln,
                                                            bias=EPS, scale=1.0 / D),
                     reads=[("ps", 6)], writes=[("rstd", blk)])
            for blk in range(4):
                tok = slice(blk * 512, (blk + 1) * 512)
                g.op("act", lambda e, tok=tok: e.activation(out=rstd[:, tok], in_=rstd[:, tok], func=AF.Exp, scale=-0.5),
                     reads=[("rstd", blk)], writes=[("rstd", blk)])

        def norm_mod(b, i, rstd, tmp, dst_fn, blk, keyfn):
            tok = slice(blk * 512, (blk + 1) * 512)
            for c in range(NCH):
                tb = tmp[c % 2]
                g.op("dve", lambda e, c=c, tb=tb: e.tensor_tensor(out=tb, in0=xT[:, c, tok], in1=rstd[:, tok], op=ALU.mult),
                     reads=[("xT", c, blk), ("rstd", blk)], writes=[("tmp", c % 2)])
                g.op("act", lambda e, c=c, tb=tb: e.activation(out=dst_fn(c), in_=tb, func=AF.Identity,
                                                               bias=shift_col(b, i, c), scale=geff[:, b, i, c:c + 1]),
                     reads=[("tmp", c % 2), ("geff", b, i), ("modT", 0 if i == 0 else 1, b)], writes=[keyfn(c)])

        def ffn(b, i, which):
            wgu_v = wgu_d[which].rearrange("(c p) n -> p c n", p=128)
            wdn_v = wdn_d[which].rearrange("(k p) n -> p k n", p=128)
            m0 = ar.mark()
            rstd = ar.alloc(S, F32)
            sq = ar.alloc(NCH * 512, BF16).rearrange("p (c n) -> p c n", c=NCH)
            tmp = [ar.alloc(512, F32) for _ in range(2)]
            hT = ar.alloc(NCH * 1024, BF16).rearrange("p (c n) -> p c n", c=NCH)
            actT = ar.alloc(NF * 1024, BF16).rearrange("p (c n) -> p c n", c=NF)
            sg = [ar.alloc(512, BF16) for _ in range(2)]
            rms_rstd(b, rstd, sq)
            cnt = 0
            for sb in range(2):
                for lb in range(2):
                    blk = sb * 2 + lb
                    norm_mod(b, i, rstd, tmp, lambda c, lb=lb: hT[:, c, lb * 512:(lb + 1) * 512], blk,
                             lambda c, lb=lb: ("hT", c, lb))
                for grp in range(11):
                    c0 = grp * 256
                    sgw = wload([wgu_v[:, :, c0:c0 + 256], wgu_v[:, :, DFF + c0:DFF + c0 + 256]],
                                [lambda sl, q=q: sl.rearrange("p (c n) -> p c n", c=NCH)[:, :, q * 256:(q + 1) * 256]
                                 for q in range(2)])
                    wgv = ring[:, sgw, :].rearrange("p (c n) -> p c n", c=NCH)
                    for j in range(2):
                        f = grp * 2 + j
                        for lb in range(2):
                            pg = cnt % 2
                            cnt += 1
                            hs = slice(lb * 512, (lb + 1) * 512)
                            for k in range(NCH):
                                g.op("pe", lambda e, k=k, j=j, wgv=wgv, pg=pg, hs=hs: e.matmul(
                                    ps[pg][:, :], lhsT=wgv[:, k, j * 128:(j + 1) * 128], rhs=hT[:, k, hs],
                                    start=(k == 0), stop=(k == NCH - 1)),
                                    reads=[("ring", sgw), ("hT", k, lb)], writes=[("ps", pg)])
                            for k in range(NCH):
                                g.op("pe", lambda e, k=k, j=j, wgv=wgv, pg=pg, hs=hs: e.matmul(
                                    ps[2 + pg][:, :], lhsT=wgv[:, k, 256 + j * 128:256 + (j + 1) * 128], rhs=hT[:, k, hs],
                                    start=(k == 0), stop=(k == NCH - 1)),
                                    reads=[("ring", sgw), ("hT", k, lb)], writes=[("ps", 2 + pg)])
                            g.op("act", lambda e, pg=pg: e.activation(out=sg[pg], in_=ps[pg][:, :], func=AF.Silu),
                                 reads=[("ps", pg)], writes=[("sg", pg)])
                            g.op("dve", lambda e, pg=pg, f=f, hs=hs: e.tensor_tensor(
                                out=actT[:, f, hs], in0=ps[2 + pg][:, :], in1=sg[pg], op=ALU.mult),
                                reads=[("ps", 2 + pg), ("sg", pg)], writes=[("actT", f, lb)])
                for dc in range(NCH):
                    sw = wload([wdn_v[:, :, dc * 128:(dc + 1) * 128]],
                               [lambda sl: sl[:, 0:NF * 128].rearrange("p (k n) -> p k n", k=NF)])
                    wd = ring[:, sw, 0:NF * 128].rearrange("p (k n) -> p k n", k=NF)
                    for lb in range(2):
                        blk = sb * 2 + lb
                        pd = 4 + (cnt % 2)
                        cnt += 1
                        hs = slice(lb * 512, (lb + 1) * 512)
                        for k in range(NF):
                            g.op("pe", lambda e, k=k, wd=wd, pd=pd, hs=hs: e.matmul(
                                ps[pd][:, :], lhsT=wd[:, k, :], rhs=actT[:, k, hs],
                                start=(k == 0), stop=(k == NF - 1)),
                                reads=[("ring", sw), ("actT", k, lb)], writes=[("ps", pd)])
                        tok = slice(blk * 512, (blk + 1) * 512)
                        g.op("dve", lambda e, pd=pd, dc=dc, tok=tok: e.scalar_tensor_tensor(
                            out=xT[:, dc, tok], in0=ps[pd][:, :], scalar=gate[:, b, i, dc:dc + 1], in1=xT[:, dc, tok],
                            op0=ALU.mult, op1=ALU.add),
                            reads=[("ps", pd), ("gate", b, i), ("xT", dc, blk)], writes=[("xT", dc, blk)])
            ar.release(m0)
            g.barrier()

        def mixer(b):
            i = 1
            win_v = win_d.rearrange("(c p) n -> p c n", p=128)
            wout_v = wout_d.rearrange("(k p) n -> p k n", p=128)
            m0 = ar.mark()
            h2T = ar.alloc(NCH * S, BF16).rearrange("p (c n) -> p c n", c=NCH)
            mixh = ar.alloc(4 * S, BF16).rearrange("p (c n) -> p c n", c=4)
            m1 = ar.mark()
            rstd = ar.alloc(S, F32)
            sq = ar.alloc(NCH * 512, BF16).rearrange("p (c n) -> p c n", c=NCH)
            tmp = [ar.alloc(512, F32) for _ in range(2)]
            rms_rstd(b, rstd, sq)
            for blk in range(4):
                norm_mod(b, i, rstd, tmp, lambda c, blk=blk: h2T[:, c, blk * 512:(blk + 1) * 512], blk,
                         lambda c, blk=blk: ("h2T", c, blk))
            dbg("h2T", h2T, [128, NCH, S], BF16, [("h2T", c, blk) for c in range(NCH) for blk in range(4)], b)
            ar.release(m1)
            g.barrier()

            def wout_half(half):
                cnt = 0
                for dcp in range(2):
                    sw = wload([wout_v[:, half * 4:half * 4 + 4, dcp * 512:(dcp + 1) * 512]],
                               [lambda sl: sl[:, 0:4 * 512].rearrange("p (k n) -> p k n", k=4)])
                    wo = ring[:, sw, 0:4 * 512].rearrange("p (k n) -> p k n", k=4)
                    for dj in range(4):
                        dc = dcp * 4 + dj
                        for blk in range(4):
                            pd = 4 + (cnt % 2)
                            cnt += 1
                            tok = slice(blk * 512, (blk + 1) * 512)
                            for k in range(4):
                                g.op("pe", lambda e, k=k, wo=wo, pd=pd, dj=dj, tok=tok: e.matmul(
                                    ps[pd][:, :], lhsT=wo[:, k, dj * 128:(dj + 1) * 128], rhs=mixh[:, k, tok],
                                    start=(k == 0), stop=(k == 3)),
                                    reads=[("ring", sw), ("mixh", k, blk)], writes=[("ps", pd)])
                            g.op("dve", lambda e, pd=pd, dc=dc, tok=tok: e.scalar_tensor_tensor(
                                out=xT[:, dc, tok], in0=ps[pd][:, :], scalar=gate[:, b, i, dc:dc + 1], in1=xT[:, dc, tok],
                                op0=ALU.mult, op1=ALU.add),
                                reads=[("ps", pd), ("gate", b, i), ("xT", dc, blk)], writes=[("xT", dc, blk)])

            m2 = ar.mark()
            aqT = [ar.alloc(S, BF16) for _ in range(2)]
            akT = [ar.alloc(S, BF16) for _ in range(2)]
            av2 = [ar.alloc(16 * 2 * 128, BF16).rearrange("p (t h n) -> p t h n", t=16, h=2) for _ in range(2)]
            ytm4 = ar.alloc(1024, F32)
            ysq4 = ar.alloc(1024, F32)
            yb4 = ar.alloc(1024, BF16)
            st16 = ar.alloc(16, F32)
            rp4 = ar.alloc(4 * 128, F32).rearrange("p (k a n) -> p k a n", k=4, a=16)
            Eb = [ar.alloc(512, BF16) for _ in range(3)]
            Pm = [ar.alloc(512, BF16) for _ in range(3)]
            rd = [ar.alloc(512, F32) for _ in range(2)]
            SBANK = [0, 1, 2]
            for par in range(2):
                g.op("dve", lambda e, par=par: e.memset(av2[par][:, :, :, 64:128], 1.0), writes=[("av2ones", par)])
            y16 = ytm4.rearrange("p (a n) -> p a n", a=16)
            s16 = ysq4.rearrange("p (a n) -> p a n", a=16)
            yb16 = yb4.rearrange("p (a n) -> p a n", a=16)
            y44 = ytm4.rearrange("p (t a n) -> p t a n", t=4, a=4)
            st_b = st16.rearrange("p (a o) -> p a o", o=1).to_broadcast([128, 16, 64])
            gqk_b = gqk[:].rearrange("p (o a) n -> p o a n", o=1).to_broadcast([128, 4, 4, 64])

            def load_wa(hp):
                sw = wload([win_v[:, :, 1552 + hp * 128:1552 + hp * 128 + 128],
                            win_v[:, :, 2064 + hp * 128:2064 + hp * 128 + 128],
                            win_v[:, :, 2576 + hp * 128:2576 + hp * 128 + 128]],
                           [lambda sl, q=q: sl[:, 0:NCH * 384].rearrange("p (c n) -> p c n", c=NCH)[:, :, q * 128:(q + 1) * 128]
                            for q in range(3)])
                return ring[:, sw, 0:NCH * 384].rearrange("p (c n) -> p c n", c=NCH), sw

            def proj_p1(hp, tg, wa, sw):
                par = hp % 2
                for tl in range(4):
                    tt = tg * 4 + tl
                    ts_ = slice(tt * 128, (tt + 1) * 128)
                    bank = 5 + tl // 2
                    co = (tl % 2) * 256
                    for c in range(NCH):
                        g.op("pe", lambda e, c=c, bank=bank, co=co, ts_=ts_: e.matmul(ps[bank][:, co:co + 256], lhsT=h2T[:, c, ts_], rhs=wa[:, c, 0:256],
                                                          start=(c == 0), stop=(c == NCH - 1), skip_group_check=True),
                             reads=[("ring", sw), ("h2T", c, tg)], writes=[("ps", bank)])
                    for c in range(NCH):
                        g.op("pe", lambda e, c=c, tl=tl, ts_=ts_: e.matmul(ps[7][:, tl * 128:(tl + 1) * 128], lhsT=h2T[:, c, ts_],
                                                          rhs=wa[:, c, 256:384], start=(c == 0), stop=(c == NCH - 1),
                                                          skip_group_check=True),
                             reads=[("ring", sw), ("h2T", c, tg)], writes=[("ps", 7)])
                g.op("act", lambda e: e.copy(out=ytm4[:, 0:512], in_=ps[5][:, :]), reads=[("ps", 5)], writes=["ytm4a"])
                g.op("act", lambda e: e.copy(out=ytm4[:, 512:1024], in_=ps[6][:, :]), reads=[("ps", 6)], writes=["ytm4b"])
                g.op("act", lambda e: e.copy(out=av2[par][:, tg * 4:(tg + 1) * 4, :, 0:64],
                                             in_=ps[7][:, :].rearrange("p (t h n) -> p t h n", t=4, h=2)),
                     reads=[("ps", 7)], writes=[("av2", par, tg)])
                yk = ["ytm4a", "ytm4b"]
                g.op("pool", lambda e: e.tensor_tensor(out=ysq4, in0=ytm4, in1=ytm4, op=ALU.mult), reads=yk, writes=["ysq4"])
                yield
                g.op("dve", lambda e: e.tensor_reduce(out=st16, in_=s16, axis=AX.X, op=ALU.add), reads=["ysq4"], writes=["st16"])
                yield
                g.op("act", lambda e: e.activation(out=st16, in_=st16, func=AF.Ln, bias=EPS, scale=1.0 / 64),
                     reads=["st16"], writes=["st16"])
                g.op("act", lambda e: e.activation(out=st16, in_=st16, func=AF.Exp, scale=-0.5), reads=["st16"], writes=["st16"])
                g.op("pool", lambda e: e.tensor_tensor(out=y16, in0=y16, in1=st_b, op=ALU.mult), reads=yk + ["st16"], writes=yk)
                g.op("pool", lambda e: e.tensor_tensor(out=y44, in0=y44, in1=gqk_b, op=ALU.mult), reads=yk + ["gqk"], writes=yk)
                yield
                yield
                g.op("act", lambda e: e.copy(out=yb4, in_=ytm4), reads=yk, writes=["yb4"])
                t1 = y16[:, :, 0:8]
                t2 = y16[:, :, 8:16]
                cs_ = cos4[:, tg * 4:(tg + 1) * 4, :, :].rearrange("p t a n -> p (t a) n")
                sn_ = sin4[:, tg * 4:(tg + 1) * 4, :, :].rearrange("p t a n -> p (t a) n")
                g.op("pool", lambda e: e.tensor_tensor(out=rp4[:, 0], in0=t1, in1=cs_, op=ALU.mult), reads=yk + ["cos4"], writes=[("rp4", 0)])
                g.op("pool", lambda e: e.tensor_tensor(out=rp4[:, 1], in0=t2, in1=sn_, op=ALU.mult), reads=yk + ["sin4"], writes=[("rp4", 1)])
                g.op("pool", lambda e: e.tensor_tensor(out=rp4[:, 2], in0=t2, in1=cs_, op=ALU.mult), reads=yk + ["cos4"], writes=[("rp4", 2)])
                g.op("pool", lambda e: e.tensor_tensor(out=rp4[:, 3], in0=t1, in1=sn_, op=ALU.mult), reads=yk + ["sin4"], writes=[("rp4", 3)])
                g.op("pool", lambda e: e.tensor_tensor(out=yb16[:, :, 0:8], in0=rp4[:, 0], in1=rp4[:, 1], op=ALU.subtract),
                     reads=[("rp4", 0), ("rp4", 1), "yb4"], writes=["yb4"])
                g.op("pool", lambda e: e.tensor_tensor(out=yb16[:, :, 8:16], in0=rp4[:, 2], in1=rp4[:, 3], op=ALU.add),
                     reads=[("rp4", 2), ("rp4", 3), "yb4"], writes=["yb4"])
                yield
                yield
                proj_p2(hp, tg)
                yield

            def proj_p2(hp, tg):
                par = hp % 2
                for tl in range(4):
                    g.op("pe", lambda e, tl=tl: e.transpose(psT[:, tl * 128:(tl + 1) * 128], yb4[:, tl * 256:tl * 256 + 128],
                                                            identb[:, :]), reads=["yb4", "identb"], writes=[PST])
                    g.op("pe", lambda e, tl=tl: e.transpose(psT[:, 512 + tl * 128:512 + (tl + 1) * 128],
                                                            yb4[:, tl * 256 + 128:tl * 256 + 256], identb[:, :]),
                         reads=["yb4", "identb"], writes=[PST])
                qs = slice(tg * 512, (tg + 1) * 512)
                g.op("act", lambda e: e.copy(out=aqT[par][:, qs], in_=psT[:, 0:512]), reads=[PST], writes=[("aqT", par, tg)])
                g.op("dve", lambda e: e.tensor_copy(out=akT[par][:, qs], in_=psT[:, 512:1024]), reads=[PST], writes=[("akT", par, tg)])

            ecnt = 0
            ocnt = 0
            wa_cur = load_wa(0)
            for tg in range(4):
                for _ in proj_p1(0, tg, *wa_cur):
                    pass
            for hp in range(4):
                par = hp % 2
                wa_nxt = load_wa(hp + 1) if hp < 3 else None
                def all_parts(hp=hp, wa_nxt=wa_nxt):
                    if wa_nxt is None:
                        return
                    for tg in range(4):
                        yield from proj_p1(hp + 1, tg, *wa_nxt)
                parts = all_parts()
                steps = []
                seg_end = []
                for hl in range(2):
                    for qb in range(4):
                        kts = list(range(max(0, qb * 4 - 8), min(15, qb * 4 + 11) + 1))
                        po = 3 + (ocnt % 2)
                        ocnt += 1
                        for n_, kt in enumerate(kts):
                            steps.append((hl, qb, kt, n_, n_ == len(kts) - 1, po, (ecnt + len(steps)) % 3))
                        seg_end.append(len(steps) - 1)
                ecnt += len(steps)
                LA = 2

                def emit_score(si, hp=hp, par=par):
                    hl, qb, kt, n_, last, po, pS = steps[si]
                    rows = slice(hl * 64, (hl + 1) * 64)
                    qs = slice(qb * 512, (qb + 1) * 512)
                    ks = slice(kt * 128, (kt + 1) * 128)
                    j0 = MASK_C - (kt * 128 - qb * 512)
                    bk = SBANK[pS]
                    g.op("pe", lambda e: e.matmul(ps[bk][:, :], lhsT=akT[par][rows, ks], rhs=aqT[par][rows, qs], start=True, stop=True),
                         reads=[("akT", par, kt // 4), ("aqT", par, qb)], writes=[("ps", bk)])
                    g.op("act", lambda e: e.activation(out=Eb[pS], in_=ps[bk][:, :], func=AF.Exp),
                         reads=[("ps", bk)], writes=[("Eb", pS)])
                    g.op("dve", lambda e: e.tensor_tensor(out=Pm[pS], in0=Eb[pS], in1=maskT[:, j0:j0 + 512], op=ALU.mult),
                         reads=[("Eb", pS), "maskT"], writes=[("Pm", pS)])

                def emit_pv(si, hp=hp, par=par):
                    hl, qb, kt, n_, last, po, pS = steps[si]
                    rows = slice(hl * 64, (hl + 1) * 64)
                    qs = slice(qb * 512, (qb + 1) * 512)
                    g.op("pe", lambda e: e.matmul(ps[po][:, :], lhsT=av2[par][:, kt, hl, :], rhs=Pm[pS], start=(n_ == 0), stop=last),
                         reads=[("av2", par, kt // 4), ("av2ones", par), ("Pm", pS)], writes=[("ps", po)])
                    if last:
                        rr = Pm[pS]
                        g.op("act", lambda e: e.activation(out=rd[po % 2][0:64, :], in_=ps[po][64:128, :], func=AF.Ln),
                             reads=[("ps", po)], writes=[("rd", po % 2)])
                        g.op("act", lambda e: e.activation(out=rd[po % 2][0:64, :], in_=rd[po % 2][0:64, :], func=AF.Exp, scale=-1.0),
                             reads=[("rd", po % 2)], writes=[("rd", po % 2)])
                        g.op("dve", lambda e: e.tensor_tensor(out=mixh[rows, hp, qs], in0=ps[po][0:64, :], in1=rd[po % 2][0:64, :],
                                                              op=ALU.mult),
                             reads=[("ps", po), ("rd", po % 2)], writes=[("mixh", hp, qb)])
                for si in range(len(steps) + LA):
                    if si < len(steps):
                        emit_score(si)
                    if si >= LA:
                        emit_pv(si - LA)
                    if si % 3 == 2:
                        next(parts, None)
                for _ in parts:
                    pass
            dbg("mixa", mixh, [128, 4, S], BF16, [("mixh", k_, q_) for k_ in range(4) for q_ in range(4)], b)
            wout_half(1)
            ar.release(m2)
            g.barrier()

            m3 = ar.mark()
            G = ar.alloc(16 * 16, F32).rearrange("p (t n) -> p t n", t=16)
            G4 = G.rearrange("p t (a h) -> p t a h", a=4)
            LF = ar.alloc(16 * 8, F32).rearrange("p (t n) -> p t n", t=16)
            LF4 = LF.rearrange("p t (a h) -> p t a h", a=2)
            sB = ar.alloc(16 * 16, F32).rearrange("p (t n) -> p t n", t=16)
            T1 = ar.alloc(16 * 8, F32).rearrange("p (t n) -> p t n", t=16)
            T2 = T1
            Call = ar.alloc(16 * 8, F32).rearrange("p (t n) -> p t n", t=16)
            Aall = ar.alloc(16 * 8, F32).rearrange("p (t n) -> p t n", t=16)
            EBa = ar.alloc(16 * 8, F32).rearrange("p (t n) -> p t n", t=16)
            EBH = ar.alloc(16 * 8, F32).rearrange("p (t n) -> p t n", t=16)
            mqT = ar.alloc(S, BF16)
            mkT = ar.alloc(S, BF16)
            ktok = ar.alloc(16 * 128, BF16).rearrange("p (t n) -> p t n", t=16)
            V1 = ar.alloc(16 * 2 * 130, BF16).rearrange("p (t h n) -> p t h n", t=16, h=2)
            sgb = [ar.alloc(256, BF16) for _ in range(2)]
            hacc = ar.alloc(16 * 256, F32).rearrange("p (t h n) -> p t h n", t=16, h=2)
            etm = [ar.alloc(256, F32) for _ in range(2)]
            Cst = ar.alloc(2 * 130, F32).rearrange("p (d n) -> p d n", d=2)
            Cbf = ar.alloc(2 * 130, BF16).rearrange("p (d n) -> p d n", d=2)
            Sm = [ar.alloc(128, BF16) for _ in range(8)]
            vw = [ar.alloc(130, BF16) for _ in range(8)]
            dn = [ar.alloc(2, F32) for _ in range(8)]
            hsq = [ar.alloc(256, F32)] * 2
            hst = [ar.alloc(2, F32) for _ in range(2)]
            hy = [ar.alloc(256, F32) for _ in range(2)]
            hyb = [ar.alloc(256, BF16) for _ in range(2)]
            g.op("dve", lambda e: e.memset(V1[:, :, :, 128:130], 1.0), writes=["V1ones"])

            for mp in range(2):
                sw = wload([win_v[:, :, mp * 128:(mp + 1) * 128], win_v[:, :, 256 + mp * 128:256 + (mp + 1) * 128]],
                           [lambda sl, q=q: sl[:, 0:NCH * 256].rearrange("p (c n) -> p c n", c=NCH)[:, :, q * 128:(q + 1) * 128]
                            for q in range(2)])
                wq = ring[:, sw, 0:NCH * 256].rearrange("p (c n) -> p c n", c=NCH)
                cnt = 0
                for q in range(2):
                    for blk in range(4):
                        pp = 5 + (cnt % 2)
                        cnt += 1
                        tok = slice(blk * 512, (blk + 1) * 512)
                        for c in range(NCH):
                            g.op("pe", lambda e, c=c, q=q, pp=pp, tok=tok, wq=wq: e.matmul(
                                ps[pp][:, :], lhsT=wq[:, c, q * 128:(q + 1) * 128], rhs=h2T[:, c, tok],
                                start=(c == 0), stop=(c == NCH - 1)),
                                reads=[("ring", sw), ("h2T", c, blk)], writes=[("ps", pp)])
                        if q == 0:
                            g.op("act", lambda e, pp=pp, tok=tok: e.copy(out=mqT[:, tok], in_=ps[pp][:, :]),
                                 reads=[("ps", pp)], writes=[("mqT", blk)])
                        else:
                            g.op("act", lambda e, pp=pp, tok=tok: e.mul(out=mkT[:, tok], in_=ps[pp][:, :], mul=0.125),
                                 reads=[("ps", pp)], writes=[("mkT", blk)])
                s1 = wload([win_v[:, :, 256 + mp * 128:256 + (mp + 1) * 128], win_v[:, :, 512 + mp * 256:512 + (mp + 1) * 256],
                            win_v[:, :, 1536:1552]],
                           [lambda sl: sl[:, 0:NCH * 400].rearrange("p (c n) -> p c n", c=NCH)[:, :, 0:128],
                            lambda sl: sl[:, 0:NCH * 400].rearrange("p (c n) -> p c n", c=NCH)[:, :, 128:384],
                            lambda sl: sl[:, 0:NCH * 400].rearrange("p (c n) -> p c n", c=NCH)[:, :, 384:400]])
                w1 = ring[:, s1, 0:NCH * 400].rearrange("p (c n) -> p c n", c=NCH)
                for tt in range(16):
                    ts_ = slice(tt * 128, (tt + 1) * 128)
                    blk = tt // 4
                    pp = 5 + (tt % 2)
                    for c in range(NCH):
                        g.op("pe", lambda e, c=c, ts_=ts_, w1=w1, pp=pp: e.matmul(
                            ps[pp][:, 0:400], lhsT=h2T[:, c, ts_], rhs=w1[:, c, :], start=(c == 0), stop=(c == NCH - 1)),
                            reads=[("ring", s1), ("h2T", c, blk)], writes=[("ps", pp)])
                    g.op("act", lambda e, tt=tt, pp=pp: e.mul(out=ktok[:, tt, :], in_=ps[pp][:, 0:128], mul=0.125),
                         reads=[("ps", pp)], writes=[("ktok", tt)])
                    g.op("act", lambda e, tt=tt, pp=pp: e.copy(out=V1[:, tt, :, 0:128],
                                                               in_=ps[pp][:, 128:384].rearrange("p (h n) -> p h n", h=2)),
                         reads=[("ps", pp)], writes=[("V1", tt)])
                    if mp == 0:
                        g.op("dve", lambda e, tt=tt, pp=pp: e.tensor_tensor(out=G[:, tt, :], in0=ps[pp][:, 384:400],
                                                                           in1=gbias[:, :], op=ALU.add),
                             reads=[("ps", pp), "gbias"], writes=[("G", tt)])
                if mp == 0:
                    allG = [("G", tt) for tt in range(16)]
                    g.op("act", lambda e: e.activation(out=LF4, in_=G4[:, :, 1:4:2, :], func=AF.Exp, scale=-1.0),
                         reads=allG, writes=["LF"])
                    g.op("act", lambda e: e.activation(out=LF, in_=LF, func=AF.Ln, bias=1.0, scale=1.0),
                         reads=["LF"], writes=["LF"])
                    g.op("dve", lambda e: e.tensor_scalar(out=LF, in0=LF, scalar1=-1.0, scalar2=None, op0=ALU.mult),
                         reads=["LF"], writes=["LF"])
                    for tt in range(16):
                        o = tt * 16
                        g.op("pe", lambda e, tt=tt, o=o: e.matmul(ps[4][:, o:o + 4], lhsT=trif[:, :], rhs=LF4[:, tt, 0, :],
                                                                  start=True, stop=True, skip_group_check=True),
                             reads=["LF", "trif"], writes=[("ps", 4)])
                        g.op("pe", lambda e, tt=tt, o=o: e.matmul(ps[4][:, o + 4:o + 8], lhsT=trib[:, :], rhs=LF4[:, tt, 1, :],
                                                                  start=True, stop=True, skip_group_check=True),
                             reads=["LF", "trib"], writes=[("ps", 4)])
                        g.op("pe", lambda e, tt=tt, o=o: e.matmul(ps[4][:, o + 8:o + 16], lhsT=onesf[:, :], rhs=LF[:, tt, :],
                                                                  start=True, stop=True, skip_group_check=True),
                             reads=["LF", "onesf"], writes=[("ps", 4)])
                    g.op("act", lambda e: e.copy(out=sB, in_=ps[4][:, 0:256].rearrange("p (t n) -> p t n", t=16)),
                         reads=[("ps", 4)], writes=["sB"])
                    LI = G4[:, :, 0:4:2, :]
                    T1v = T1.rearrange("p t (a h) -> p t a h", a=2)
                    bc4 = sB[:, :, 0:8].rearrange("p t (a h) -> p t a h", a=2)
                    g.op("dve", lambda e: e.tensor_tensor(out=T1v, in0=LI, in1=bc4, op=ALU.subtract),
                         reads=allG + ["sB"], writes=["T1"])
                    g.op("dve", lambda e: e.scalar_tensor_tensor(out=T1, in0=sB[:, :, 8:16], scalar=0.5, in1=T1,
                                                                 op0=ALU.mult, op1=ALU.add), reads=["sB", "T1"], writes=["T1"])
                    g.op("act", lambda e: e.activation(out=Call, in_=T1, func=AF.Exp), reads=["T1"], writes=["Call"])
                    g.op("dve", lambda e: e.scalar_tensor_tensor(out=T2, in0=sB[:, :, 8:16], scalar=-0.5, in1=sB[:, :, 0:8],
                                                                 op0=ALU.mult, op1=ALU.add), reads=["sB"], writes=["T1"])
                    g.op("act", lambda e: e.activation(out=Aall, in_=T2, func=AF.Exp), reads=["T1"], writes=["Aall"])
                    g.op("act", lambda e: e.activation(out=EBa, in_=sB[:, :, 8:16], func=AF.Exp), reads=["sB"], writes=["EBa"])
                    g.op("act", lambda e: e.activation(out=EBH, in_=sB[:, :, 8:16], func=AF.Exp, scale=0.5),
                         reads=["sB"], writes=["EBH"])
                g.op("dve", lambda e: e.memset(Cst, 0.0), writes=[("Cst", d_, hl_) for d_ in range(2) for hl_ in range(2)])
                g.op("dve", lambda e: e.memset(Cbf, 0.0), writes=[("Cbf", d_, hl_) for d_ in range(2) for hl_ in range(2)])
                qcnt = [0]

                def chain_a(step, hl, dr, mp=mp):
                    head = mp * 2 + hl
                    rows = slice(hl * 64, (hl + 1) * 64)
                    tt = step if dr == 0 else 15 - step
                    ts_ = slice(tt * 128, (tt + 1) * 128)
                    j = dr * 4 + head
                    c_ = hl * 2 + dr
                    z = c_ * 2 + (step % 2)
                    pq = c_
                    g.op("pe", lambda e: e.matmul(ps[pq][:, 0:128], lhsT=mkT[rows, ts_], rhs=mqT[rows, ts_], start=True, stop=True),
                         reads=[("mkT", tt // 4), ("mqT", tt // 4)], writes=[("ps", pq)])
                    g.op("act", lambda e: e.activation(out=vw[z][:, 0:129], in_=V1[:, tt, hl, 0:129], func=AF.Identity,
                                                       scale=Call[:, tt, j:j + 1]),
                         reads=[("V1", tt), "V1ones", "Call"], writes=[("vw", z)])
                    yield
                    mk_ = mkf if dr == 0 else mkb
                    g.op("dve", lambda e: e.tensor_tensor(out=Sm[z], in0=ps[pq][:, 0:128], in1=mk_[:, :], op=ALU.mult),
                         reads=[("ps", pq), "mkf", "mkb"], writes=[("Sm", z)])
                    yield

                def chain_b(step, hl, dr, mp=mp):
                    head = mp * 2 + hl
                    rows = slice(hl * 64, (hl + 1) * 64)
                    tt = step if dr == 0 else 15 - step
                    ts_ = slice(tt * 128, (tt + 1) * 128)
                    j = dr * 4 + head
                    c_ = hl * 2 + dr
                    z = c_ * 2 + (step % 2)
                    pc = 4 + c_
                    pso = ps[pc][:, 256:385]
                    g.op("pe", lambda e: e.matmul(pso, lhsT=Sm[z], rhs=vw[z][:, 0:129], start=True, stop=False),
                         reads=[("Sm", z), ("vw", z)], writes=[("ps", pc)])
                    g.op("pe", lambda e: e.matmul(pso, lhsT=mqT[rows, ts_], rhs=Cbf[rows, dr, 0:129], start=False, stop=True),
                         reads=[("mqT", tt // 4), ("Cbf", dr, hl)], writes=[("ps", pc)])
                    g.op("pe", lambda e: e.matmul(ps[pc][rows, 0:129], lhsT=ktok[:, tt, rows], rhs=vw[z][:, 0:129], start=True, stop=True,
                                                  skip_group_check=True),
                         reads=[("ktok", tt), ("vw", z)], writes=[("ps", pc)])
                    yield
                    g.op("dve", lambda e: e.tensor_scalar(out=Cst[rows, dr, 0:129], in0=Cst[rows, dr, 0:129],
                                                          scalar1=EBa[rows, tt, j:j + 1], scalar2=None, op0=ALU.mult),
                         reads=[("Cst", dr, hl), "EBa"], writes=[("Cst", dr, hl)])
                    acol = Aall[:, tt, j:j + 1]
                    g.op("act", lambda e: e.activation(out=dn[z][:, 0:1], in_=ps[pc][:, 384:385], func=AF.Abs, scale=acol),
                         reads=[("ps", pc), "Aall"], writes=[("dn", z)])
                    yield
                    g.op("dve", lambda e: e.scalar_tensor_tensor(out=Cst[rows, dr, 0:129], in0=ps[pc][rows, 0:129],
                                                                 scalar=EBH[rows, tt, j:j + 1], in1=Cst[rows, dr, 0:129],
                                                                 op0=ALU.mult, op1=ALU.add),
                         reads=[("ps", pc), ("Cst", dr, hl), "EBH"], writes=[("Cst", dr, hl)])
                    yield
                    if step < 15:
                        tn = tt + 1 if dr == 0 else tt - 1
                        g.op("act", lambda e: e.activation(out=Cbf[rows, dr, 0:129], in_=Cst[rows, dr, 0:129], func=AF.Identity,
                                                           scale=EBH[rows, tn, j:j + 1]),
                             reads=[("Cst", dr, hl), "EBH"], writes=[("Cbf", dr, hl)])
                    g.op("dve", lambda e: e.tensor_scalar(out=dn[z][:, 0:1], in0=dn[z][:, 0:1], scalar1=1.0, scalar2=None, op0=ALU.max),
                         reads=[("dn", z)], writes=[("dn", z)])
                    yield
                    g.op("dve", lambda e: e.reciprocal(out=dn[z][:, 0:1], in_=dn[z][:, 0:1]), reads=[("dn", z)], writes=[("dn", z)])
                    yield
                    g.op("dve", lambda e: e.tensor_tensor(out=dn[z][:, 1:2], in0=dn[z][:, 0:1], in1=acol, op=ALU.mult),
                         reads=[("dn", z), "Aall"], writes=[("dn", z)])
                    yield
                    is_first = (dr == 0 and tt <= 15 - tt) or (dr == 1 and (15 - tt) < tt)
                    if is_first:
                        g.op("act", lambda e: e.activation(out=hacc[:, tt, hl, :], in_=ps[pc][:, 256:384], func=AF.Identity,
                                                           scale=dn[z][:, 1:2]),
                             reads=[("ps", pc), ("dn", z)], writes=[("hacc", tt, hl)])
                    else:
                        g.op("dve", lambda e: e.scalar_tensor_tensor(out=hacc[:, tt, hl, :], in0=ps[pc][:, 256:384], scalar=dn[z][:, 1:2],
                                                                     in1=hacc[:, tt, hl, :], op0=ALU.mult, op1=ALU.add),
                             reads=[("ps", pc), ("dn", z), ("hacc", tt, hl)], writes=[("hacc", tt, hl)])
                    yield

                def round_robin(gens):
                    gens = list(gens)
                    while gens:
                        alive = []
                        for ge in gens:
                            try:
                                next(ge)
                                alive.append(ge)
                            except StopIteration:
                                pass
                        gens = alive

                chains = [(hl_, dr_) for hl_ in range(2) for dr_ in range(2)]
                round_robin([chain_a(0, hl_, dr_) for hl_, dr_ in chains])
                for step in range(16):
                    if step < 15:
                        round_robin([chain_a(step + 1, hl_, dr_) for hl_, dr_ in chains])
                    round_robin([chain_b(step, hl_, dr_) for hl_, dr_ in chains])
                s3 = wload([win_v[:, :, 1024 + mp * 256:1024 + (mp + 1) * 256]],
                           [lambda sl: sl[:, 0:NCH * 256].rearrange("p (c n) -> p c n", c=NCH)])
                w3 = ring[:, s3, 0:NCH * 256].rearrange("p (c n) -> p c n", c=NCH)
                for tt in range(16):
                    u = tt % 2
                    ts_ = slice(tt * 128, (tt + 1) * 128)
                    pp = 5 + (tt % 2)
                    for c in range(NCH):
                        g.op("pe", lambda e, c=c, ts_=ts_, w3=w3, pp=pp: e.matmul(
                            ps[pp][:, 0:256], lhsT=h2T[:, c, ts_], rhs=w3[:, c, :], start=(c == 0), stop=(c == NCH - 1)),
                            reads=[("ring", s3), ("h2T", c, tt // 4)], writes=[("ps", pp)])
                    g.op("act", lambda e, u=u, pp=pp: e.activation(out=etm[u], in_=ps[pp][:, 0:256], func=AF.Exp, scale=-1.0),
                         reads=[("ps", pp)], writes=[("etm", u)])
                    g.op("dve", lambda e, u=u: e.tensor_scalar(out=etm[u], in0=etm[u], scalar1=1.0, scalar2=None, op0=ALU.add),
                         reads=[("etm", u)], writes=[("etm", u)])
                    g.op("dve", lambda e, u=u: e.reciprocal(out=etm[u], in_=etm[u]),
                         reads=[("etm", u)], writes=[("etm", u)])
                    hk = [("hacc", tt, 0), ("hacc", tt, 1)]
                    hv = hacc[:, tt, :, :]
                    h3 = hsq[u].rearrange("p (h n) -> p h n", h=2)
                    y3 = hy[u].rearrange("p (h n) -> p h n", h=2)
                    g.op("dve", lambda e, hv=hv, h3=h3: e.tensor_tensor(out=h3, in0=hv, in1=hv, op=ALU.mult),
                         reads=hk, writes=["hsq"])
                    g.op("dve", lambda e, u=u, h3=h3: e.tensor_reduce(out=hst[u], in_=h3, axis=AX.X, op=ALU.add),
                         reads=["hsq"], writes=[("hst", u)])
                    g.op("act", lambda e, u=u: e.activation(out=hst[u], in_=hst[u], func=AF.Ln, bias=EPS, scale=1.0 / 128),
                         reads=[("hst", u)], writes=[("hst", u)])
                    g.op("act", lambda e, u=u: e.activation(out=hst[u], in_=hst[u], func=AF.Exp, scale=-0.5),
                         reads=[("hst", u)], writes=[("hst", u)])
                    for hl in range(2):
                        head = mp * 2 + hl
                        g.op("dve", lambda e, u=u, hl=hl, head=head, hv=hv, y3=y3: e.scalar_tensor_tensor(
                            out=y3[:, hl, :], in0=hv[:, hl, :], scalar=hst[u][:, hl:hl + 1], in1=gmh[:, head * 128:(head + 1) * 128],
                            op0=ALU.mult, op1=ALU.mult), reads=hk + [("hst", u), "gmh"], writes=[("hy", u, hl)])
                    g.op("dve", lambda e, u=u, tt=tt: e.tensor_tensor(out=hyb[u], in0=hy[u], in1=etm[u], op=ALU.mult),
                         reads=[("hy", u, 0), ("hy", u, 1), ("etm", u)], writes=[("hyb", u)])
                    def fin_tr(tt, mp=mp):
                        u = tt % 2
                        ts_ = slice(tt * 128, (tt + 1) * 128)
                        for hl in range(2):
                            g.op("pe", lambda e, hl=hl: e.transpose(psT[:, 256 + hl * 128:256 + (hl + 1) * 128],
                                                                    hyb[u][:, hl * 128:(hl + 1) * 128], identb[:, :]),
                                 reads=[("hyb", u), "identb"], writes=[PST])
                        g.op("act", lambda e: e.copy(
                            out=mixh[:, mp * 2:mp * 2 + 2, ts_], in_=psT[:, 256:512].rearrange("p (h n) -> p h n", h=2)),
                            reads=[PST], writes=[("mixh", mp * 2, tt // 4), ("mixh", mp * 2 + 1, tt // 4)])
                    if tt >= 1:
                        fin_tr(tt - 1)
                    if tt == 15:
                        fin_tr(15)
            dbg("mixm", mixh, [128, 4, S], BF16, [("mixh", k_, q_) for k_ in range(4) for q_ in range(4)], b)
            dbg("G", G, [128, 16, 16], F32, [("G", t_) for t_ in range(16)], b)
            dbg("LF", LF, [128, 16, 8], F32, ["LF"], b)
            dbg("sB", sB, [128, 16, 16], F32, ["sB"], b)
            dbg("Call", Call, [128, 16, 8], F32, ["Call"], b)
            dbg("Aall", Aall, [128, 16, 8], F32, ["Aall"], b)
            dbg("hacc", hacc, [128, 16, 2, 128], F32, [("hacc", t_, h_) for t_ in range(16) for h_ in range(2)], b)
            wout_half(0)
            ar.release(m0)
            g.barrier()

        def final(b, raw=False):
            m0 = ar.mark()
            outs = []
            if raw:
                for c in range(NCH):
                    outs.append(g.dma("sp", out_d[b, c * 128:(c + 1) * 128, :], xT[:, c, :],
                                      reads=[("xT", c, blk) for blk in range(4)]))
                return outs
            rstd = ar.alloc(S, F32)
            sq = ar.alloc(NCH * 512, BF16).rearrange("p (c n) -> p c n", c=NCH)
            ob = [ar.alloc(512, F32) for _ in range(4)]
            rms_rstd(b, rstd, sq)
            n = 0
            for blk in range(4):
                tok = slice(blk * 512, (blk + 1) * 512)
                for c in range(NCH):
                    o_ = ob[n % 4]
                    g.op("dve", lambda e, c=c, tok=tok, o_=o_: e.scalar_tensor_tensor(
                        out=o_, in0=xT[:, c, tok], scalar=g4[:, 3, c:c + 1], in1=rstd[:, tok], op0=ALU.mult, op1=ALU.mult),
                        reads=[("xT", c, blk), ("rstd", blk), "g4"], writes=[("ob", n % 4)])
                    outs.append(g.dma("sp", out_d[b, c * 128:(c + 1) * 128, tok], o_, reads=[("ob", n % 4)]))
                    n += 1
            ar.release(m0)
            g.barrier()
            return outs

        outs = []
        adaln(list(range(0, 6)))
        derive(0)
        for b in range(2):
            load_x(b)
            ffn(b, 0, 0)
            if b == 0:
                adaln(list(range(6, 18)))
                derive(1)
                derive(2)
            if stage >= 2:
                mixer(b)
            if stage >= 3:
                ffn(b, 2, 1)
            outs += final(b, raw=(stage < 3))
        g.emit(final_wait_ops=outs + dbg_outs)
    return nc


def _consts():
    p = np.arange(128)[:, None]
    j = np.arange(MASK_W)[None, :]
    dlt = p - j + MASK_C
    a = np.abs(dlt)
    m = (a <= 64).astype(np.float32) + ((dlt % 4 == 0) & (a <= 256)) + ((dlt % 16 == 0) & (a <= 1024))
    maskT = m.astype(ml_dtypes.bfloat16)
    identb = np.eye(128, dtype=np.float32).astype(ml_dtypes.bfloat16)
    u = np.arange(128)[:, None]
    t = np.arange(128)[None, :]
    trif = (u <= t).astype(np.float32)
    trib = (u >= t).astype(np.float32)
    maskf = (u <= t).astype(np.float32).astype(ml_dtypes.bfloat16)
    maskb = (u >= t).astype(np.float32).astype(ml_dtypes.bfloat16)
    half = 8
    inv_freq = (500000.0 ** (-2.0 * np.arange(half, dtype=np.float32) / 16.0)).astype(np.float32)
    pos = np.arange(S, dtype=np.float32)
    ang = (pos[:, None] * inv_freq[None, :]).astype(np.float32)
    cos = np.cos(ang).astype(np.float32).reshape(16, 128, 8).transpose(1, 0, 2)
    sin = np.sin(ang).astype(np.float32).reshape(16, 128, 8).transpose(1, 0, 2)
    cos4 = np.ascontiguousarray(np.broadcast_to(cos[:, :, None, :], (128, 16, 4, 8))).astype(np.float32)
    sin4 = np.ascontiguousarray(np.broadcast_to(sin[:, :, None, :], (128, 16, 4, 8))).astype(np.float32)
    return dict(maskT=maskT, identb=identb, trif=trif, trib=trib, maskf=maskf, maskb=maskb, cos4=cos4, sin4=sin4)


def _cols(v):
    return np.ascontiguousarray(np.asarray(v, np.float32).reshape(-1, 128).T)


_NC_CACHE = {}


def kernel(x, c, w_ada, b_ada, g_ffn1, w_gu1, w_down1, g_mix, w_in, gate_bias, g_q, g_k, g_mh,
           w_out, g_ffn2, w_gu2, w_down2, g_final):
    stage = int(os.environ.get("MK_STAGE", "3"))
    x = np.asarray(x, np.float32)
    c = np.asarray(c, np.float32)
    if stage not in _NC_CACHE:
        _NC_CACHE[stage] = build_program(stage)
    nc = _NC_CACHE[stage]
    consts = _consts()
    shared = dict(
        w_ada=np.ascontiguousarray(np.asarray(w_ada, np.float32)[0]),
        b_adaT=_cols(np.asarray(b_ada)[0]),
        g4=np.ascontiguousarray(np.stack([_cols(np.asarray(v)[0]) for v in (g_ffn1, g_mix, g_ffn2, g_final)], axis=1)),
        w_gu1=np.ascontiguousarray(np.asarray(w_gu1, np.float32)[0]),
        w_gu2=np.ascontiguousarray(np.asarray(w_gu2, np.float32)[0]),
        w_down1=np.ascontiguousarray(np.asarray(w_down1, np.float32)[0]),
        w_down2=np.ascontiguousarray(np.asarray(w_down2, np.float32)[0]),
        w_in=np.ascontiguousarray(np.asarray(w_in, np.float32)[0]),
        w_out=np.ascontiguousarray(np.asarray(w_out, np.float32)[0]),
        gbias_rep=np.ascontiguousarray(np.broadcast_to(np.asarray(gate_bias, np.float32)[0].reshape(1, 16), (128, 16))),
        gqk_rep=np.ascontiguousarray(np.broadcast_to(
            np.stack([np.asarray(g_q, np.float32)[0]] * 2 + [np.asarray(g_k, np.float32)[0]] * 2)[None], (128, 4, 64))),
        gmh_rep=np.ascontiguousarray(np.broadcast_to(np.asarray(g_mh, np.float32)[0][None, :], (128, 512))),
        **consts,
    )
    in_maps = []
    for i in range(8):
        m = dict(shared)
        m["xT"] = np.ascontiguousarray(x[2 * i:2 * i + 2].transpose(0, 2, 1))
        m["cT"] = np.ascontiguousarray(c[2 * i:2 * i + 2].reshape(2, NCH, 128).transpose(2, 1, 0))
        in_maps.append(m)
    res = run_bass_kernel_spmd(nc, in_maps, core_ids=list(range(8)))
    out = np.empty((16, S, D), np.float32)
    for i in range(8):
        out[2 * i:2 * i + 2] = res.results[i]["outT"].transpose(0, 2, 1)
    return out
```

```python
import os
import contextlib
import numpy as np
import ml_dtypes
import concourse.bass as bass
import concourse.mybir as mybir
from concourse.bass_utils import run_bass_kernel_spmd

F32 = mybir.dt.float32
BF16 = mybir.dt.bfloat16
AF = mybir.ActivationFunctionType
ALU = mybir.AluOpType
AX = mybir.AxisListType

ENG_NAMES = ("pe", "act", "dve", "pool", "sp")
D = 1024
S = 2048
DFF = 2816
NCH = 8
NF = 22
EPS = 1e-6
MASK_C = 1408
MASK_W = 2944
NSLOT = 3
SLOT_ELEMS = 4096


class Op:
    __slots__ = ("eng", "fn", "deps", "is_dma", "sem", "val", "has_dep", "idx", "name", "prewait")

    def __init__(self, eng, fn, name=""):
        self.eng = eng
        self.fn = fn
        self.deps = []
        self.is_dma = False
        self.sem = None
        self.val = 0
        self.has_dep = False
        self.idx = 0
        self.name = name
        self.prewait = None


class Graph:
    def __init__(self, nc, n_dma_sems=16):
        self.nc = nc
        self.ops = {e: [] for e in ENG_NAMES}
        self.last_writer = {}
        self.readers = {}
        self.n_dma_sems = n_dma_sems
        self.dma_count = {e: 0 for e in ENG_NAMES}
        self.dma_ops = {e: [] for e in ENG_NAMES}
        self.barrier_deps = {}
        self.sp_since_barrier = []

    def _link(self, op, deps):
        latest = {}
        keep = []
        for d in deps:
            if d is op:
                continue
            if d.is_dma:
                keep.append(d)
                continue
            if d.eng == "pe" and op.eng == "pe":
                continue
            cur = latest.get(d.eng)
            if cur is None or d.idx > cur.idx:
                latest[d.eng] = d
        keep.extend(latest.values())
        seen = set(id(d) for d in op.deps)
        for d in keep:
            if id(d) in seen:
                continue
            seen.add(id(d))
            op.deps.append(d)
            d.has_dep = True

    def _add_deps(self, op, reads, writes):
        deps = []
        for r in reads:
            w = self.last_writer.get(r)
            if w is not None:
                deps.append(w)
            if (isinstance(r, tuple) and r[0] == "ps") or (isinstance(r, str) and r.startswith("psT")):
                deps.extend(x for x in self.readers.get(r, ()) if x.eng != op.eng)
        for w_ in writes:
            w = self.last_writer.get(w_)
            if w is not None:
                deps.append(w)
            deps.extend(self.readers.get(w_, ()))
        b = self.barrier_deps.pop(op.eng, None)
        if b:
            deps.extend(b)
        self._link(op, deps)
        for r in reads:
            self.readers.setdefault(r, []).append(op)
        for w_ in writes:
            self.last_writer[w_] = op
            self.readers[w_] = []

    def op(self, eng, fn, reads=(), writes=(), name=""):
        o = Op(eng, fn, name)
        o.idx = len(self.ops[eng])
        self._add_deps(o, reads, writes)
        self.ops[eng].append(o)
        return o

    def dma(self, eng, out, in_, reads=(), writes=(), name=""):
        def fn(e, out=out, in_=in_):
            return e.dma_start(out=out, in_=in_)
        o = Op(eng, fn, name)
        o.is_dma = True
        i = self.dma_count[eng]
        self.dma_count[eng] += 1
        o.idx = i
        o.val = 16 * (i // self.n_dma_sems + 1)
        if i >= self.n_dma_sems:
            o.prewait = self.dma_ops[eng][i - self.n_dma_sems]
        self.dma_ops[eng].append(o)
        self._add_deps(o, reads, writes)
        self.ops[eng].append(o)
        if eng == "sp":
            self.sp_since_barrier.append(o)
        return o

    def barrier(self):
        b = []
        for e in ("pe", "act", "dve"):
            for o in reversed(self.ops[e]):
                b.append(o)
                break
        b.extend(self.sp_since_barrier)
        self.sp_since_barrier = []
        for e in ("pe", "act", "dve", "sp"):
            self.barrier_deps[e] = list(b) + list(self.barrier_deps.get(e, ()))

    def emit(self, final_wait_ops=()):
        nc = self.nc
        with contextlib.ExitStack() as st:
            esem = {e: st.enter_context(nc.semaphore("s_" + e)) for e in ENG_NAMES}
            dsem = {}
            for e in ENG_NAMES:
                if self.dma_count[e]:
                    dsem[e] = [st.enter_context(nc.semaphore("d_%s_%d" % (e, i)))
                               for i in range(min(self.n_dma_sems, self.dma_count[e]))]
            for e in ENG_NAMES:
                m = 0
                for o in self.ops[e]:
                    if o.is_dma:
                        o.sem = dsem[e][o.idx % self.n_dma_sems]
                    elif o.has_dep:
                        m += 1
                        o.sem = esem[e]
                        o.val = m
            block = st.enter_context(nc.Block())
            handles = {"pe": block.tensor, "act": block.scalar, "dve": block.vector,
                       "pool": block.gpsimd, "sp": block.sync}

            def make(e):
                ops = self.ops[e]

                def body(eng):
                    waited = {}

                    def wait(d):
                        key = id(d.sem)
                        if waited.get(key, 0) >= d.val:
                            return
                        waited[key] = d.val
                        eng.wait_ge(d.sem, d.val)
                    for o in ops:
                        if o.prewait is not None:
                            wait(o.prewait)
                        for d in o.deps:
                            wait(d)
                        inst = o.fn(eng)
                        if o.is_dma:
                            inst.then_inc(o.sem, 16)
                        elif o.has_dep:
                            inst.then_inc(o.sem, 1)
                    if e == "sp":
                        for d in final_wait_ops:
                            wait(d)
                return body
            for e in ENG_NAMES:
                if self.ops[e] or (e == "sp" and final_wait_ops):
                    handles[e](make(e))


class Arena:
    def __init__(self, ap2d, nelem_bf16):
        self.ap = ap2d
        self.cap = nelem_bf16
        self.off = 0

    def mark(self):
        return self.off

    def release(self, m):
        self.off = m

    def alloc(self, nelem, dt):
        n16 = nelem * (2 if dt == F32 else 1)
        self.off = (self.off + 1) // 2 * 2
        assert self.off + n16 <= self.cap, ("arena overflow", self.off, n16, self.cap)
        a = self.ap[:, self.off:self.off + n16]
        self.off += n16
        if dt == F32:
            a = a.bitcast(F32)
        return a


def build_program(stage=3):
    nc = bass.Bass("TRN2", target_bir_lowering=False)

    def din(name, shape, dt=F32):
        return nc.dram_tensor(name, list(shape), dt, kind="ExternalInput").ap()
    xT_d = din("xT", [2, D, S])
    cT_d = din("cT", [128, NCH, 2])
    wada_d = din("w_ada", [D, 9 * D])
    bada_d = din("b_adaT", [128, 72])
    g4_d = din("g4", [128, 4, NCH])
    wgu_d = [din("w_gu1", [D, 2 * DFF]), din("w_gu2", [D, 2 * DFF])]
    wdn_d = [din("w_down1", [DFF, D]), din("w_down2", [DFF, D])]
    win_d = din("w_in", [D, 3088])
    wout_d = din("w_out", [D, D])
    gbias_d = din("gbias_rep", [128, 16])
    gqk_d = din("gqk_rep", [128, 4, 64])
    gmh_d = din("gmh_rep", [128, 512])
    mask_d = din("maskT", [128, MASK_W], BF16)
    identb_d = din("identb", [128, 128], BF16)
    trif_d = din("trif", [128, 128])
    trib_d = din("trib", [128, 128])
    mkf_d = din("maskf", [128, 128], BF16)
    mkb_d = din("maskb", [128, 128], BF16)
    cos_d = din("cos4", [128, 16, 4, 8])
    sin_d = din("sin4", [128, 16, 4, 8])
    out_d = nc.dram_tensor("outT", [2, D, S], F32, kind="ExternalOutput").ap()

    st = contextlib.ExitStack()
    with st:
        def sbt(name, shape, dt):
            return st.enter_context(nc.sbuf_tensor(name, shape, dt))
        xT = sbt("xT_sb", [128, NCH, S], F32)
        ring = sbt("ring", [128, NSLOT, SLOT_ELEMS], BF16)
        maskT = sbt("maskT_sb", [128, MASK_W], BF16)
        identb = sbt("identb_sb", [128, 128], BF16)
        onesb = sbt("onesb", [128, 128], BF16)
        onesf = sbt("onesf", [128, 128], F32)
        trif = sbt("trif_sb", [128, 128], F32)
        trib = sbt("trib_sb", [128, 128], F32)
        mkf = sbt("mkf_sb", [128, 128], BF16)
        mkb = sbt("mkb_sb", [128, 128], BF16)
        cos4 = sbt("cos4_sb", [128, 16, 4, 8], F32)
        sin4 = sbt("sin4_sb", [128, 16, 4, 8], F32)
        gqk = sbt("gqk_sb", [128, 4, 64], F32)
        gmh = sbt("gmh_sb", [128, 512], F32)
        gbias = sbt("gbias_sb", [128, 16], F32)
        g4 = sbt("g4_sb", [128, 4, NCH], F32)
        badaT = sbt("bada_sb", [128, 72], F32)
        cT = sbt("cT_sb", [128, NCH, 2], F32)
        csT = sbt("csT_sb", [128, NCH, 2], BF16)
        modT = sbt("modT", [128, 72, 2], F32)
        geff = sbt("geff", [128, 2, 3, NCH], F32)
        gate = sbt("gate", [128, 2, 3, NCH], F32)
        ARENA_N = 52736
        arena_t = sbt("arena", [128, ARENA_N], BF16)
        ar = Arena(arena_t[:, :], ARENA_N)
        ps = [st.enter_context(nc.psum_tensor("ps%d" % i, [128, 512], F32)) for i in range(8)]
        psT = ps[7][:, :].bitcast(BF16)
        PST = ("ps", 7)

        g = Graph(nc)
        ring_ctr = [0]
        DBG = os.environ.get("MK_DEBUG") == "1"
        dbg_outs = []

        def dbg(name, ap, shape, dt, reads, b=0):
            if not DBG or b != 0:
                return
            t = nc.dram_tensor("dbg_" + name, list(shape), dt, kind="ExternalOutput").ap()
            dbg_outs.append(g.dma("sp", t, ap, reads=reads))

        def wload(src_aps, views, name=""):
            s = ring_ctr[0] % NSLOT
            ring_ctr[0] += 1
            for src, vw in zip(src_aps, views):
                g.dma("pool", vw(ring[:, s, :]), src, writes=[("ring", s)], name=name)
            return s

        for dst, src, key in ((maskT[:, :], mask_d, "maskT"), (identb[:, :], identb_d, "identb"),
                              (trif[:, :], trif_d, "trif"), (trib[:, :], trib_d, "trib"),
                              (mkf[:, :], mkf_d, "mkf"), (mkb[:, :], mkb_d, "mkb"),
                              (cos4[:], cos_d, "cos4"), (sin4[:], sin_d, "sin4"),
                              (gqk[:], gqk_d, "gqk"), (gmh[:, :], gmh_d, "gmh"), (gbias[:, :], gbias_d, "gbias"),
                              (g4[:], g4_d, "g4"), (badaT[:, :], bada_d, "badaT"), (cT[:], cT_d, "cT")):
            g.dma("sp", dst, src, writes=[key])
        g.op("dve", lambda e: e.memset(onesb[:, :], 1.0), writes=["onesb"])
        g.op("dve", lambda e: e.memset(onesf[:, :], 1.0), writes=["onesf"])
        g.op("dve", lambda e: e.tensor_scalar(out=gqk[:, 0:2, :], in0=gqk[:, 0:2, :], scalar1=0.125, scalar2=None,
                                              op0=ALU.mult), reads=["gqk"], writes=["gqk"])

        def load_x(b):
            for c in range(NCH):
                g.dma("sp", xT[:, c, :], xT_d[b, c * 128:(c + 1) * 128, :],
                      writes=[("xT", c, blk) for blk in range(4)])

        g.op("act", lambda e: e.activation(out=csT[:], in_=cT[:], func=AF.Silu), reads=["cT"], writes=["csT"])
        wada_v = wada_d.rearrange("(c p) n -> p c n", p=128)

        def adaln(groups):
            for grp in groups:
                s = wload([wada_v[:, :, grp * 512:(grp + 1) * 512]],
                          [lambda sl: sl.rearrange("p (c n) -> p c n", c=NCH)])
                wv = ring[:, s, :].rearrange("p (c n) -> p c n", c=NCH)
                for j in range(4):
                    n = grp * 4 + j
                    for k in range(NCH):
                        g.op("pe", lambda e, n=n, k=k, j=j, wv=wv: e.matmul(
                            ps[6][:, 2 * n:2 * n + 2], lhsT=wv[:, k, j * 128:(j + 1) * 128], rhs=csT[:, k, :],
                            start=(k == 0), stop=(k == NCH - 1), skip_group_check=True),
                            reads=[("ring", s), "csT"], writes=[("ps", 6)])
            n0, n1 = groups[0] * 4, groups[-1] * 4 + 4
            part = 0 if n0 == 0 else 1
            for b in range(2):
                g.op("dve", lambda e, b=b: e.tensor_tensor(
                    out=modT[:, n0:n1, b], in0=ps[6][:, 2 * n0 + b:2 * n1 + b:2], in1=badaT[:, n0:n1], op=ALU.add),
                    reads=[("ps", 6), "badaT"], writes=[("modT", part, b)])

        def derive(i):
            n0 = 3 * i * 8
            part = 0 if i == 0 else 1
            for b in range(2):
                g.op("dve", lambda e, b=b: e.scalar_tensor_tensor(
                    out=geff[:, b, i, :], in0=modT[:, n0 + 8:n0 + 16, b], scalar=1.0, in1=g4[:, i, :],
                    op0=ALU.add, op1=ALU.mult), reads=[("modT", part, b), "g4"], writes=[("geff", b, i)])
                g.op("dve", lambda e, b=b: e.tensor_scalar(
                    out=gate[:, b, i, :], in0=modT[:, n0 + 16:n0 + 24, b], scalar1=(1.0 if i == 1 else 0.5),
                    scalar2=None, op0=ALU.mult), reads=[("modT", part, b)], writes=[("gate", b, i)])

        def shift_col(b, i, c):
            return modT[:, 3 * i * 8 + c, b:b + 1]

        def rms_rstd(b, rstd, sq):
            for blk in range(4):
                tok = slice(blk * 512, (blk + 1) * 512)
                for c in range(NCH):
                    g.op("act", lambda e, c=c, tok=tok: e.activation(out=sq[:, c, :], in_=xT[:, c, tok], func=AF.Square),
                         reads=[("xT", c, blk)], writes=[("sq", c)])
                for c in range(NCH):
                    g.op("pe", lambda e, c=c: e.matmul(ps[6][:, :], lhsT=onesb[:, :], rhs=sq[:, c, :],
                                                      start=(c == 0), stop=(c == NCH - 1)),
                         reads=["onesb", ("sq", c)], writes=[("ps", 6)])
                g.op("act", lambda e, tok=tok: e.activation(out=rstd[:, tok], in_=ps[6][:, :], func=AF.Ln,
                                                            bias=EPS, scale=1.0 / D),
                     reads=[("ps", 6)], writes=[("rstd", blk)])
            for blk in range(4):
                tok = slice(blk * 512, (blk + 1) * 512)
                g.op("act", lambda e, tok=tok: e.activation(out=rstd[:, tok], in_=rstd[:, tok], func=AF.Exp, scale=-0.5),
                     reads=[("rstd", blk)], writes=[("rstd", blk)])

        def norm_mod(b, i, rstd, tmp, dst_fn, blk, keyfn):
            tok = slice(blk * 512, (blk + 1) * 512)
            for c in range(NCH):
                tb = tmp[c % 2]
                g.op("dve", lambda e, c=c, tb=tb: e.tensor_tensor(out=tb, in0=xT[:, c, tok], in1=rstd[:, tok], op=ALU.mult),
                     reads=[("xT", c, blk), ("rstd", blk)], writes=[("tmp", c % 2)])
                g.op("act", lambda e, c=c, tb=tb: e.activation(out=dst_fn(c), in_=tb, func=AF.Identity,
                                                               bias=shift_col(b, i, c), scale=geff[:, b, i, c:c + 1]),
                     reads=[("tmp", c % 2), ("geff", b, i), ("modT", 0 if i == 0 else 1, b)], writes=[keyfn(c)])

        def ffn(b, i, which):
            wgu_v = wgu_d[which].rearrange("(c p) n -> p c n", p=128)
            wdn_v = wdn_d[which].rearrange("(k p) n -> p k n", p=128)
            m0 = ar.mark()
            rstd = ar.alloc(S, F32)
            sq = ar.alloc(NCH * 512, BF16).rearrange("p (c n) -> p c n", c=NCH)
            tmp = [ar.alloc(512, F32) for _ in range(2)]
            hT = ar.alloc(NCH * 1024, BF16).rearrange("p (c n) -> p c n", c=NCH)
            actT = ar.alloc(NF * 1024, BF16).rearrange("p (c n) -> p c n", c=NF)
            sg = [ar.alloc(512, BF16) for _ in range(2)]
            rms_rstd(b, rstd, sq)
            cnt = 0
            for sb in range(2):
                for lb in range(2):
                    blk = sb * 2 + lb
                    norm_mod(b, i, rstd, tmp, lambda c, lb=lb: hT[:, c, lb * 512:(lb + 1) * 512], blk,
                             lambda c, lb=lb: ("hT", c, lb))
                for grp in range(11):
                    c0 = grp * 256
                    sgw = wload([wgu_v[:, :, c0:c0 + 256], wgu_v[:, :, DFF + c0:DFF + c0 + 256]],
                                [lambda sl, q=q: sl.rearrange("p (c n) -> p c n", c=NCH)[:, :, q * 256:(q + 1) * 256]
                                 for q in range(2)])
                    wgv = ring[:, sgw, :].rearrange("p (c n) -> p c n", c=NCH)
                    for j in range(2):
                        f = grp * 2 + j
                        for lb in range(2):
                            pg = cnt % 2
                            cnt += 1
                            hs = slice(lb * 512, (lb + 1) * 512)
                            for k in range(NCH):
                                g.op("pe", lambda e, k=k, j=j, wgv=wgv, pg=pg, hs=hs: e.matmul(
                                    ps[pg][:, :], lhsT=wgv[:, k, j * 128:(j + 1) * 128], rhs=hT[:, k, hs],
                                    start=(k == 0), stop=(k == NCH - 1)),
                                    reads=[("ring", sgw), ("hT", k, lb)], writes=[("ps", pg)])
                            for k in range(NCH):
                                g.op("pe", lambda e, k=k, j=j, wgv=wgv, pg=pg, hs=hs: e.matmul(
                                    ps[2 + pg][:, :], lhsT=wgv[:, k, 256 + j * 128:256 + (j + 1) * 128], rhs=hT[:, k, hs],
                                    start=(k == 0), stop=(k == NCH - 1)),
                                    reads=[("ring", sgw), ("hT", k, lb)], writes=[("ps", 2 + pg)])
                            g.op("act", lambda e, pg=pg: e.activation(out=sg[pg], in_=ps[pg][:, :], func=AF.Silu),
                                 reads=[("ps", pg)], writes=[("sg", pg)])
                            g.op("dve", lambda e, pg=pg, f=f, hs=hs: e.tensor_tensor(
                                out=actT[:, f, hs], in0=ps[2 + pg][:, :], in1=sg[pg], op=ALU.mult),
                                reads=[("ps", 2 + pg), ("sg", pg)], writes=[("actT", f, lb)])
                for dc in range(NCH):
                    sw = wload([wdn_v[:, :, dc * 128:(dc + 1) * 128]],
                               [lambda sl: sl[:, 0:NF * 128].rearrange("p (k n) -> p k n", k=NF)])
                    wd = ring[:, sw, 0:NF * 128].rearrange("p (k n) -> p k n", k=NF)
                    for lb in range(2):
                        blk = sb * 2 + lb
                        pd = 4 + (cnt % 2)
                        cnt += 1
                        hs = slice(lb * 512, (lb + 1) * 512)
                        for k in range(NF):
                            g.op("pe", lambda e, k=k, wd=wd, pd=pd, hs=hs: e.matmul(
                                ps[pd][:, :], lhsT=wd[:, k, :], rhs=actT[:, k, hs],
                                start=(k == 0), stop=(k == NF - 1)),
                                reads=[("ring", sw), ("actT", k, lb)], writes=[("ps", pd)])
                        tok = slice(blk * 512, (blk + 1) * 512)
                        g.op("dve", lambda e, pd=pd, dc=dc, tok=tok: e.scalar_tensor_tensor(
                            out=xT[:, dc, tok], in0=ps[pd][:, :], scalar=gate[:, b, i, dc:dc + 1], in1=xT[:, dc, tok],
                            op0=ALU.mult, op1=ALU.add),
                            reads=[("ps", pd), ("gate", b, i), ("xT", dc, blk)], writes=[("xT", dc, blk)])
            ar.release(m0)
            g.barrier()

        def mixer(b):
            i = 1
            win_v = win_d.rearrange("(c p) n -> p c n", p=128)
            wout_v = wout_d.rearrange("(k p) n -> p k n", p=128)
            m0 = ar.mark()
            h2T = ar.alloc(NCH * S, BF16).rearrange("p (c n) -> p c n", c=NCH)
            mixh = ar.alloc(4 * S, BF16).rearrange("p (c n) -> p c n", c=4)
            m1 = ar.mark()
            rstd = ar.alloc(S, F32)
            sq = ar.alloc(NCH * 512, BF16).rearrange("p (c n) -> p c n", c=NCH)
            tmp = [ar.alloc(512, F32) for _ in range(2)]
            rms_rstd(b, rstd, sq)
            for blk in range(4):
                norm_mod(b, i, rstd, tmp, lambda c, blk=blk: h2T[:, c, blk * 512:(blk + 1) * 512], blk,
                         lambda c, blk=blk: ("h2T", c, blk))
            dbg("h2T", h2T, [128, NCH, S], BF16, [("h2T", c, blk) for c in range(NCH) for blk in range(4)], b)
            ar.release(m1)
            g.barrier()

            def wout_half(half):
                cnt = 0
                for dcp in range(2):
                    sw = wload([wout_v[:, half * 4:half * 4 + 4, dcp * 512:(dcp + 1) * 512]],
                               [lambda sl: sl[:, 0:4 * 512].rearrange("p (k n) -> p k n", k=4)])
                    wo = ring[:, sw, 0:4 * 512].rearrange("p (k n) -> p k n", k=4)
                    for dj in range(4):
                        dc = dcp * 4 + dj
                        for blk in range(4):
                            pd = 4 + (cnt % 2)
                            cnt += 1
                            tok = slice(blk * 512, (blk + 1) * 512)
                            for k in range(4):
                                g.op("pe", lambda e, k=k, wo=wo, pd=pd, dj=dj, tok=tok: e.matmul(
                                    ps[pd][:, :], lhsT=wo[:, k, dj * 128:(dj + 1) * 128], rhs=mixh[:, k, tok],
                                    start=(k == 0), stop=(k == 3)),
                                    reads=[("ring", sw), ("mixh", k, blk)], writes=[("ps", pd)])
                            g.op("dve", lambda e, pd=pd, dc=dc, tok=tok: e.scalar_tensor_tensor(
                                out=xT[:, dc, tok], in0=ps[pd][:, :], scalar=gate[:, b, i, dc:dc + 1], in1=xT[:, dc, tok],
                                op0=ALU.mult, op1=ALU.add),
                                reads=[("ps", pd), ("gate", b, i), ("xT", dc, blk)], writes=[("xT", dc, blk)])

            m2 = ar.mark()
            aqT = [ar.alloc(S, BF16) for _ in range(2)]
            akT = [ar.alloc(S, BF16) for _ in range(2)]
            av2 = [ar.alloc(16 * 2 * 128, BF16).rearrange("p (t h n) -> p t h n", t=16, h=2) for _ in range(2)]
            ytm4 = ar.alloc(1024, F32)
            ysq4 = ar.alloc(1024, F32)
            yb4 = ar.alloc(1024, BF16)
            st16 = ar.alloc(16, F32)
            rp4 = ar.alloc(4 * 128, F32).rearrange("p (k a n) -> p k a n", k=4, a=16)
            NSB = 4
            Eb = [ar.alloc(512, BF16) for _ in range(NSB)]
            Pm = Eb
            rd = [ar.alloc(512, F32) for _ in range(2)]
            SBANK = [0, 1, 2, 4]
            for par in range(2):
                g.op("dve", lambda e, par=par: e.memset(av2[par][:, :, :, 64:128], 1.0), writes=[("av2ones", par)])
            y16 = ytm4.rearrange("p (a n) -> p a n", a=16)
            s16 = ysq4.rearrange("p (a n) -> p a n", a=16)
            yb16 = yb4.rearrange("p (a n) -> p a n", a=16)
            y44 = ytm4.rearrange("p (t a n) -> p t a n", t=4, a=4)
            st_b = st16.rearrange("p (a o) -> p a o", o=1).to_broadcast([128, 16, 64])
            gqk_b = gqk[:].rearrange("p (o a) n -> p o a n", o=1).to_broadcast([128, 4, 4, 64])

            def load_wa(hp):
                sw = wload([win_v[:, :, 1552 + hp * 128:1552 + hp * 128 + 128],
                            win_v[:, :, 2064 + hp * 128:2064 + hp * 128 + 128],
                            win_v[:, :, 2576 + hp * 128:2576 + hp * 128 + 128]],
                           [lambda sl, q=q: sl[:, 0:NCH * 384].rearrange("p (c n) -> p c n", c=NCH)[:, :, q * 128:(q + 1) * 128]
                            for q in range(3)])
                return ring[:, sw, 0:NCH * 384].rearrange("p (c n) -> p c n", c=NCH), sw

            def proj_p1(hp, tg, wa, sw):
                par = hp % 2
                for tl in range(4):
                    tt = tg * 4 + tl
                    ts_ = slice(tt * 128, (tt + 1) * 128)
                    bank = 5 + tl // 2
                    co = (tl % 2) * 256
                    for c in range(NCH):
                        g.op("pe", lambda e, c=c, bank=bank, co=co, ts_=ts_: e.matmul(ps[bank][:, co:co + 256], lhsT=h2T[:, c, ts_], rhs=wa[:, c, 0:256],
                                                          start=(c == 0), stop=(c == NCH - 1), skip_group_check=True),
                             reads=[("ring", sw), ("h2T", c, tg)], writes=[("ps", bank)])
                    for c in range(NCH):
                        g.op("pe", lambda e, c=c, tl=tl, ts_=ts_: e.matmul(ps[7][:, tl * 128:(tl + 1) * 128], lhsT=h2T[:, c, ts_],
                                                          rhs=wa[:, c, 256:384], start=(c == 0), stop=(c == NCH - 1),
                                                          skip_group_check=True),
                             reads=[("ring", sw), ("h2T", c, tg)], writes=[("ps", 7)])
                g.op("act", lambda e: e.copy(out=ytm4[:, 0:512], in_=ps[5][:, :]), reads=[("ps", 5)], writes=["ytm4a"])
                g.op("act", lambda e: e.copy(out=ytm4[:, 512:1024], in_=ps[6][:, :]), reads=[("ps", 6)], writes=["ytm4b"])
                g.op("act", lambda e: e.copy(out=av2[par][:, tg * 4:(tg + 1) * 4, :, 0:64],
                                             in_=ps[7][:, :].rearrange("p (t h n) -> p t h n", t=4, h=2)),
                     reads=[("ps", 7)], writes=[("av2", par, tg)])
                yk = ["ytm4a", "ytm4b"]
                g.op("pool", lambda e: e.tensor_tensor(out=ysq4, in0=ytm4, in1=ytm4, op=ALU.mult), reads=yk, writes=["ysq4"])
                yield
                g.op("dve", lambda e: e.tensor_reduce(out=st16, in_=s16, axis=AX.X, op=ALU.add), reads=["ysq4"], writes=["st16"])
                yield
                g.op("act", lambda e: e.activation(out=st16, in_=st16, func=AF.Ln, bias=EPS, scale=1.0 / 64),
                     reads=["st16"], writes=["st16"])
                g.op("act", lambda e: e.activation(out=st16, in_=st16, func=AF.Exp, scale=-0.5), reads=["st16"], writes=["st16"])
                g.op("pool", lambda e: e.tensor_tensor(out=y16, in0=y16, in1=st_b, op=ALU.mult), reads=yk + ["st16"], writes=yk)
                g.op("pool", lambda e: e.tensor_tensor(out=y44, in0=y44, in1=gqk_b, op=ALU.mult), reads=yk + ["gqk"], writes=yk)
                yield
                yield
                g.op("act", lambda e: e.copy(out=yb4, in_=ytm4), reads=yk, writes=["yb4"])
                t1 = y16[:, :, 0:8]
                t2 = y16[:, :, 8:16]
                cs_ = cos4[:, tg * 4:(tg + 1) * 4, :, :].rearrange("p t a n -> p (t a) n")
                sn_ = sin4[:, tg * 4:(tg + 1) * 4, :, :].rearrange("p t a n -> p (t a) n")
                g.op("pool", lambda e: e.tensor_tensor(out=rp4[:, 0], in0=t1, in1=cs_, op=ALU.mult), reads=yk + ["cos4"], writes=[("rp4", 0)])
                g.op("pool", lambda e: e.tensor_tensor(out=rp4[:, 1], in0=t2, in1=sn_, op=ALU.mult), reads=yk + ["sin4"], writes=[("rp4", 1)])
                g.op("pool", lambda e: e.tensor_tensor(out=rp4[:, 2], in0=t2, in1=cs_, op=ALU.mult), reads=yk + ["cos4"], writes=[("rp4", 2)])
                g.op("pool", lambda e: e.tensor_tensor(out=rp4[:, 3], in0=t1, in1=sn_, op=ALU.mult), reads=yk + ["sin4"], writes=[("rp4", 3)])
                g.op("pool", lambda e: e.tensor_tensor(out=yb16[:, :, 0:8], in0=rp4[:, 0], in1=rp4[:, 1], op=ALU.subtract),
                     reads=[("rp4", 0), ("rp4", 1), "yb4"], writes=["yb4"])
                g.op("pool", lambda e: e.tensor_tensor(out=yb16[:, :, 8:16], in0=rp4[:, 2], in1=rp4[:, 3], op=ALU.add),
                     reads=[("rp4", 2), ("rp4", 3), "yb4"], writes=["yb4"])
                yield
                yield
                proj_p2(hp, tg)
                yield

            def proj_p2(hp, tg):
                par = hp % 2
                for tl in range(4):
                    g.op("pe", lambda e, tl=tl: e.transpose(psT[:, tl * 128:(tl + 1) * 128], yb4[:, tl * 256:tl * 256 + 128],
                                                            identb[:, :]), reads=["yb4", "identb"], writes=[PST])
                    g.op("pe", lambda e, tl=tl: e.transpose(psT[:, 512 + tl * 128:512 + (tl + 1) * 128],
                                                            yb4[:, tl * 256 + 128:tl * 256 + 256], identb[:, :]),
                         reads=["yb4", "identb"], writes=[PST])
                qs = slice(tg * 512, (tg + 1) * 512)
                g.op("act", lambda e: e.copy(out=aqT[par][:, qs], in_=psT[:, 0:512]), reads=[PST], writes=[("aqT", par, tg)])
                g.op("dve", lambda e: e.tensor_copy(out=akT[par][:, qs], in_=psT[:, 512:1024]), reads=[PST], writes=[("akT", par, tg)])

            ecnt = 0
            ocnt = 0
            wa_cur = load_wa(0)
            for tg in range(4):
                for _ in proj_p1(0, tg, *wa_cur):
                    pass
            for hp in range(4):
                par = hp % 2
                wa_nxt = load_wa(hp + 1) if hp < 3 else None
                def all_parts(hp=hp, wa_nxt=wa_nxt):
                    if wa_nxt is None:
                        return
                    for tg in range(4):
                        yield from proj_p1(hp + 1, tg, *wa_nxt)
                parts = all_parts()
                steps = []
                seg_end = []
                for hl in range(2):
                    for qb in range(4):
                        kts = list(range(max(0, qb * 4 - 8), min(15, qb * 4 + 11) + 1))
                        po = 3
                        ocnt += 1
                        for n_, kt in enumerate(kts):
                            steps.append((hl, qb, kt, n_, n_ == len(kts) - 1, po, (ecnt + len(steps)) % NSB))
                        seg_end.append(len(steps) - 1)
                ecnt += len(steps)
                LA = 2

                def emit_score(si, hp=hp, par=par):
                    hl, qb, kt, n_, last, po, pS = steps[si]
                    rows = slice(hl * 64, (hl + 1) * 64)
                    qs = slice(qb * 512, (qb + 1) * 512)
                    ks = slice(kt * 128, (kt + 1) * 128)
                    j0 = MASK_C - (kt * 128 - qb * 512)
                    bk = SBANK[pS]
                    g.op("pe", lambda e: e.matmul(ps[bk][:, :], lhsT=akT[par][rows, ks], rhs=aqT[par][rows, qs], start=True, stop=True),
                         reads=[("akT", par, kt // 4), ("aqT", par, qb)], writes=[("ps", bk)])
                    g.op("act", lambda e: e.activation(out=Eb[pS], in_=ps[bk][:, :], func=AF.Exp),
                         reads=[("ps", bk)], writes=[("Eb", pS)])
                    g.op("dve", lambda e: e.tensor_tensor(out=Eb[pS], in0=Eb[pS], in1=maskT[:, j0:j0 + 512], op=ALU.mult),
                         reads=[("Eb", pS), "maskT"], writes=[("Eb", pS)])

                def emit_pv(si, hp=hp, par=par):
                    hl, qb, kt, n_, last, po, pS = steps[si]
                    rows = slice(hl * 64, (hl + 1) * 64)
                    qs = slice(qb * 512, (qb + 1) * 512)
                    g.op("pe", lambda e: e.matmul(ps[po][:, :], lhsT=av2[par][:, kt, hl, :], rhs=Pm[pS], start=(n_ == 0), stop=last),
                         reads=[("av2", par, kt // 4), ("av2ones", par), ("Eb", pS)], writes=[("ps", po)])
                    if last:
                        rr = Pm[pS]
                        g.op("act", lambda e: e.activation(out=rd[po % 2][0:64, :], in_=ps[po][64:128, :], func=AF.Ln),
                             reads=[("ps", po)], writes=[("rd", po % 2)])
                        g.op("act", lambda e: e.activation(out=rd[po % 2][0:64, :], in_=rd[po % 2][0:64, :], func=AF.Exp, scale=-1.0),
                             reads=[("rd", po % 2)], writes=[("rd", po % 2)])
                        g.op("dve", lambda e: e.tensor_tensor(out=mixh[rows, hp, qs], in0=ps[po][0:64, :], in1=rd[po % 2][0:64, :],
                                                              op=ALU.mult),
                             reads=[("ps", po), ("rd", po % 2)], writes=[("mixh", hp, qb)])
                for s0 in range(0, len(steps) + LA, 2):
                    for si in (s0, s0 + 1):
                        if si < len(steps):
                            emit_score(si)
                    for si in (s0, s0 + 1):
                        if LA <= si < len(steps) + LA:
                            emit_pv(si - LA)
                    if (s0 // 2) % 3 == 2 or True:
                        if (s0 // 2) % 2 == 1:
                            next(parts, None)
                    else:
                        pass
                for _ in parts:
                    pass
            dbg("mixa", mixh, [128, 4, S], BF16, [("mixh", k_, q_) for k_ in range(4) for q_ in range(4)], b)
            wout_half(1)
            ar.release(m2)
            g.barrier()

            m3 = ar.mark()
            G = ar.alloc(16 * 16, F32).rearrange("p (t n) -> p t n", t=16)
            G4 = G.rearrange("p t (a h) -> p t a h", a=4)
            LF = ar.alloc(16 * 8, F32).rearrange("p (t n) -> p t n", t=16)
            LF4 = LF.rearrange("p t (a h) -> p t a h", a=2)
            sB = ar.alloc(16 * 16, F32).rearrange("p (t n) -> p t n", t=16)
            T1 = ar.alloc(16 * 8, F32).rearrange("p (t n) -> p t n", t=16)
            T2 = T1
            Call = ar.alloc(16 * 8, F32).rearrange("p (t n) -> p t n", t=16)
            Aall = ar.alloc(16 * 8, F32).rearrange("p (t n) -> p t n", t=16)
            EBa = ar.alloc(16 * 8, F32).rearrange("p (t n) -> p t n", t=16)
            EBH = ar.alloc(16 * 8, F32).rearrange("p (t n) -> p t n", t=16)
            mqT = ar.alloc(S, BF16)
            mkT = ar.alloc(S, BF16)
            ktok = ar.alloc(16 * 128, BF16).rearrange("p (t n) -> p t n", t=16)
            V1 = ar.alloc(16 * 2 * 130, BF16).rearrange("p (t h n) -> p t h n", t=16, h=2)
            sgb = [ar.alloc(256, BF16) for _ in range(2)]
            hacc = ar.alloc(16 * 256, F32).rearrange("p (t h n) -> p t h n", t=16, h=2)
            etm = [ar.alloc(256, F32) for _ in range(2)]
            Cst = ar.alloc(2 * 130, F32).rearrange("p (d n) -> p d n", d=2)
            Cbf = ar.alloc(2 * 130, BF16).rearrange("p (d n) -> p d n", d=2)
            Sm = [ar.alloc(128, BF16) for _ in range(8)]
            vw = [ar.alloc(130, BF16) for _ in range(8)]
            dn = [ar.alloc(2, F32) for _ in range(8)]
            hsq = [ar.alloc(256, F32)] * 2
            hst = [ar.alloc(2, F32) for _ in range(2)]
            hy = [ar.alloc(256, F32) for _ in range(2)]
            hyb = [ar.alloc(256, BF16) for _ in range(2)]
            g.op("dve", lambda e: e.memset(V1[:, :, :, 128:130], 1.0), writes=["V1ones"])

            for mp in range(2):
                sw = wload([win_v[:, :, mp * 128:(mp + 1) * 128], win_v[:, :, 256 + mp * 128:256 + (mp + 1) * 128]],
                           [lambda sl, q=q: sl[:, 0:NCH * 256].rearrange("p (c n) -> p c n", c=NCH)[:, :, q * 128:(q + 1) * 128]
                            for q in range(2)])
                wq = ring[:, sw, 0:NCH * 256].rearrange("p (c n) -> p c n", c=NCH)
                cnt = 0
                for q in range(2):
                    for blk in range(4):
                        pp = 5 + (cnt % 2)
                        cnt += 1
                        tok = slice(blk * 512, (blk + 1) * 512)
                        for c in range(NCH):
                            g.op("pe", lambda e, c=c, q=q, pp=pp, tok=tok, wq=wq: e.matmul(
                                ps[pp][:, :], lhsT=wq[:, c, q * 128:(q + 1) * 128], rhs=h2T[:, c, tok],
                                start=(c == 0), stop=(c == NCH - 1)),
                                reads=[("ring", sw), ("h2T", c, blk)], writes=[("ps", pp)])
                        if q == 0:
                            g.op("act", lambda e, pp=pp, tok=tok: e.copy(out=mqT[:, tok], in_=ps[pp][:, :]),
                                 reads=[("ps", pp)], writes=[("mqT", blk)])
                        else:
                            g.op("act", lambda e, pp=pp, tok=tok: e.mul(out=mkT[:, tok], in_=ps[pp][:, :], mul=0.125),
                                 reads=[("ps", pp)], writes=[("mkT", blk)])
                s1 = wload([win_v[:, :, 256 + mp * 128:256 + (mp + 1) * 128], win_v[:, :, 512 + mp * 256:512 + (mp + 1) * 256],
                            win_v[:, :, 1536:1552]],
                           [lambda sl: sl[:, 0:NCH * 400].rearrange("p (c n) -> p c n", c=NCH)[:, :, 0:128],
                            lambda sl: sl[:, 0:NCH * 400].rearrange("p (c n) -> p c n", c=NCH)[:, :, 128:384],
                            lambda sl: sl[:, 0:NCH * 400].rearrange("p (c n) -> p c n", c=NCH)[:, :, 384:400]])
                w1 = ring[:, s1, 0:NCH * 400].rearrange("p (c n) -> p c n", c=NCH)
                for tt in range(16):
                    ts_ = slice(tt * 128, (tt + 1) * 128)
                    blk = tt // 4
                    pp = 5 + (tt % 2)
                    for c in range(NCH):
                        g.op("pe", lambda e, c=c, ts_=ts_, w1=w1, pp=pp: e.matmul(
                            ps[pp][:, 0:400], lhsT=h2T[:, c, ts_], rhs=w1[:, c, :], start=(c == 0), stop=(c == NCH - 1)),
                            reads=[("ring", s1), ("h2T", c, blk)], writes=[("ps", pp)])
                    g.op("act", lambda e, tt=tt, pp=pp: e.mul(out=ktok[:, tt, :], in_=ps[pp][:, 0:128], mul=0.125),
                         reads=[("ps", pp)], writes=[("ktok", tt)])
                    g.op("act", lambda e, tt=tt, pp=pp: e.copy(out=V1[:, tt, :, 0:128],
                                                               in_=ps[pp][:, 128:384].rearrange("p (h n) -> p h n", h=2)),
                         reads=[("ps", pp)], writes=[("V1", tt)])
                    if mp == 0:
                        g.op("dve", lambda e, tt=tt, pp=pp: e.tensor_tensor(out=G[:, tt, :], in0=ps[pp][:, 384:400],
                                                                           in1=gbias[:, :], op=ALU.add),
                             reads=[("ps", pp), "gbias"], writes=[("G", tt)])
                if mp == 0:
                    allG = [("G", tt) for tt in range(16)]
                    g.op("act", lambda e: e.activation(out=LF4, in_=G4[:, :, 1:4:2, :], func=AF.Exp, scale=-1.0),
                         reads=allG, writes=["LF"])
                    g.op("act", lambda e: e.activation(out=LF, in_=LF, func=AF.Ln, bias=1.0, scale=1.0),
                         reads=["LF"], writes=["LF"])
                    g.op("dve", lambda e: e.tensor_scalar(out=LF, in0=LF, scalar1=-1.0, scalar2=None, op0=ALU.mult),
                         reads=["LF"], writes=["LF"])
                    for tt in range(16):
                        o = tt * 16
                        g.op("pe", lambda e, tt=tt, o=o: e.matmul(ps[4][:, o:o + 4], lhsT=trif[:, :], rhs=LF4[:, tt, 0, :],
                                                                  start=True, stop=True, skip_group_check=True),
                             reads=["LF", "trif"], writes=[("ps", 4)])
                        g.op("pe", lambda e, tt=tt, o=o: e.matmul(ps[4][:, o + 4:o + 8], lhsT=trib[:, :], rhs=LF4[:, tt, 1, :],
                                                                  start=True, stop=True, skip_group_check=True),
                             reads=["LF", "trib"], writes=[("ps", 4)])
                        g.op("pe", lambda e, tt=tt, o=o: e.matmul(ps[4][:, o + 8:o + 16], lhsT=onesf[:, :], rhs=LF[:, tt, :],
                                                                  start=True, stop=True, skip_group_check=True),
                             reads=["LF", "onesf"], writes=[("ps", 4)])
                    g.op("act", lambda e: e.copy(out=sB, in_=ps[4][:, 0:256].rearrange("p (t n) -> p t n", t=16)),
                         reads=[("ps", 4)], writes=["sB"])
                    LI = G4[:, :, 0:4:2, :]
                    T1v = T1.rearrange("p t (a h) -> p t a h", a=2)
                    bc4 = sB[:, :, 0:8].rearrange("p t (a h) -> p t a h", a=2)
                    g.op("dve", lambda e: e.tensor_tensor(out=T1v, in0=LI, in1=bc4, op=ALU.subtract),
                         reads=allG + ["sB"], writes=["T1"])
                    g.op("dve", lambda e: e.scalar_tensor_tensor(out=T1, in0=sB[:, :, 8:16], scalar=0.5, in1=T1,
                                                                 op0=ALU.mult, op1=ALU.add), reads=["sB", "T1"], writes=["T1"])
                    g.op("act", lambda e: e.activation(out=Call, in_=T1, func=AF.Exp), reads=["T1"], writes=["Call"])
                    g.op("dve", lambda e: e.scalar_tensor_tensor(out=T2, in0=sB[:, :, 8:16], scalar=-0.5, in1=sB[:, :, 0:8],
                                                                 op0=ALU.mult, op1=ALU.add), reads=["sB"], writes=["T1"])
                    g.op("act", lambda e: e.activation(out=Aall, in_=T2, func=AF.Exp), reads=["T1"], writes=["Aall"])
                    g.op("act", lambda e: e.activation(out=EBa, in_=sB[:, :, 8:16], func=AF.Exp), reads=["sB"], writes=["EBa"])
                    g.op("act", lambda e: e.activation(out=EBH, in_=sB[:, :, 8:16], func=AF.Exp, scale=0.5),
                         reads=["sB"], writes=["EBH"])
                g.op("dve", lambda e: e.memset(Cst, 0.0), writes=[("Cst", d_, hl_) for d_ in range(2) for hl_ in range(2)])
                g.op("dve", lambda e: e.memset(Cbf, 0.0), writes=[("Cbf", d_, hl_) for d_ in range(2) for hl_ in range(2)])
                qcnt = [0]

                def chain_a(step, hl, dr, mp=mp):
                    head = mp * 2 + hl
                    rows = slice(hl * 64, (hl + 1) * 64)
                    tt = step if dr == 0 else 15 - step
                    ts_ = slice(tt * 128, (tt + 1) * 128)
                    j = dr * 4 + head
                    c_ = hl * 2 + dr
                    z = c_ * 2 + (step % 2)
                    pq = c_
                    g.op("pe", lambda e: e.matmul(ps[pq][:, 0:128], lhsT=mkT[rows, ts_], rhs=mqT[rows, ts_], start=True, stop=True),
                         reads=[("mkT", tt // 4), ("mqT", tt // 4)], writes=[("ps", pq)])
                    g.op("act", lambda e: e.activation(out=vw[z][:, 0:129], in_=V1[:, tt, hl, 0:129], func=AF.Identity,
                                                       scale=Call[:, tt, j:j + 1]),
                         reads=[("V1", tt), "V1ones", "Call"], writes=[("vw", z)])
                    yield
                    mk_ = mkf if dr == 0 else mkb
                    g.op("dve", lambda e: e.tensor_tensor(out=Sm[z], in0=ps[pq][:, 0:128], in1=mk_[:, :], op=ALU.mult),
                         reads=[("ps", pq), "mkf", "mkb"], writes=[("Sm", z)])
                    yield

                def chain_b(step, hl, dr, mp=mp):
                    head = mp * 2 + hl
                    rows = slice(hl * 64, (hl + 1) * 64)
                    tt = step if dr == 0 else 15 - step
                    ts_ = slice(tt * 128, (tt + 1) * 128)
                    j = dr * 4 + head
                    c_ = hl * 2 + dr
                    z = c_ * 2 + (step % 2)
                    pc = 4 + c_
                    pso = ps[pc][:, 256:385]
                    g.op("pe", lambda e: e.matmul(pso, lhsT=Sm[z], rhs=vw[z][:, 0:129], start=True, stop=False),
                         reads=[("Sm", z), ("vw", z)], writes=[("ps", pc)])
                    g.op("pe", lambda e: e.matmul(pso, lhsT=mqT[rows, ts_], rhs=Cbf[rows, dr, 0:129], start=False, stop=True),
                         reads=[("mqT", tt // 4), ("Cbf", dr, hl)], writes=[("ps", pc)])
                    g.op("pe", lambda e: e.matmul(ps[pc][rows, 0:129], lhsT=ktok[:, tt, rows], rhs=vw[z][:, 0:129], start=True, stop=True,
                                                  skip_group_check=True),
                         reads=[("ktok", tt), ("vw", z)], writes=[("ps", pc)])
                    yield
                    g.op("dve", lambda e: e.tensor_scalar(out=Cst[rows, dr, 0:129], in0=Cst[rows, dr, 0:129],
                                                          scalar1=EBa[rows, tt, j:j + 1], scalar2=None, op0=ALU.mult),
                         reads=[("Cst", dr, hl), "EBa"], writes=[("Cst", dr, hl)])
                    acol = Aall[:, tt, j:j + 1]
                    g.op("act", lambda e: e.activation(out=dn[z][:, 0:1], in_=ps[pc][:, 384:385], func=AF.Abs, scale=acol),
                         reads=[("ps", pc), "Aall"], writes=[("dn", z)])
                    yield
                    g.op("dve", lambda e: e.scalar_tensor_tensor(out=Cst[rows, dr, 0:129], in0=ps[pc][rows, 0:129],
                                                                 scalar=EBH[rows, tt, j:j + 1], in1=Cst[rows, dr, 0:129],
                                                                 op0=ALU.mult, op1=ALU.add),
                         reads=[("ps", pc), ("Cst", dr, hl), "EBH"], writes=[("Cst", dr, hl)])
                    yield
                    if step < 15:
                        tn = tt + 1 if dr == 0 else tt - 1
                        g.op("act", lambda e: e.activation(out=Cbf[rows, dr, 0:129], in_=Cst[rows, dr, 0:129], func=AF.Identity,
                                                           scale=EBH[rows, tn, j:j + 1]),
                             reads=[("Cst", dr, hl), "EBH"], writes=[("Cbf", dr, hl)])
                    g.op("dve", lambda e: e.tensor_scalar(out=dn[z][:, 0:1], in0=dn[z][:, 0:1], scalar1=1.0, scalar2=None, op0=ALU.max),
                         reads=[("dn", z)], writes=[("dn", z)])
                    yield
                    g.op("dve", lambda e: e.reciprocal(out=dn[z][:, 0:1], in_=dn[z][:, 0:1]), reads=[("dn", z)], writes=[("dn", z)])
                    yield
                    g.op("dve", lambda e: e.tensor_tensor(out=dn[z][:, 1:2], in0=dn[z][:, 0:1], in1=acol, op=ALU.mult),
                         reads=[("dn", z), "Aall"], writes=[("dn", z)])
                    yield
                    is_first = (dr == 0 and tt <= 15 - tt) or (dr == 1 and (15 - tt) < tt)
                    if is_first:
                        g.op("act", lambda e: e.activation(out=hacc[:, tt, hl, :], in_=ps[pc][:, 256:384], func=AF.Identity,
                                                           scale=dn[z][:, 1:2]),
                             reads=[("ps", pc), ("dn", z)], writes=[("hacc", tt, hl)])
                    else:
                        g.op("dve", lambda e: e.scalar_tensor_tensor(out=hacc[:, tt, hl, :], in0=ps[pc][:, 256:384], scalar=dn[z][:, 1:2],
                                                                     in1=hacc[:, tt, hl, :], op0=ALU.mult, op1=ALU.add),
                             reads=[("ps", pc), ("dn", z), ("hacc", tt, hl)], writes=[("hacc", tt, hl)])
                    yield

                def round_robin(gens):
                    gens = list(gens)
                    while gens:
                        alive = []
                        for ge in gens:
                            try:
                                next(ge)
                                alive.append(ge)
                            except StopIteration:
                                pass
                        gens = alive

                chains = [(hl_, dr_) for hl_ in range(2) for dr_ in range(2)]
                round_robin([chain_a(0, hl_, dr_) for hl_, dr_ in chains])
                for step in range(16):
                    if step < 15:
                        round_robin([chain_a(step + 1, hl_, dr_) for hl_, dr_ in chains])
                    round_robin([chain_b(step, hl_, dr_) for hl_, dr_ in chains])
                s3 = wload([win_v[:, :, 1024 + mp * 256:1024 + (mp + 1) * 256]],
                           [lambda sl: sl[:, 0:NCH * 256].rearrange("p (c n) -> p c n", c=NCH)])
                w3 = ring[:, s3, 0:NCH * 256].rearrange("p (c n) -> p c n", c=NCH)
                for tt in range(16):
                    u = tt % 2
                    ts_ = slice(tt * 128, (tt + 1) * 128)
                    pp = 5 + (tt % 2)
                    for c in range(NCH):
                        g.op("pe", lambda e, c=c, ts_=ts_, w3=w3, pp=pp: e.matmul(
                            ps[pp][:, 0:256], lhsT=h2T[:, c, ts_], rhs=w3[:, c, :], start=(c == 0), stop=(c == NCH - 1)),
                            reads=[("ring", s3), ("h2T", c, tt // 4)], writes=[("ps", pp)])
                    g.op("act", lambda e, u=u, pp=pp: e.activation(out=etm[u], in_=ps[pp][:, 0:256], func=AF.Exp, scale=-1.0),
                         reads=[("ps", pp)], writes=[("etm", u)])
                    g.op("dve", lambda e, u=u: e.tensor_scalar(out=etm[u], in0=etm[u], scalar1=1.0, scalar2=None, op0=ALU.add),
                         reads=[("etm", u)], writes=[("etm", u)])
                    g.op("dve", lambda e, u=u: e.reciprocal(out=etm[u], in_=etm[u]),
                         reads=[("etm", u)], writes=[("etm", u)])
                    hk = [("hacc", tt, 0), ("hacc", tt, 1)]
                    hv = hacc[:, tt, :, :]
                    h3 = hsq[u].rearrange("p (h n) -> p h n", h=2)
                    y3 = hy[u].rearrange("p (h n) -> p h n", h=2)
                    g.op("dve", lambda e, hv=hv, h3=h3: e.tensor_tensor(out=h3, in0=hv, in1=hv, op=ALU.mult),
                         reads=hk, writes=["hsq"])
                    g.op("dve", lambda e, u=u, h3=h3: e.tensor_reduce(out=hst[u], in_=h3, axis=AX.X, op=ALU.add),
                         reads=["hsq"], writes=[("hst", u)])
                    g.op("act", lambda e, u=u: e.activation(out=hst[u], in_=hst[u], func=AF.Ln, bias=EPS, scale=1.0 / 128),
                         reads=[("hst", u)], writes=[("hst", u)])
                    g.op("act", lambda e, u=u: e.activation(out=hst[u], in_=hst[u], func=AF.Exp, scale=-0.5),
                         reads=[("hst", u)], writes=[("hst", u)])
                    for hl in range(2):
                        head = mp * 2 + hl
                        g.op("dve", lambda e, u=u, hl=hl, head=head, hv=hv, y3=y3: e.scalar_tensor_tensor(
                            out=y3[:, hl, :], in0=hv[:, hl, :], scalar=hst[u][:, hl:hl + 1], in1=gmh[:, head * 128:(head + 1) * 128],
                            op0=ALU.mult, op1=ALU.mult), reads=hk + [("hst", u), "gmh"], writes=[("hy", u, hl)])
                    g.op("dve", lambda e, u=u, tt=tt: e.tensor_tensor(out=hyb[u], in0=hy[u], in1=etm[u], op=ALU.mult),
                         reads=[("hy", u, 0), ("hy", u, 1), ("etm", u)], writes=[("hyb", u)])
                    def fin_tr(tt, mp=mp):
                        u = tt % 2
                        ts_ = slice(tt * 128, (tt + 1) * 128)
                        for hl in range(2):
                            g.op("pe", lambda e, hl=hl: e.transpose(psT[:, 256 + hl * 128:256 + (hl + 1) * 128],
                                                                    hyb[u][:, hl * 128:(hl + 1) * 128], identb[:, :]),
                                 reads=[("hyb", u), "identb"], writes=[PST])
                        g.op("act", lambda e: e.copy(
                            out=mixh[:, mp * 2:mp * 2 + 2, ts_], in_=psT[:, 256:512].rearrange("p (h n) -> p h n", h=2)),
                            reads=[PST], writes=[("mixh", mp * 2, tt // 4), ("mixh", mp * 2 + 1, tt // 4)])
                    if tt >= 1:
                        fin_tr(tt - 1)
                    if tt == 15:
                        fin_tr(15)
            dbg("mixm", mixh, [128, 4, S], BF16, [("mixh", k_, q_) for k_ in range(4) for q_ in range(4)], b)
            dbg("G", G, [128, 16, 16], F32, [("G", t_) for t_ in range(16)], b)
            dbg("LF", LF, [128, 16, 8], F32, ["LF"], b)
            dbg("sB", sB, [128, 16, 16], F32, ["sB"], b)
            dbg("Call", Call, [128, 16, 8], F32, ["Call"], b)
            dbg("Aall", Aall, [128, 16, 8], F32, ["Aall"], b)
            dbg("hacc", hacc, [128, 16, 2, 128], F32, [("hacc", t_, h_) for t_ in range(16) for h_ in range(2)], b)
            wout_half(0)
            ar.release(m0)
            g.barrier()

        def final(b, raw=False):
            m0 = ar.mark()
            outs = []
            if raw:
                for c in range(NCH):
                    outs.append(g.dma("sp", out_d[b, c * 128:(c + 1) * 128, :], xT[:, c, :],
                                      reads=[("xT", c, blk) for blk in range(4)]))
                return outs
            rstd = ar.alloc(S, F32)
            sq = ar.alloc(NCH * 512, BF16).rearrange("p (c n) -> p c n", c=NCH)
            ob = [ar.alloc(512, F32) for _ in range(4)]
            rms_rstd(b, rstd, sq)
            n = 0
            for blk in range(4):
                tok = slice(blk * 512, (blk + 1) * 512)
                for c in range(NCH):
                    o_ = ob[n % 4]
                    g.op("dve", lambda e, c=c, tok=tok, o_=o_: e.scalar_tensor_tensor(
                        out=o_, in0=xT[:, c, tok], scalar=g4[:, 3, c:c + 1], in1=rstd[:, tok], op0=ALU.mult, op1=ALU.mult),
                        reads=[("xT", c, blk), ("rstd", blk), "g4"], writes=[("ob", n % 4)])
                    outs.append(g.dma("sp", out_d[b, c * 128:(c + 1) * 128, tok], o_, reads=[("ob", n % 4)]))
                    n += 1
            ar.release(m0)
            g.barrier()
            return outs

        outs = []
        adaln(list(range(0, 6)))
        derive(0)
        for b in range(2):
            load_x(b)
            ffn(b, 0, 0)
            if b == 0:
                adaln(list(range(6, 18)))
                derive(1)
                derive(2)
            if stage >= 2:
                mixer(b)
            if stage >= 3:
                ffn(b, 2, 1)
            outs += final(b, raw=(stage < 3))
        g.emit(final_wait_ops=outs + dbg_outs)
    return nc


def _consts():
    p = np.arange(128)[:, None]
    j = np.arange(MASK_W)[None, :]
    dlt = p - j + MASK_C
    a = np.abs(dlt)
    m = (a <= 64).astype(np.float32) + ((dlt % 4 == 0) & (a <= 256)) + ((dlt % 16 == 0) & (a <= 1024))
    maskT = m.astype(ml_dtypes.bfloat16)
    identb = np.eye(128, dtype=np.float32).astype(ml_dtypes.bfloat16)
    u = np.arange(128)[:, None]
    t = np.arange(128)[None, :]
    trif = (u <= t).astype(np.float32)
    trib = (u >= t).astype(np.float32)
    maskf = (u <= t).astype(np.float32).astype(ml_dtypes.bfloat16)
    maskb = (u >= t).astype(np.float32).astype(ml_dtypes.bfloat16)
    half = 8
    inv_freq = (500000.0 ** (-2.0 * np.arange(half, dtype=np.float32) / 16.0)).astype(np.float32)
    pos = np.arange(S, dtype=np.float32)
    ang = (pos[:, None] * inv_freq[None, :]).astype(np.float32)
    cos = np.cos(ang).astype(np.float32).reshape(16, 128, 8).transpose(1, 0, 2)
    sin = np.sin(ang).astype(np.float32).reshape(16, 128, 8).transpose(1, 0, 2)
    cos4 = np.ascontiguousarray(np.broadcast_to(cos[:, :, None, :], (128, 16, 4, 8))).astype(np.float32)
    sin4 = np.ascontiguousarray(np.broadcast_to(sin[:, :, None, :], (128, 16, 4, 8))).astype(np.float32)
    return dict(maskT=maskT, identb=identb, trif=trif, trib=trib, maskf=maskf, maskb=maskb, cos4=cos4, sin4=sin4)


def _cols(v):
    return np.ascontiguousarray(np.asarray(v, np.float32).reshape(-1, 128).T)


_NC_CACHE = {}


def kernel(x, c, w_ada, b_ada, g_ffn1, w_gu1, w_down1, g_mix, w_in, gate_bias, g_q, g_k, g_mh,
           w_out, g_ffn2, w_gu2, w_down2, g_final):
    stage = int(os.environ.get("MK_STAGE", "3"))
    x = np.asarray(x, np.float32)
    c = np.asarray(c, np.float32)
    if stage not in _NC_CACHE:
        _NC_CACHE[stage] = build_program(stage)
    nc = _NC_CACHE[stage]
    consts = _consts()
    shared = dict(
        w_ada=np.ascontiguousarray(np.asarray(w_ada, np.float32)[0]),
        b_adaT=_cols(np.asarray(b_ada)[0]),
        g4=np.ascontiguousarray(np.stack([_cols(np.asarray(v)[0]) for v in (g_ffn1, g_mix, g_ffn2, g_final)], axis=1)),
        w_gu1=np.ascontiguousarray(np.asarray(w_gu1, np.float32)[0]),
        w_gu2=np.ascontiguousarray(np.asarray(w_gu2, np.float32)[0]),
        w_down1=np.ascontiguousarray(np.asarray(w_down1, np.float32)[0]),
        w_down2=np.ascontiguousarray(np.asarray(w_down2, np.float32)[0]),
        w_in=np.ascontiguousarray(np.asarray(w_in, np.float32)[0]),
        w_out=np.ascontiguousarray(np.asarray(w_out, np.float32)[0]),
        gbias_rep=np.ascontiguousarray(np.broadcast_to(np.asarray(gate_bias, np.float32)[0].reshape(1, 16), (128, 16))),
        gqk_rep=np.ascontiguousarray(np.broadcast_to(
            np.stack([np.asarray(g_q, np.float32)[0]] * 2 + [np.asarray(g_k, np.float32)[0]] * 2)[None], (128, 4, 64))),
        gmh_rep=np.ascontiguousarray(np.broadcast_to(np.asarray(g_mh, np.float32)[0][None, :], (128, 512))),
        **consts,
    )
    in_maps = []
    for i in range(8):
        m = dict(shared)
        m["xT"] = np.ascontiguousarray(x[2 * i:2 * i + 2].transpose(0, 2, 1))
        m["cT"] = np.ascontiguousarray(c[2 * i:2 * i + 2].reshape(2, NCH, 128).transpose(2, 1, 0))
        in_maps.append(m)
    res = run_bass_kernel_spmd(nc, in_maps, core_ids=list(range(8)))
    out = np.empty((16, S, D), np.float32)
    for i in range(8):
        out[2 * i:2 * i + 2] = res.results[i]["outT"].transpose(0, 2, 1)
    return out
```
